# Optimizing a Trainium2 kernel written in Bass

```python
import math
import jax, jax.numpy as jnp
from jax import lax
import numpy as np

D_MODEL = 4096
BATCH = 1
SEQ = 8192
DEPTH = 2

CHUNK = 64
D_MIX = D_MODEL
GDN_HEAD_DIM = 128
GDN_WIDTH = (3 * D_MIX) // 4
GDN_HEADS = GDN_WIDTH // GDN_HEAD_DIM
CONV_WIDTH = 4
DT_MIN = 1e-3
DT_MAX = 1e-1
S5_WIDTH = D_MIX - GDN_WIDTH
S5_GROUP = 16
S5_GROUPS = S5_WIDTH // S5_GROUP
S5_STATE = 64
PROJ_COLS = 4 * GDN_WIDTH + 2 * GDN_HEADS + S5_WIDTH
N_GROUPS = 4
EXPERTS_PER_GROUP = 8
N_EXPERTS = N_GROUPS * EXPERTS_PER_GROUP
TOP_K = 2
D_EXPERT = D_MODEL // 8
MOE_BLOCK = 128
DN_ALPHA = (2 * DEPTH) ** 0.25
DN_BETA = (8 * DEPTH) ** -0.25
LN_EPS = 1e-5
RMS_EPS = 1e-6
L2_EPS = 1e-6

kernel_name = "hybrid_gdn_s5_hmoe_deepnorm"


def layer_norm(x, g, b):
    xf = x.astype(jnp.float32)
    mu = jnp.mean(xf, axis=-1, keepdims=True)
    var = jnp.mean(jnp.square(xf - mu), axis=-1, keepdims=True)
    return ((xf - mu) * lax.rsqrt(var + LN_EPS) * g + b).astype(x.dtype)


def causal_depthwise_conv(x, w):
    k = w.shape[0]
    xp = jnp.pad(x, ((0, 0), (k - 1, 0), (0, 0)))
    return lax.conv_general_dilated(xp, w[:, None, :], window_strides=(1,), padding='VALID',
                                    dimension_numbers=('NWC', 'WIO', 'NWC'),
                                    feature_group_count=x.shape[-1])


def chunk_gated_delta_rule(q, k, v, g, beta):
    bsz, l, h, dk = q.shape
    dv = v.shape[-1]
    n = l // CHUNK

    def to_chunks(t):
        t = t.reshape((bsz, n, CHUNK, h) + t.shape[3:])
        return jnp.moveaxis(t, 3, 1)

    q, k, v, g, beta = (to_chunks(t) for t in (q, k, v, g, beta))
    q = q * (dk ** -0.5)
    decay = jnp.cumsum(g, axis=-1)
    causal = jnp.tril(jnp.ones((CHUNK, CHUNK), dtype=bool))
    strict = jnp.tril(jnp.ones((CHUNK, CHUNK), dtype=bool), k=-1)
    diff = decay[..., :, None] - decay[..., None, :]
    gamma = jnp.where(causal, jnp.exp(jnp.where(causal, diff, 0.0)), 0.0)
    k_beta = k * beta[..., None]
    kk = jnp.einsum('bhnid,bhnjd->bhnij', k_beta, k) * gamma
    t_mat = jnp.eye(CHUNK, dtype=q.dtype) + jnp.where(strict, kk, 0.0)
    u = lax.linalg.triangular_solve(t_mat, v * beta[..., None], left_side=True, lower=True,
                                    unit_diagonal=True)
    w = lax.linalg.triangular_solve(t_mat, k_beta * jnp.exp(decay)[..., None], left_side=True,
                                    lower=True, unit_diagonal=True)
    qk = jnp.einsum('bhnid,bhnjd->bhnij', q, k) * gamma
    decay_last = decay[..., -1]
    k_tail = k * jnp.exp(decay_last[..., None] - decay)[..., None]
    q_dec = q * jnp.exp(decay)[..., None]

    def step(s, inp):
        qk_c, u_c, w_c, qd_c, kt_c, dl_c = inp
        v_new = u_c - jnp.einsum('bhcd,bhde->bhce', w_c, s)
        o = jnp.einsum('bhcd,bhde->bhce', qd_c, s) + jnp.einsum('bhij,bhje->bhie', qk_c, v_new)
        s = s * jnp.exp(dl_c)[..., None, None] + jnp.einsum('bhcd,bhce->bhde', kt_c, v_new)
        return s, o

    xs = tuple(jnp.moveaxis(t, 2, 0) for t in (qk, u, w, q_dec, k_tail, decay_last))
    s0 = jnp.zeros((bsz, h, dk, dv), q.dtype)
    _, o = lax.scan(step, s0, xs)
    return jnp.transpose(o, (1, 0, 3, 2, 4)).reshape(bsz, l, h, dv)


def gated_deltanet(qkv, z, a, b, conv_w, a_log, dt_bias, norm_w):
    bsz, l = qkv.shape[:2]
    out_dtype = qkv.dtype
    f32 = jnp.float32
    qkv = jax.nn.silu(causal_depthwise_conv(qkv, conv_w)).astype(f32)
    q, k, v = jnp.split(qkv, 3, axis=-1)

    def heads(t):
        return t.reshape(bsz, l, GDN_HEADS, GDN_HEAD_DIM)

    q, k, v = heads(q), heads(k), heads(v)
    q = q * lax.rsqrt(jnp.sum(q * q, axis=-1, keepdims=True) + L2_EPS)
    k = k * lax.rsqrt(jnp.sum(k * k, axis=-1, keepdims=True) + L2_EPS)
    g = -jnp.exp(a_log.astype(f32)) * jax.nn.softplus(a.astype(f32) + dt_bias.astype(f32))
    beta = jax.nn.sigmoid(b.astype(f32))
    o = chunk_gated_delta_rule(q, k, v, g, beta)
    o = o * lax.rsqrt(jnp.mean(o * o, axis=-1, keepdims=True) + RMS_EPS) * norm_w.astype(f32)
    o = o * jax.nn.silu(heads(z.astype(f32)))
    return o.reshape(bsz, l, GDN_WIDTH).astype(out_dtype)


def s5_layer(u, lambda_re, lambda_im, b_re, b_im, c_re, c_im, d_skip, log_dt, w_glu):
    bsz, l = u.shape[:2]
    out_dtype = u.dtype
    f32 = jnp.float32
    uf = u.astype(f32).reshape(bsz, l, S5_GROUPS, S5_GROUP)
    lr, li = lambda_re.astype(f32), lambda_im.astype(f32)
    dt = jnp.exp(log_dt.astype(f32))[:, None]
    mag = jnp.exp(lr * dt)
    ab_re, ab_im = mag * jnp.cos(li * dt), mag * jnp.sin(li * dt)
    den = lr * lr + li * li
    nr, ni = ab_re - 1.0, ab_im
    coef_re = (nr * lr + ni * li) / den
    coef_im = (ni * lr - nr * li) / den
    br, bi = b_re.astype(f32), b_im.astype(f32)
    bb_re = coef_re[..., None] * br - coef_im[..., None] * bi
    bb_im = coef_re[..., None] * bi + coef_im[..., None] * br
    bu_re = jnp.einsum('blgh,gph->blgp', uf, bb_re)
    bu_im = jnp.einsum('blgh,gph->blgp', uf, bb_im)
    a_re = jnp.broadcast_to(ab_re, bu_re.shape)
    a_im = jnp.broadcast_to(ab_im, bu_re.shape)

    def combine(e1, e2):
        a1r, a1i, b1r, b1i = e1
        a2r, a2i, b2r, b2i = e2
        return (a2r * a1r - a2i * a1i, a2r * a1i + a2i * a1r,
                a2r * b1r - a2i * b1i + b2r, a2r * b1i + a2i * b1r + b2i)

    _, _, x_re, x_im = lax.associative_scan(combine, (a_re, a_im, bu_re, bu_im), axis=1)
    y = (jnp.einsum('blgp,ghp->blgh', x_re, c_re.astype(f32))
         - jnp.einsum('blgp,ghp->blgh', x_im, c_im.astype(f32)))
    y = y.reshape(bsz, l, S5_WIDTH) + d_skip.astype(f32) * uf.reshape(bsz, l, S5_WIDTH)
    y = jax.nn.gelu(y)
    y = y * jax.nn.sigmoid(y @ w_glu.astype(f32))
    return y.astype(out_dtype)


def hierarchical_moe(x, w_rg, b_rg, w_re, b_re, w_gate, w_up, w_down):
    bsz, l, d = x.shape
    f32 = jnp.float32
    n_tok = bsz * l
    xt = x.reshape(n_tok, d)
    grp_logits = (xt @ w_rg).astype(f32) + b_rg.astype(f32)
    grp_p, grp_idx = lax.top_k(jax.nn.softmax(grp_logits, axis=-1), 1)
    exp_logits = jnp.einsum('td,gde->tge', xt, w_re).astype(f32) + b_re.astype(f32)
    sel = exp_logits[jnp.arange(n_tok), grp_idx[:, 0]]
    top_logit, top_idx = lax.top_k(sel, TOP_K)
    top_w = jax.nn.softmax(top_logit, axis=-1) * grp_p
    expert_id = (grp_idx * EXPERTS_PER_GROUP + top_idx).reshape(-1)
    n_assign = n_tok * TOP_K
    tok_id = jnp.arange(n_assign, dtype=jnp.int32) // TOP_K
    wts = top_w.reshape(-1)
    order = jnp.argsort(expert_id)
    e_sorted = expert_id[order]
    counts = jnp.zeros((N_EXPERTS,), jnp.int32).at[expert_id].add(1)
    start = jnp.cumsum(counts) - counts
    padded = (counts + MOE_BLOCK - 1) // MOE_BLOCK * MOE_BLOCK
    pend = jnp.cumsum(padded)
    pstart = pend - padded
    dest = pstart[e_sorted] + (jnp.arange(n_assign, dtype=jnp.int32) - start[e_sorted])
    n_blocks = (n_assign + MOE_BLOCK - 1) // MOE_BLOCK + N_EXPERTS
    n_rows = n_blocks * MOE_BLOCK
    row_tok = jnp.zeros((n_rows,), jnp.int32).at[dest].set(tok_id[order])
    row_w = jnp.zeros((n_rows,), f32).at[dest].set(wts[order])
    blk_start = jnp.arange(n_blocks, dtype=jnp.int32) * MOE_BLOCK
    blk_expert = jnp.minimum(jnp.searchsorted(pend, blk_start, side='right'), N_EXPERTS - 1)
    xb = xt[row_tok].reshape(n_blocks, MOE_BLOCK, d)

    def expert_block(args):
        xblk, e = args
        hid = jax.nn.silu(xblk @ w_gate[e]) * (xblk @ w_up[e])
        return hid @ w_down[e]

    yb = lax.map(expert_block, (xb, blk_expert)).reshape(n_rows, d)
    y = jax.ops.segment_sum(yb * row_w[:, None].astype(yb.dtype), row_tok, num_segments=n_tok)
    return y.reshape(bsz, l, d)


def setup_inputs(seed: int = 0) -> dict:
    key = jax.random.key(seed)
    ks = jax.random.split(key, 32)
    f32 = jnp.float32

    def nrm(k, shape, scale):
        return jax.random.normal(k, shape, f32) * scale

    x = nrm(ks[0], (BATCH, SEQ, D_MODEL), 1.0)
    w_in = nrm(ks[1], (DEPTH, D_MODEL, PROJ_COLS), D_MODEL ** -0.5)
    gdn_conv_w = nrm(ks[2], (DEPTH, CONV_WIDTH, 3 * GDN_WIDTH), CONV_WIDTH ** -0.5)
    gdn_a_log = jnp.log(jax.random.uniform(ks[3], (DEPTH, GDN_HEADS), f32, 1.0, 16.0))
    dt = jnp.exp(jax.random.uniform(ks[4], (DEPTH, GDN_HEADS), f32, math.log(DT_MIN), math.log(DT_MAX)))
    gdn_dt_bias = dt + jnp.log(-jnp.expm1(-dt))
    gdn_norm_w = 1.0 + nrm(ks[5], (DEPTH, GDN_HEAD_DIM), 0.01)
    n_idx = jnp.arange(S5_STATE, dtype=f32)
    s5_lambda_re = -0.5 + nrm(ks[6], (DEPTH, S5_GROUPS, S5_STATE), 0.01)
    s5_lambda_im = math.pi * n_idx + nrm(ks[7], (DEPTH, S5_GROUPS, S5_STATE), 0.01)
    b_scale = (2 * S5_GROUP) ** -0.5
    s5_b_re = nrm(ks[8], (DEPTH, S5_GROUPS, S5_STATE, S5_GROUP), b_scale)
    s5_b_im = nrm(ks[9], (DEPTH, S5_GROUPS, S5_STATE, S5_GROUP), b_scale)
    c_scale = (2 * S5_STATE) ** -0.5
    s5_c_re = nrm(ks[10], (DEPTH, S5_GROUPS, S5_GROUP, S5_STATE), c_scale)
    s5_c_im = nrm(ks[11], (DEPTH, S5_GROUPS, S5_GROUP, S5_STATE), c_scale)
    s5_d = nrm(ks[12], (DEPTH, S5_WIDTH), 1.0)
    s5_log_dt = jax.random.uniform(ks[13], (DEPTH, S5_GROUPS), f32, math.log(DT_MIN), math.log(DT_MAX))
    s5_w_glu = nrm(ks[14], (DEPTH, S5_WIDTH, S5_WIDTH), S5_WIDTH ** -0.5)
    w_out = nrm(ks[15], (DEPTH, D_MIX, D_MODEL), DN_BETA * D_MIX ** -0.5)
    ln1_g = 1.0 + nrm(ks[16], (DEPTH, D_MODEL), 0.01)
    ln1_b = nrm(ks[17], (DEPTH, D_MODEL), 0.01)
    router_group_w = nrm(ks[18], (DEPTH, D_MODEL, N_GROUPS), D_MODEL ** -0.5)
    router_group_b = nrm(ks[19], (DEPTH, N_GROUPS), 0.01)
    router_expert_w = nrm(ks[20], (DEPTH, N_GROUPS, D_MODEL, EXPERTS_PER_GROUP), D_MODEL ** -0.5)
    router_expert_b = nrm(ks[21], (DEPTH, N_GROUPS, EXPERTS_PER_GROUP), 0.01)
    expert_w_gate = nrm(ks[22], (DEPTH, N_EXPERTS, D_MODEL, D_EXPERT), D_MODEL ** -0.5)
    expert_w_up = nrm(ks[23], (DEPTH, N_EXPERTS, D_MODEL, D_EXPERT), D_MODEL ** -0.5)
    expert_w_down = nrm(ks[24], (DEPTH, N_EXPERTS, D_EXPERT, D_MODEL), DN_BETA * D_EXPERT ** -0.5)
    ln2_g = 1.0 + nrm(ks[25], (DEPTH, D_MODEL), 0.01)
    ln2_b = nrm(ks[26], (DEPTH, D_MODEL), 0.01)
    return {"x": x, "w_in": w_in, "gdn_conv_w": gdn_conv_w, "gdn_a_log": gdn_a_log,
            "gdn_dt_bias": gdn_dt_bias, "gdn_norm_w": gdn_norm_w,
            "s5_lambda_re": s5_lambda_re, "s5_lambda_im": s5_lambda_im,
            "s5_b_re": s5_b_re, "s5_b_im": s5_b_im, "s5_c_re": s5_c_re, "s5_c_im": s5_c_im,
            "s5_d": s5_d, "s5_log_dt": s5_log_dt, "s5_w_glu": s5_w_glu, "w_out": w_out,
            "ln1_g": ln1_g, "ln1_b": ln1_b, "router_group_w": router_group_w,
            "router_group_b": router_group_b, "router_expert_w": router_expert_w,
            "router_expert_b": router_expert_b, "expert_w_gate": expert_w_gate,
            "expert_w_up": expert_w_up, "expert_w_down": expert_w_down,
            "ln2_g": ln2_g, "ln2_b": ln2_b}


def reference(x, w_in, gdn_conv_w, gdn_a_log, gdn_dt_bias, gdn_norm_w,
              s5_lambda_re, s5_lambda_im, s5_b_re, s5_b_im, s5_c_re, s5_c_im,
              s5_d, s5_log_dt, s5_w_glu, w_out, ln1_g, ln1_b,
              router_group_w, router_group_b, router_expert_w, router_expert_b,
              expert_w_gate, expert_w_up, expert_w_down, ln2_g, ln2_b):
    c_q = 3 * GDN_WIDTH
    c_z = 4 * GDN_WIDTH
    c_a = c_z + GDN_HEADS
    c_b = c_a + GDN_HEADS
    for i in range(DEPTH):
        proj = x @ w_in[i]
        y_gdn = gated_deltanet(proj[..., :c_q], proj[..., c_q:c_z], proj[..., c_z:c_a],
                               proj[..., c_a:c_b], gdn_conv_w[i], gdn_a_log[i],
                               gdn_dt_bias[i], gdn_norm_w[i])
        y_s5 = s5_layer(proj[..., c_b:], s5_lambda_re[i], s5_lambda_im[i], s5_b_re[i],
                        s5_b_im[i], s5_c_re[i], s5_c_im[i], s5_d[i], s5_log_dt[i], s5_w_glu[i])
        mix = jnp.concatenate([y_gdn, y_s5], axis=-1) @ w_out[i]
        x = layer_norm(DN_ALPHA * x + mix, ln1_g[i], ln1_b[i])
        ffn = hierarchical_moe(x, router_group_w[i], router_group_b[i], router_expert_w[i],
                               router_expert_b[i], expert_w_gate[i], expert_w_up[i],
                               expert_w_down[i])
        x = layer_norm(DN_ALPHA * x + ffn, ln2_g[i], ln2_b[i])
    return x
```

```python
import numpy as np
import ml_dtypes
from contextlib import ExitStack
import concourse.bass as bass
import concourse.mybir as mybir
from concourse.bass_utils import run_bass_kernel_spmd

F32 = mybir.dt.float32
BF16 = mybir.dt.bfloat16
I32 = mybir.dt.int32
AF = mybir.ActivationFunctionType
ALU = mybir.AluOpType
AX = mybir.AxisListType

D_MODEL = 4096
SEQ = 8192
DEPTH = 2
NCORES = 8
KC = D_MODEL // 128
GDN_HEADS = 24
HPC = 3
GDN_WIDTH = 3072
S5_WIDTH = 1024
S5_STATE = 64
GPC = 8
CH = 64
N_EXPERTS = 32
D_EXPERT = 512
DN_ALPHA = (2 * DEPTH) ** 0.25
LN_EPS = 1e-5
RMS_EPS = 1e-6
L2_EPS = 1e-6
CAP = 128
TWO_PI = float(2 * np.pi)
MAGIC = 12582912.0


class Ev:
    __slots__ = ("sem", "sid", "val", "eng")

    def __init__(self, sem, sid, val, eng):
        self.sem, self.sid, self.val, self.eng = sem, sid, val, eng


class Buf:
    def __init__(self, name, excl=False):
        self.name = name
        self.w = None
        self.r = {}
        self.excl = excl


class Sched:
    ROT = 8000
    NSLOT = 12

    def __init__(self, nc, es):
        self.nc, self.es = nc, es
        self.engs = dict(pe=nc.tensor, act=nc.scalar, dve=nc.vector, pool=nc.gpsimd, sp=nc.sync)
        self.nsem = 0
        self.cur = {}
        for e in self.engs:
            self.cur[e] = [self._newsem(e), 0]
        self.known = {e: {} for e in self.engs}
        self.slots = {}
        self.slot_i = {}
        self.nops = 0
        self.prev = {}

    def _newsem(self, tag):
        self.nsem += 1
        s = self.es.enter_context(self.nc.semaphore(f"s{self.nsem}_{tag}"))
        return (s, self.nsem)

    def _wait(self, e, ev):
        if ev is None:
            return
        if e == "pe" and ev.eng == "pe":
            return
        k = self.known[e]
        if k.get(ev.sid, -1) >= ev.val:
            return
        self.engs[e].wait_ge(ev.sem, ev.val)
        k[ev.sid] = ev.val

    def _deps(self, e, reads, writes):
        for b in reads:
            self._wait(e, b.w)
            if b.excl:
                for ev in list(b.r.values()):
                    self._wait(e, ev)
        for b in writes:
            self._wait(e, b.w)
            for ev in list(b.r.values()):
                self._wait(e, ev)

    def _commit(self, ev, reads, writes):
        for b in reads:
            if b.excl:
                b.w = ev
                b.r = {}
            else:
                b.r[ev.sid] = ev
        for b in writes:
            b.w = ev
            b.r = {}

    def op(self, e, fn, reads=(), writes=()):
        self._deps(e, reads, writes)
        ins = fn(self.engs[e])
        (sem, sid), cnt = self.cur[e]
        cnt += 1
        ins.then_inc(sem, 1)
        ev = Ev(sem, sid, cnt, e)
        self.cur[e][1] = cnt
        if cnt >= self.ROT:
            self.prev[e] = ev
            self.cur[e] = [self._newsem(e), 0]
        self._commit(ev, reads, writes)
        self.nops += 1
        return ev

    def dma(self, q, out, in_, reads=(), writes=(), **kw):
        if q not in self.slots:
            self.slots[q] = [[self._newsem("d" + q), 0, None] for _ in range(self.NSLOT)]
            self.slot_i[q] = 0
        i = self.slot_i[q]
        self.slot_i[q] = (i + 1) % self.NSLOT
        slot = self.slots[q][i]
        self._wait(q, slot[2])
        self._deps(q, reads, writes)
        ins = self.engs[q].dma_start(out=out, in_=in_, **kw)
        slot[1] += 16
        (sem, sid) = slot[0]
        ins.then_inc(sem, 16)
        ev = Ev(sem, sid, slot[1], "dma")
        slot[2] = ev
        self._commit(ev, reads, writes)
        self.nops += 1
        return ev

    def dma_ins(self, q, mk, reads=(), writes=()):
        if q not in self.slots:
            self.slots[q] = [[self._newsem("d" + q), 0, None] for _ in range(self.NSLOT)]
            self.slot_i[q] = 0
        i = self.slot_i[q]
        self.slot_i[q] = (i + 1) % self.NSLOT
        slot = self.slots[q][i]
        self._wait(q, slot[2])
        self._deps(q, reads, writes)
        ins = mk(self.engs[q])
        slot[1] += 16
        (sem, sid) = slot[0]
        ins.then_inc(sem, 16)
        ev = Ev(sem, sid, slot[1], "dma")
        slot[2] = ev
        self._commit(ev, reads, writes)
        return ev

    def barrier(self):
        evs = []
        for e in self.engs:
            (sem, sid), cnt = self.cur[e]
            if cnt > 0:
                evs.append(Ev(sem, sid, cnt, e))
            elif e in self.prev:
                evs.append(self.prev[e])
        for q in self.slots:
            for slot in self.slots[q]:
                if slot[2] is not None:
                    evs.append(slot[2])
        for e in self.engs:
            for ev in evs:
                self._wait(e, ev)

    def finish(self, bufs):
        for b in bufs:
            self._wait("sp", b.w)


class T:
    def __init__(self, t, name, excl=False):
        self.t = t
        self.b = Buf(name, excl)

    def __getitem__(self, k):
        return self.t[k]


_uid = [0]


class Ctx:
    def __init__(self, nc, es, S=None):
        self.nc, self.es = nc, es
        self.S = S if S is not None else Sched(nc, es)

    @property
    def n(self):
        return _uid[0]

    @n.setter
    def n(self, v):
        _uid[0] = v

    def sb(self, shape, dt=F32, name=None):
        self.n += 1
        nm = f"{name or 't'}_{self.n}"
        return T(self.es.enter_context(self.nc.sbuf_tensor(nm, list(shape), dt)), nm)

    def psum_banks(self, n=8):
        out = []
        for i in range(n):
            self.n += 1
            nm = f"ps{i}_{self.n}"
            out.append(T(self.es.enter_context(self.nc.psum_tensor(nm, [128, 512], F32)), nm, excl=True))
        return out


class View:
    def __init__(self, fn, b):
        self.fn, self.b = fn, b

    def __getitem__(self, k):
        return self.fn(k)


class BankPool:
    def __init__(self, banks):
        self.banks = list(banks)
        self.i = 0

    def get(self):
        b = self.banks[self.i]
        self.i = (self.i + 1) % len(self.banks)
        return b


GDN_COLS = 4 * HPC * 128 + 2 * HPC
NEG = -30000.0


def gdn_consts():
    c = np.zeros((128, 8, 128), np.float32)
    c[:, 0, :] = np.eye(128)
    i = np.arange(64)
    U = (i[:, None] <= i[None, :]).astype(np.float32)
    c[:64, 1, :64] = U
    c[:64, 2, :64] = -U
    c[:64, 3, :64] = (i[:, None] > i[None, :]).astype(np.float32)
    c[:, 4, :] = 1.0
    c[:64, 5, :64] = np.where(i[:, None] > i[None, :], 0.0, NEG)
    c[:64, 6, :64] = np.where(i[None, :] >= i[:, None], 0.0, NEG)
    return c.reshape(128, 8 * 128)


def emit_gdn(nc, S, L, x_bf16, xT, wg, convw, hp, cst, yT):
    NR = max(1, L // 1024)
    TR = min(L, 1024)
    NSB = L // 512
    xq = "sp" if x_bf16 else "pool"

    with ExitStack() as es:
        C = Ctx(nc, es, S)
        W = C.sb([128, KC, GDN_COLS], BF16, "W")
        XT = [C.sb([128, 8, 512], BF16, "XT") for _ in range(4)]
        CW = C.sb([128, 36], F32, "CW")
        HP = C.sb([128, 16], F32, "HP")
        CS = C.sb([128, 8, 128], F32, "CS")
        CSb = C.sb([128, 2, 128], BF16, "CSb")
        EAL = C.sb([128, 3], F32, "EAL")
        halo = C.sb([128, 9, 3], F32, "halo")
        raw = [C.sb([128, 515], F32, "raw") for _ in range(2)]
        cv = [C.sb([128, 512], F32, "cv") for _ in range(2)]
        actb = [C.sb([128, 512], F32, "actb") for _ in range(2)]
        sqb = [C.sb([128, 512], BF16, "sqb") for _ in range(2)]
        rnb = [C.sb([128, 512], F32, "rnb") for _ in range(2)]
        qn = [C.sb([128, 512], BF16, "qn") for _ in range(HPC)]
        kn = [C.sb([128, 512], BF16, "kn") for _ in range(HPC)]
        vT = [C.sb([128, 512], BF16, "vT") for _ in range(HPC)]
        zs = [C.sb([128, 512], F32, "zs") for _ in range(HPC)]
        abT = C.sb([8, 512], F32, "abT")
        yo = [C.sb([128, 512], BF16, "yo") for _ in range(HPC)]
        Sst = [C.sb([128, 128], F32, "S") for _ in range(HPC)]
        Sb = [C.sb([128, 128], BF16, "Sb") for _ in range(HPC)]
        PS = C.psum_banks(8)
        proj_banks = BankPool(PS[0:2])
        wk_banks = BankPool(PS[2:8])

        ident = CS[:, 0, :]
        identb = CSb[:, 0, :]
        onesb = CSb[:, 1, :]

        wv = wg.rearrange("(kc p) n -> p kc n", p=128)
        for j in range(8):
            S.dma("pool", W[:, 4 * j:4 * j + 4, :], wv[:, 4 * j:4 * j + 4, :], writes=[W.b])
        S.dma("sp", CW[:], convw, writes=[CW.b])
        S.dma("sp", HP[:], hp, writes=[HP.b])
        S.dma("sp", CS[:].rearrange("p a b -> p (a b)"), cst, writes=[CS.b])
        S.op("dve", lambda e: e.tensor_copy(CSb[:, 0, :], CS[:, 0, :]), reads=[CS.b], writes=[CSb.b])
        S.op("dve", lambda e: e.tensor_copy(CSb[:, 1, :], CS[:, 4, :]), reads=[CS.b], writes=[CSb.b])
        S.op("act", lambda e: e.activation(out=EAL[:], in_=HP[:, 0:3], func=AF.Exp), reads=[HP.b], writes=[EAL.b])
        S.op("dve", lambda e: e.memset(halo[:].rearrange("p a b -> p (a b)"), 0.0), writes=[halo.b])
        for h in range(HPC):
            S.op("dve", lambda e, h=h: e.memset(Sst[h][:], 0.0), writes=[Sst[h].b])
            S.op("dve", lambda e, h=h: e.memset(Sb[h][:], 0.0), writes=[Sb[h].b])

        xv = xT.rearrange("r (kc p) t -> r p kc t", p=128)

        def small(shape, dt=F32, name="w"):
            return C.sb(shape, dt, name)

        G = dict(
            x1=small([64, 3]), ex=small([64, 3]), sp=small([64, 3]), g=small([64, 3]), beta=small([64, 3]),
            edec=small([64, 3]), edlm=small([64, 3]), edl=small([128, 3]), bedec=small([64, 3]),
        )
        HW = []
        for h in range(HPC):
            HW.append(dict(
                Gb=small([64, 64]), gsa=small([64, 64]), gs=small([64, 64]), gTa=small([64, 64]), gT=small([64, 64]),
                ktok=small([64, 128], BF16), kbe=small([64, 128], BF16), ktail=small([64, 128], BF16),
                bv=small([64, 128], BF16), N=small([64, 64]),
                P=[small([64, 64]), small([64, 64])], Q=[small([64, 64]), small([64, 64])],
                X=small([64, 64]), Xb=small([64, 64], BF16), qkT=small([64, 64], BF16),
                nwT=small([128, 64], BF16), Lm=small([64, 128]), qd=small([128, 64], BF16),
                vn=small([64, 128], BF16), ssq=small([64, 1]), rt=small([64, 1]), rstd=small([64, 1]),
                on=small([64, 128]), junk=small([64, 128]),
            ))

        def proj_tile(c0, m):
            ps = proj_banks.get()
            for kc in range(KC):
                S.op("pe", lambda e, kc=kc: e.matmul(ps[0:m, :], W[:, kc, c0:c0 + m], XT[kc // 8][:, kc % 8, :],
                                                     start=(kc == 0), stop=(kc == KC - 1)),
                     reads=[W.b, XT[kc // 8].b], writes=[ps.b])
            return ps

        for sb in range(NSB):
            r = (sb * 512) // TR
            t0 = (sb * 512) % TR
            for j in range(4):
                S.dma(xq, XT[j][:], xv[r, :, 8 * j:8 * j + 8, t0:t0 + 512], writes=[XT[j].b])
            rot = 0
            for kind, base in (("k", 384), ("q", 0), ("v", 768)):
                for h in range(HPC):
                    ct = {"q": 0, "k": 3, "v": 6}[kind] + h
                    ps = proj_tile(base + 128 * h, 128)
                    rw, cvb, ab_, sq_, rn_ = raw[rot], cv[rot], actb[rot], sqb[rot], rnb[rot]
                    rot ^= 1
                    S.op("act", lambda e: e.copy(rw[:, 3:515], ps[:, :]), reads=[ps.b], writes=[rw.b])
                    S.op("dve", lambda e: e.tensor_copy(rw[:, 0:3], halo[:, ct, :]), reads=[halo.b], writes=[rw.b])
                    S.op("dve", lambda e: e.tensor_scalar(out=cvb[:], in0=rw[:, 0:512], scalar1=CW[:, 4 * ct:4 * ct + 1],
                                                          scalar2=None, op0=ALU.mult), reads=[rw.b, CW.b], writes=[cvb.b])
                    for j in range(1, 4):
                        S.op("dve", lambda e, j=j: e.scalar_tensor_tensor(out=cvb[:], in0=rw[:, j:j + 512],
                                                                          scalar=CW[:, 4 * ct + j:4 * ct + j + 1], in1=cvb[:],
                                                                          op0=ALU.mult, op1=ALU.add),
                             reads=[rw.b, CW.b, cvb.b], writes=[cvb.b])
                    S.op("dve", lambda e: e.tensor_copy(halo[:, ct, :], rw[:, 512:515]), reads=[rw.b], writes=[halo.b])
                    if kind == "v":
                        S.op("act", lambda e: e.activation(out=vT[h][:], in_=cvb[:], func=AF.Silu), reads=[cvb.b], writes=[vT[h].b])
                        continue
                    S.op("act", lambda e: e.activation(out=ab_[:], in_=cvb[:], func=AF.Silu), reads=[cvb.b], writes=[ab_.b])
                    S.op("act", lambda e: e.activation(out=sq_[:], in_=ab_[:], func=AF.Square), reads=[ab_.b], writes=[sq_.b])
                    ps2 = proj_banks.get()
                    S.op("pe", lambda e: e.matmul(ps2[:, :], onesb, sq_[:], start=True, stop=True), reads=[CSb.b, sq_.b], writes=[ps2.b])
                    S.op("act", lambda e: e.activation(out=rn_[:], in_=ps2[:, :], func=AF.Sqrt, bias=L2_EPS), reads=[ps2.b], writes=[rn_.b])
                    S.op("dve", lambda e: e.reciprocal(rn_[:], rn_[:]), reads=[rn_.b], writes=[rn_.b])
                    dst = qn[h] if kind == "q" else kn[h]
                    sc = 128.0 ** -0.5 if kind == "q" else 1.0
                    S.op("dve", lambda e: e.scalar_tensor_tensor(out=dst[:], in0=ab_[:], scalar=sc, in1=rn_[:], op0=ALU.mult, op1=ALU.mult),
                         reads=[ab_.b, rn_.b], writes=[dst.b])
            for h in range(HPC):
                ps = proj_tile(1152 + 128 * h, 128)
                S.op("act", lambda e: e.activation(out=zs[h][:], in_=ps[:, :], func=AF.Silu), reads=[ps.b], writes=[zs[h].b])
            ps = proj_tile(1536, 6)
            S.op("act", lambda e: e.copy(abT[0:6, :], ps[0:6, :]), reads=[ps.b], writes=[abT.b])

            for c in range(8):
                cs = slice(64 * c, 64 * c + 64)
                pa = wk_banks.get()
                S.op("pe", lambda e: e.transpose(pa[0:64, 0:6], abT[0:6, cs], ident[0:6, 0:6]), reads=[abT.b, CS.b], writes=[pa.b])
                S.op("dve", lambda e: e.tensor_tensor(out=G["x1"][:], in0=pa[0:64, 0:3], in1=HP[0:64, 3:6], op=ALU.add),
                     reads=[pa.b, HP.b], writes=[G["x1"].b])
                S.op("act", lambda e: e.activation(out=G["beta"][:], in_=pa[0:64, 3:6], func=AF.Sigmoid), reads=[pa.b], writes=[G["beta"].b])
                S.op("act", lambda e: e.activation(out=G["ex"][:], in_=G["x1"][:], func=AF.Exp), reads=[G["x1"].b], writes=[G["ex"].b])
                S.op("act", lambda e: e.activation(out=G["sp"][:], in_=G["ex"][:], func=AF.Ln, bias=1.0), reads=[G["ex"].b], writes=[G["sp"].b])
                S.op("dve", lambda e: e.scalar_tensor_tensor(out=G["g"][:], in0=G["sp"][:], scalar=-1.0, in1=EAL[0:64, :], op0=ALU.mult, op1=ALU.mult),
                     reads=[G["sp"].b, EAL.b], writes=[G["g"].b])
                pd = wk_banks.get()
                S.op("pe", lambda e: e.matmul(pd[0:64, 0:3], CS[0:64, 1, 0:64], G["g"][:], start=True, stop=True), reads=[CS.b, G["g"].b], writes=[pd.b])
                S.op("pe", lambda e: e.matmul(pd[0:64, 8:11], CS[0:64, 3, 0:64], G["g"][:], start=True, stop=True), reads=[CS.b, G["g"].b], writes=[pd.b])
                S.op("pe", lambda e: e.matmul(pd[:, 16:19], CS[0:64, 4, :], G["g"][:], start=True, stop=True), reads=[CS.b, G["g"].b], writes=[pd.b])
                S.op("act", lambda e: e.activation(out=G["edec"][:], in_=pd[0:64, 0:3], func=AF.Exp), reads=[pd.b], writes=[G["edec"].b])
                S.op("act", lambda e: e.activation(out=G["edlm"][:], in_=pd[0:64, 8:11], func=AF.Exp), reads=[pd.b], writes=[G["edlm"].b])
                S.op("act", lambda e: e.activation(out=G["edl"][:], in_=pd[:, 16:19], func=AF.Exp), reads=[pd.b], writes=[G["edl"].b])
                S.op("dve", lambda e: e.tensor_tensor(out=G["bedec"][:], in0=G["beta"][:], in1=G["edec"][:], op=ALU.mult),
                     reads=[G["beta"].b, G["edec"].b], writes=[G["bedec"].b])

                U_ = CS[0:64, 1, 0:64]
                nU_ = CS[0:64, 2, 0:64]
                for h in range(HPC):
                    w = HW[h]
                    S.op("dve", lambda e, w=w, h=h: e.tensor_scalar(out=w["Gb"][:], in0=CS[0:64, 4, 0:64], scalar1=G["g"][:, h:h + 1], scalar2=None, op0=ALU.mult),
                         reads=[CS.b, G["g"].b], writes=[w["Gb"].b])
                    p1 = wk_banks.get()
                    S.op("pe", lambda e, w=w: e.matmul(p1[0:64, 0:64], U_, w["Gb"][:], start=True, stop=False), reads=[CS.b, w["Gb"].b], writes=[p1.b])
                    S.op("pe", lambda e, w=w: e.matmul(p1[0:64, 0:64], w["Gb"][:], nU_, start=False, stop=True), reads=[CS.b, w["Gb"].b], writes=[p1.b])
                    S.op("pe", lambda e, w=w: e.matmul(p1[0:64, 64:128], w["Gb"][:], U_, start=True, stop=False), reads=[CS.b, w["Gb"].b], writes=[p1.b])
                    S.op("pe", lambda e, w=w: e.matmul(p1[0:64, 64:128], nU_, w["Gb"][:], start=False, stop=True), reads=[CS.b, w["Gb"].b], writes=[p1.b])
                    S.op("dve", lambda e, w=w: e.tensor_tensor(out=w["gsa"][:], in0=p1[0:64, 0:64], in1=CS[0:64, 5, 0:64], op=ALU.add),
                         reads=[p1.b, CS.b], writes=[w["gsa"].b])
                    S.op("dve", lambda e, w=w: e.tensor_tensor(out=w["gTa"][:], in0=p1[0:64, 64:128], in1=CS[0:64, 6, 0:64], op=ALU.add),
                         reads=[p1.b, CS.b], writes=[w["gTa"].b])
                    S.op("act", lambda e, w=w: e.activation(out=w["gs"][:], in_=w["gsa"][:], func=AF.Exp), reads=[w["gsa"].b], writes=[w["gs"].b])
                    S.op("act", lambda e, w=w: e.activation(out=w["gT"][:], in_=w["gTa"][:], func=AF.Exp), reads=[w["gTa"].b], writes=[w["gT"].b])
                    p2 = wk_banks.get()
                    p2b = p2[:].bitcast(BF16)
                    S.op("pe", lambda e, h=h: e.transpose(p2b[0:64, 0:128], kn[h][:, cs], identb), reads=[kn[h].b, CSb.b], writes=[p2.b])
                    S.op("pe", lambda e, h=h: e.transpose(p2b[0:64, 128:256], vT[h][:, cs], identb), reads=[vT[h].b, CSb.b], writes=[p2.b])
                    S.op("pe", lambda e, h=h: e.matmul(p2[0:64, 256:320], kn[h][:, cs], kn[h][:, cs], start=True, stop=True), reads=[kn[h].b], writes=[p2.b])
                    S.op("pe", lambda e, h=h: e.matmul(p2[0:64, 320:384], kn[h][:, cs], qn[h][:, cs], start=True, stop=True), reads=[kn[h].b, qn[h].b], writes=[p2.b])
                    S.op("dve", lambda e, w=w, h=h: e.tensor_scalar(out=w["kbe"][:], in0=p2b[0:64, 0:128], scalar1=G["bedec"][:, h:h + 1], scalar2=None, op0=ALU.mult),
                         reads=[p2.b, G["bedec"].b], writes=[w["kbe"].b])
                    S.op("dve", lambda e, w=w, h=h: e.tensor_scalar(out=w["ktail"][:], in0=p2b[0:64, 0:128], scalar1=G["edlm"][:, h:h + 1], scalar2=None, op0=ALU.mult),
                         reads=[p2.b, G["edlm"].b], writes=[w["ktail"].b])
                    S.op("dve", lambda e, w=w, h=h: e.tensor_scalar(out=w["bv"][:], in0=p2b[0:64, 128:256], scalar1=G["beta"][:, h:h + 1], scalar2=None, op0=ALU.mult),
                         reads=[p2.b, G["beta"].b], writes=[w["bv"].b])
                    S.op("dve", lambda e, w=w, h=h: e.scalar_tensor_tensor(out=w["N"][:], in0=p2[0:64, 256:320], scalar=G["beta"][:, h:h + 1], in1=w["gs"][:],
                                                                         op0=ALU.mult, op1=ALU.mult),
                         reads=[p2.b, G["beta"].b, w["gs"].b], writes=[w["N"].b])
                    S.op("dve", lambda e, w=w: e.tensor_tensor(out=w["qkT"][:], in0=p2[0:64, 320:384], in1=w["gT"][:], op=ALU.mult),
                         reads=[p2.b, w["gT"].b], writes=[w["qkT"].b])
                    S.op("dve", lambda e, w=w, h=h: e.tensor_scalar(out=w["Lm"][:], in0=CS[0:64, 4, :], scalar1=G["edec"][:, h:h + 1], scalar2=None, op0=ALU.mult),
                         reads=[CS.b, G["edec"].b], writes=[w["Lm"].b])
                    p3 = wk_banks.get()
                    S.op("pe", lambda e, w=w: e.matmul(p3[:, 0:64], w["Lm"][:], ident[0:64, 0:64], start=True, stop=True), reads=[w["Lm"].b, CS.b], writes=[p3.b])
                    S.op("dve", lambda e, w=w, h=h: e.tensor_tensor(out=w["qd"][:], in0=qn[h][:, cs], in1=p3[:, 0:64], op=ALU.mult),
                         reads=[qn[h].b, p3.b], writes=[w["qd"].b])
                    S.op("pe", lambda e, w=w: e.transpose(p3[0:64, 64:128], w["N"][:], ident[0:64, 0:64]), reads=[w["N"].b, CS.b], writes=[p3.b])
                    S.op("act", lambda e, w=w: e.copy(w["P"][0][:], p3[0:64, 64:128]), reads=[p3.b], writes=[w["P"][0].b])
                    S.op("dve", lambda e, w=w: e.tensor_tensor(out=w["X"][:], in0=ident[0:64, 0:64], in1=p3[0:64, 64:128], op=ALU.subtract),
                         reads=[p3.b, CS.b], writes=[w["X"].b])
                cur = [0] * HPC
                for lvl in range(1, 6):
                    for h in range(HPC):
                        w = HW[h]
                        Pc = w["P"][cur[h]]
                        Qc = w["N"] if lvl == 1 else w["Q"][cur[h]]
                        Pn = w["P"][1 - cur[h]]
                        Qn = w["Q"][1 - cur[h]]
                        pp = wk_banks.get()
                        if lvl < 5:
                            S.op("pe", lambda e, Pc=Pc, Qc=Qc: e.matmul(pp[0:64, 0:64], Qc[:], Pc[:], start=True, stop=True), reads=[Pc.b, Qc.b], writes=[pp.b])
                        S.op("pe", lambda e, Pc=Pc, Qc=Qc: e.matmul(pp[0:64, 64:128], Pc[:], Qc[:], start=True, stop=True), reads=[Pc.b, Qc.b], writes=[pp.b])
                        if lvl < 5:
                            S.op("act", lambda e, Pn=Pn: e.copy(Pn[:], pp[0:64, 0:64]), reads=[pp.b], writes=[Pn.b])
                        S.op("dve", lambda e, Qn=Qn: e.tensor_copy(Qn[:], pp[0:64, 64:128]), reads=[pp.b], writes=[Qn.b])
                        S.op("pe", lambda e, w=w, Qn=Qn: e.matmul(pp[0:64, 128:192], Qn[:], w["X"][:], start=True, stop=True), reads=[Qn.b, w["X"].b], writes=[pp.b])
                        S.op("dve", lambda e, w=w: e.tensor_tensor(out=w["X"][:], in0=w["X"][:], in1=pp[0:64, 128:192], op=ALU.add),
                             reads=[pp.b, w["X"].b], writes=[w["X"].b])
                        cur[h] = 1 - cur[h]
                for h in range(HPC):
                    w = HW[h]
                    S.op("act", lambda e, w=w: e.copy(w["Xb"][:], w["X"][:]), reads=[w["X"].b], writes=[w["Xb"].b])
                    p4 = wk_banks.get()
                    S.op("pe", lambda e, w=w: e.matmul(p4[:, 0:64], w["kbe"][:], w["Xb"][:], start=True, stop=True), reads=[w["kbe"].b, w["Xb"].b], writes=[p4.b])
                    S.op("act", lambda e, w=w: e.mul(w["nwT"][:], p4[:, 0:64], -1.0), reads=[p4.b], writes=[w["nwT"].b])
                    S.op("pe", lambda e, w=w: e.matmul(p4[0:64, 128:256], w["Xb"][:], w["bv"][:], start=True, stop=False), reads=[w["Xb"].b, w["bv"].b], writes=[p4.b])
                    S.op("pe", lambda e, w=w, h=h: e.matmul(p4[0:64, 128:256], w["nwT"][:], Sb[h][:], start=False, stop=True), reads=[w["nwT"].b, Sb[h].b], writes=[p4.b])
                    S.op("act", lambda e, w=w: e.copy(w["vn"][:], p4[0:64, 128:256]), reads=[p4.b], writes=[w["vn"].b])
                    p5 = wk_banks.get()
                    S.op("pe", lambda e, w=w, h=h: e.matmul(p5[0:64, 0:128], w["qd"][:], Sb[h][:], start=True, stop=False), reads=[w["qd"].b, Sb[h].b], writes=[p5.b])
                    S.op("pe", lambda e, w=w: e.matmul(p5[0:64, 0:128], w["qkT"][:], w["vn"][:], start=False, stop=True), reads=[w["qkT"].b, w["vn"].b], writes=[p5.b])
                    S.op("pe", lambda e, w=w: e.matmul(p5[:, 128:256], w["ktail"][:], w["vn"][:], start=True, stop=True), reads=[w["ktail"].b, w["vn"].b], writes=[p5.b])
                    S.op("dve", lambda e, h=h: e.scalar_tensor_tensor(out=Sst[h][:], in0=Sst[h][:], scalar=G["edl"][:, h:h + 1], in1=p5[:, 128:256],
                                                                      op0=ALU.mult, op1=ALU.add),
                         reads=[Sst[h].b, G["edl"].b, p5.b], writes=[Sst[h].b])
                    S.op("act", lambda e, h=h: e.copy(Sb[h][:], Sst[h][:]), reads=[Sst[h].b], writes=[Sb[h].b])
                    S.op("act", lambda e, w=w: e.activation(out=w["junk"][:], in_=p5[0:64, 0:128], func=AF.Square, accum_out=w["ssq"][:]),
                         reads=[p5.b], writes=[w["junk"].b, w["ssq"].b])
                    S.op("act", lambda e, w=w: e.activation(out=w["rt"][:], in_=w["ssq"][:], func=AF.Sqrt, bias=RMS_EPS, scale=1.0 / 128.0),
                         reads=[w["ssq"].b], writes=[w["rt"].b])
                    S.op("dve", lambda e, w=w: e.reciprocal(w["rstd"][:], w["rt"][:]), reads=[w["rt"].b], writes=[w["rstd"].b])
                    S.op("dve", lambda e, w=w: e.tensor_scalar(out=w["on"][:], in0=p5[0:64, 0:128], scalar1=w["rstd"][:, 0:1], scalar2=None, op0=ALU.mult),
                         reads=[p5.b, w["rstd"].b], writes=[w["on"].b])
                    p6 = wk_banks.get()
                    S.op("pe", lambda e, w=w: e.transpose(p6[:, 0:64], w["on"][:], ident[0:64, 0:64]), reads=[w["on"].b, CS.b], writes=[p6.b])
                    S.op("dve", lambda e, h=h: e.scalar_tensor_tensor(out=yo[h][:, cs], in0=p6[:, 0:64], scalar=HP[:, 8:9], in1=zs[h][:, cs],
                                                                      op0=ALU.mult, op1=ALU.mult),
                         reads=[p6.b, HP.b, zs[h].b], writes=[yo[h].b])
            for h in range(HPC):
                S.dma("sp", yT[128 * h:128 * h + 128, sb * 512:sb * 512 + 512], yo[h][:], reads=[yo[h].b])
        S.barrier()


def x_to_xT(x2d):
    L = x2d.shape[0]
    TR = min(L, 1024)
    return np.ascontiguousarray(x2d.reshape(L // TR, TR, D_MODEL).transpose(0, 2, 1))


def prep_gdn(c, layer, inp):
    hs = [HPC * c + i for i in range(HPC)]
    w_in = inp["w_in"][layer]
    cols = []
    for blk in range(4):
        for h in hs:
            cols.append(np.arange(blk * GDN_WIDTH + h * 128, blk * GDN_WIDTH + (h + 1) * 128))
    cols.append(np.array([4 * GDN_WIDTH + h for h in hs]))
    cols.append(np.array([4 * GDN_WIDTH + GDN_HEADS + h for h in hs]))
    cols = np.concatenate(cols)
    wg = np.ascontiguousarray(w_in[:, cols])
    cw = inp["gdn_conv_w"][layer]
    convw = np.zeros((128, 36), np.float32)
    for blk in range(3):
        for i, h in enumerate(hs):
            ct = blk * 3 + i
            ch = blk * GDN_WIDTH + h * 128 + np.arange(128)
            convw[:, 4 * ct:4 * ct + 4] = cw[:, ch].T
    hp = np.zeros((128, 16), np.float32)
    hp[:, 0:3] = inp["gdn_a_log"][layer][hs][None, :]
    hp[:, 3:6] = inp["gdn_dt_bias"][layer][hs][None, :]
    hp[:, 8] = inp["gdn_norm_w"][layer]
    return {"wg": wg, "convw": convw, "hp": hp, "cst": gdn_consts()}


S5C = 256


def s5_consts():
    c = np.zeros((128, 5, 256), np.float32)
    c[:, 0, :128] = np.eye(128)
    k = np.arange(128)
    sw = np.zeros((128, 128), np.float32)
    sw[k, (k + 64) % 128] = 1.0
    c[:, 1, :128] = sw
    c[:, 2, :] = np.arange(256)[None, :]
    g = np.arange(128) // 16
    c[:, 3, :8] = (g[:, None] == np.arange(8)[None, :])
    c[:64, 3, 8] = 1.0
    c[64:, 3, 8] = -1.0
    c[:, 3, 9] = -1.0
    return c.reshape(128, 5 * 256)


def prep_s5(c, layer, inp):
    gs = np.arange(GPC * c, GPC * c + GPC)
    w_in = inp["w_in"][layer]
    c_u = 4 * GDN_WIDTH + 2 * GDN_HEADS
    wu = np.ascontiguousarray(w_in[:, c_u + 128 * c:c_u + 128 * c + 128])
    lre = inp["s5_lambda_re"][layer][gs]
    lim = inp["s5_lambda_im"][layer][gs]
    ldt = inp["s5_log_dt"][layer][gs]
    bre = inp["s5_b_re"][layer][gs]
    bim = inp["s5_b_im"][layer][gs]
    cre = inp["s5_c_re"][layer][gs]
    cim = inp["s5_c_im"][layer][gs]
    pr = np.zeros((128, 8, 64), np.float32)
    pr[:, 0, :] = np.repeat(lre, 16, axis=0)
    pr[:, 1, :] = np.repeat(lim, 16, axis=0)
    pr[:, 2, :] = bre.transpose(0, 2, 1).reshape(128, 64)
    pr[:, 3, :] = bim.transpose(0, 2, 1).reshape(128, 64)
    pr[:, 4, 0] = np.repeat(ldt, 16)
    pr[:, 4, 1] = inp["s5_d"][layer][128 * c:128 * c + 128]
    pc = np.zeros((128, 4, 128), np.float32)
    cTre = cre.transpose(2, 0, 1).reshape(64, 128)
    cTim = cim.transpose(2, 0, 1).reshape(64, 128)
    pc[:64, 0, :] = cTre
    pc[64:, 0, :] = cTim
    pc[:64, 1, :] = cTim
    pc[64:, 1, :] = cTre
    pc[:, 2, 0:8] = np.tile(lre.T, (2, 1))
    pc[:, 2, 8:16] = np.tile(lim.T, (2, 1))
    pc[:, 2, 16:24] = ldt[None, :]
    return {"wu": wu, "pr": pr.reshape(128, 512), "pc": pc.reshape(128, 512), "cst": s5_consts()}


def emit_s5(nc, S, L, x_bf16, xT, wu, pr_d, pc_d, cst, yT):
    NR = max(1, L // 1024)
    TR = min(L, 1024)
    NSB = L // 512
    xq = "sp" if x_bf16 else "pool"

    with ExitStack() as es:
        C = Ctx(nc, es, S)
        W = C.sb([128, KC, 128], BF16, "W")
        XT = [C.sb([128, 8, 512], BF16, "XT") for _ in range(8)]
        PR = C.sb([128, 8, 64], F32, "PR")
        PC = C.sb([128, 4, 128], F32, "PC")
        CS = C.sb([128, 5, 256], F32, "CS")
        PS = C.psum_banks(8)
        ident = CS[:, 0, 0:128]
        swap = CS[:, 1, 0:128]
        iota = CS[:, 2, :]

        wv = wu.rearrange("(kc p) n -> p kc n", p=128)
        S.dma("pool", W[:], wv, writes=[W.b])
        S.dma("sp", PR[:].rearrange("p a b -> p (a b)"), pr_d, writes=[PR.b])
        S.dma("sp", PC[:].rearrange("p a b -> p (a b)"), pc_d, writes=[PC.b])
        S.dma("sp", CS[:].rearrange("p a b -> p (a b)"), cst, writes=[CS.b])

        n_tmp = [0]

        def tmp(shape, dt=F32):
            n_tmp[0] += 1
            return C.sb(shape, dt, "tmp")

        def dve(fn, reads, writes):
            return S.op("dve", fn, reads=[t.b for t in reads], writes=[t.b for t in writes])

        def act(fn, reads, writes):
            return S.op("act", fn, reads=[t.b for t in reads], writes=[t.b for t in writes])

        sin_tmp = {}

        def sin_of(dst, ang, shift):
            key = tuple(dst.shape_)
            if key not in sin_tmp:
                sin_tmp[key] = (tmp(list(key)), tmp(list(key)))
            k, r = sin_tmp[key]
            dve(lambda e: e.tensor_scalar(out=k[:], in0=ang[:], scalar1=shift, scalar2=1.0 / TWO_PI, op0=ALU.add, op1=ALU.mult), [ang], [k])
            dve(lambda e: e.tensor_scalar(out=k[:], in0=k[:], scalar1=MAGIC, scalar2=MAGIC, op0=ALU.add, op1=ALU.subtract), [k], [k])
            dve(lambda e: e.scalar_tensor_tensor(out=r[:], in0=k[:], scalar=-TWO_PI, in1=ang[:], op0=ALU.mult, op1=ALU.add), [k, ang], [r])
            dve(lambda e: e.tensor_scalar(out=r[:], in0=r[:], scalar1=shift, scalar2=3.1415925, op0=ALU.add, op1=ALU.min), [r], [r])
            dve(lambda e: e.tensor_scalar(out=r[:], in0=r[:], scalar1=-3.1415925, scalar2=None, op0=ALU.max), [r], [r])
            act(lambda e: e.activation(out=dst[:], in_=r[:], func=AF.Sin), [r], [dst])

        def dst_shape(t):
            return t.shape_

        def mk(shape, dt=F32):
            t = tmp(shape, dt)
            t.shape_ = shape
            return t

        dtc = mk([128, 1])
        act(lambda e: e.activation(out=dtc[:], in_=PR[:, 4, 0:1], func=AF.Exp), [PR], [dtc])
        lrd = mk([128, 64]); lid = mk([128, 64]); mag = mk([128, 64]); sn = mk([128, 64]); cs_ = mk([128, 64])
        dve(lambda e: e.tensor_scalar(out=lrd[:], in0=PR[:, 0, :], scalar1=dtc[:, 0:1], scalar2=None, op0=ALU.mult), [PR, dtc], [lrd])
        dve(lambda e: e.tensor_scalar(out=lid[:], in0=PR[:, 1, :], scalar1=dtc[:, 0:1], scalar2=None, op0=ALU.mult), [PR, dtc], [lid])
        act(lambda e: e.activation(out=mag[:], in_=lrd[:], func=AF.Exp), [lrd], [mag])
        sin_of(sn, lid, 0.0)
        sin_of(cs_, lid, float(np.pi / 2))
        nr = mk([128, 64]); ni = mk([128, 64]); den = mk([128, 64]); t1 = mk([128, 64]); t2 = mk([128, 64])
        cre = mk([128, 64]); cim = mk([128, 64])
        dve(lambda e: e.tensor_tensor(out=nr[:], in0=mag[:], in1=cs_[:], op=ALU.mult), [mag, cs_], [nr])
        dve(lambda e: e.tensor_scalar(out=nr[:], in0=nr[:], scalar1=-1.0, scalar2=None, op0=ALU.add), [nr], [nr])
        dve(lambda e: e.tensor_tensor(out=ni[:], in0=mag[:], in1=sn[:], op=ALU.mult), [mag, sn], [ni])
        dve(lambda e: e.tensor_tensor(out=den[:], in0=PR[:, 0, :], in1=PR[:, 0, :], op=ALU.mult), [PR], [den])
        dve(lambda e: e.tensor_tensor(out=t1[:], in0=PR[:, 1, :], in1=PR[:, 1, :], op=ALU.mult), [PR], [t1])
        dve(lambda e: e.tensor_tensor(out=den[:], in0=den[:], in1=t1[:], op=ALU.add), [den, t1], [den])
        dve(lambda e: e.reciprocal(den[:], den[:]), [den], [den])
        dve(lambda e: e.tensor_tensor(out=t1[:], in0=nr[:], in1=PR[:, 0, :], op=ALU.mult), [nr, PR], [t1])
        dve(lambda e: e.tensor_tensor(out=t2[:], in0=ni[:], in1=PR[:, 1, :], op=ALU.mult), [ni, PR], [t2])
        dve(lambda e: e.tensor_tensor(out=t1[:], in0=t1[:], in1=t2[:], op=ALU.add), [t1, t2], [t1])
        dve(lambda e: e.tensor_tensor(out=cre[:], in0=t1[:], in1=den[:], op=ALU.mult), [t1, den], [cre])
        dve(lambda e: e.tensor_tensor(out=t1[:], in0=ni[:], in1=PR[:, 0, :], op=ALU.mult), [ni, PR], [t1])
        dve(lambda e: e.tensor_tensor(out=t2[:], in0=nr[:], in1=PR[:, 1, :], op=ALU.mult), [nr, PR], [t2])
        dve(lambda e: e.tensor_tensor(out=t1[:], in0=t1[:], in1=t2[:], op=ALU.subtract), [t1, t2], [t1])
        dve(lambda e: e.tensor_tensor(out=cim[:], in0=t1[:], in1=den[:], op=ALU.mult), [t1, den], [cim])
        BB1 = mk([128, 128]); BB2 = mk([128, 128])
        dve(lambda e: e.tensor_tensor(out=t1[:], in0=cre[:], in1=PR[:, 2, :], op=ALU.mult), [cre, PR], [t1])
        dve(lambda e: e.tensor_tensor(out=t2[:], in0=cim[:], in1=PR[:, 3, :], op=ALU.mult), [cim, PR], [t2])
        dve(lambda e: e.tensor_tensor(out=BB1[:, 0:64], in0=t1[:], in1=t2[:], op=ALU.subtract), [t1, t2], [BB1])
        dve(lambda e: e.tensor_tensor(out=t1[:], in0=cre[:], in1=PR[:, 3, :], op=ALU.mult), [cre, PR], [t1])
        dve(lambda e: e.tensor_tensor(out=t2[:], in0=cim[:], in1=PR[:, 2, :], op=ALU.mult), [cim, PR], [t2])
        dve(lambda e: e.tensor_tensor(out=BB1[:, 64:128], in0=t1[:], in1=t2[:], op=ALU.add), [t1, t2], [BB1])
        dve(lambda e: e.tensor_copy(BB2[:, 0:64], BB1[:, 64:128]), [BB1], [BB2])
        dve(lambda e: e.tensor_scalar(out=BB2[:, 64:128], in0=BB1[:, 0:64], scalar1=-1.0, scalar2=None, op0=ALU.mult), [BB1], [BB2])
        Bm1 = mk([128, GPC, 128], BF16); Bm2 = mk([128, GPC, 128], BF16)
        for g in range(GPC):
            dve(lambda e, g=g: e.tensor_scalar(out=Bm1[:, g, :], in0=BB1[:], scalar1=CS[:, 3, g:g + 1], scalar2=None, op0=ALU.mult), [BB1, CS], [Bm1])
            dve(lambda e, g=g: e.tensor_scalar(out=Bm2[:, g, :], in0=BB2[:], scalar1=CS[:, 3, g:g + 1], scalar2=None, op0=ALU.mult), [BB2, CS], [Bm2])
        W1 = mk([128, GPC, 128], BF16); W2 = mk([128, GPC, 128], BF16)
        dve(lambda e: e.memset(W1[:].rearrange("p a b -> p (a b)"), 0.0), [], [W1])
        dve(lambda e: e.memset(W2[:].rearrange("p a b -> p (a b)"), 0.0), [], [W2])
        for g in range(GPC):
            sl = slice(16 * g, 16 * g + 16)
            dve(lambda e, g=g, sl=sl: e.tensor_scalar(out=W1[:, g, sl], in0=PC[:, 0, sl], scalar1=CS[:, 3, 8:9], scalar2=None, op0=ALU.mult), [PC, CS], [W1])
            dve(lambda e, g=g, sl=sl: e.tensor_scalar(out=W2[:, g, sl], in0=PC[:, 1, sl], scalar1=CS[:, 3, 9:10], scalar2=None, op0=ALU.mult), [PC, CS], [W2])
        dt2 = mk([128, 8]); th = mk([128, 8]); rho = mk([128, 8]); lr2 = mk([128, 8])
        act(lambda e: e.activation(out=dt2[:], in_=PC[:, 2, 16:24], func=AF.Exp), [PC], [dt2])
        dve(lambda e: e.tensor_tensor(out=th[:], in0=PC[:, 2, 8:16], in1=dt2[:], op=ALU.mult), [PC, dt2], [th])
        dve(lambda e: e.tensor_tensor(out=lr2[:], in0=PC[:, 2, 0:8], in1=dt2[:], op=ALU.mult), [PC, dt2], [lr2])
        act(lambda e: e.activation(out=rho[:], in_=lr2[:], func=AF.Exp), [lr2], [rho])
        C2 = mk([128, GPC, S5C]); S2 = mk([128, GPC, S5C])
        ang = mk([128, S5C]); sg = mk([128, S5C]); cg = mk([128, S5C])
        for g in range(GPC):
            dve(lambda e, g=g: e.tensor_scalar(out=ang[:], in0=iota, scalar1=th[:, g:g + 1], scalar2=None, op0=ALU.mult), [CS, th], [ang])
            sin_of(sg, ang, 0.0)
            sin_of(cg, ang, float(np.pi / 2))
            dve(lambda e, g=g, sg=sg: e.tensor_copy(S2[:, g, :], sg[:]), [sg], [S2])
            dve(lambda e, g=g, cg=cg: e.tensor_copy(C2[:, g, :], cg[:]), [cg], [C2])
        angc = mk([128, 8]); crr = mk([128, 8]); srr = mk([128, 8])
        dve(lambda e: e.tensor_scalar(out=angc[:], in0=th[:], scalar1=float(S5C), scalar2=None, op0=ALU.mult), [th], [angc])
        sin_of(srr, angc, 0.0)
        sin_of(crr, angc, float(np.pi / 2))
        dve(lambda e: e.tensor_scalar(out=srr[:], in0=srr[:], scalar1=CS[:, 3, 8:9], scalar2=None, op0=ALU.mult), [srr, CS], [srr])
        ROT = mk([128, GPC, 128])
        for g in range(GPC):
            dve(lambda e, g=g: e.tensor_scalar(out=ROT[:, g, :], in0=ident, scalar1=crr[:, g:g + 1], scalar2=None, op0=ALU.mult), [CS, crr], [ROT])
            dve(lambda e, g=g: e.scalar_tensor_tensor(out=ROT[:, g, :], in0=swap, scalar=srr[:, g:g + 1], in1=ROT[:, g, :], op0=ALU.mult, op1=ALU.add),
                [CS, srr, ROT], [ROT])

        uT = [mk([128, 512]) for _ in range(2)]
        uTb = [mk([128, 512], BF16) for _ in range(2)]
        mbuf = [mk([128, S5C]) for _ in range(2)]
        tbuf = [mk([128, S5C]) for _ in range(2)]
        zeta = [mk([128, S5C]) for _ in range(2)]
        Zc = [mk([128, S5C], BF16) for _ in range(2)]
        Zs = [mk([128, S5C], BF16) for _ in range(2)]
        zl = [mk([128, 1]) for _ in range(GPC)]
        zi = [mk([128, 1]) for _ in range(GPC)]
        yf = [mk([128, S5C]) for _ in range(2)]
        yo = [mk([128, S5C], BF16) for _ in range(2)]
        gl = [(mk([128, S5C]), mk([128, S5C])) for _ in range(2)]
        proj_banks = BankPool(PS[0:2])
        p_banks = BankPool(PS[2:6])
        y_banks = BankPool(PS[6:8])
        xv = xT.rearrange("r (kc p) t -> r p kc t", p=128)
        rot = 0
        nchunk = 0
        for sb in range(NSB):
            r = (sb * 512) // TR
            t0 = (sb * 512) % TR
            xs = XT[4 * (sb % 2):4 * (sb % 2) + 4]
            for j in range(4):
                S.dma(xq, xs[j][:], xv[r, :, 8 * j:8 * j + 8, t0:t0 + 512], writes=[xs[j].b])
            ps = proj_banks.get()
            for kc in range(KC):
                S.op("pe", lambda e, kc=kc: e.matmul(ps[:, :], W[:, kc, :], xs[kc // 8][:, kc % 8, :], start=(kc == 0), stop=(kc == KC - 1)),
                     reads=[W.b, xs[kc // 8].b], writes=[ps.b])
            u_, ub_ = uT[sb % 2], uTb[sb % 2]
            act(lambda e: e.copy(u_[:], ps[:, :]), [ps], [u_])
            dve(lambda e: e.tensor_copy(ub_[:], ps[:, :]), [ps], [ub_])
            for cc in range(512 // S5C):
                csl = slice(cc * S5C, (cc + 1) * S5C)
                yps = y_banks.get()
                for g in range(GPC):
                    pb = p_banks.get()
                    m_, t_, z_, zc_, zs_ = mbuf[rot], tbuf[rot], zeta[rot], Zc[rot], Zs[rot]
                    rot ^= 1
                    S.op("pe", lambda e, g=g: e.matmul(pb[:, 0:S5C], Bm1[:, g, :], ub_[:, csl], start=True, stop=True), reads=[Bm1.b, ub_.b], writes=[pb.b])
                    S.op("pe", lambda e, g=g: e.matmul(pb[:, S5C:2 * S5C], Bm2[:, g, :], ub_[:, csl], start=True, stop=True), reads=[Bm2.b, ub_.b], writes=[pb.b])
                    dve(lambda e, g=g: e.tensor_tensor(out=m_[:], in0=pb[:, 0:S5C], in1=C2[:, g, :], op=ALU.mult), [pb, C2], [m_])
                    dve(lambda e, g=g: e.tensor_tensor(out=t_[:], in0=pb[:, S5C:2 * S5C], in1=S2[:, g, :], op=ALU.mult), [pb, S2], [t_])
                    dve(lambda e: e.tensor_tensor(out=m_[:], in0=m_[:], in1=t_[:], op=ALU.add), [m_, t_], [m_])
                    if nchunk == 0:
                        dve(lambda e, g=g: e.tensor_tensor_scan(out=z_[:], data0=rho[:, g:g + 1].to_broadcast([128, S5C]), data1=m_[:], initial=0.0,
                                                              op0=ALU.mult, op1=ALU.add), [rho, m_], [z_])
                    else:
                        pr_ = p_banks.get()
                        S.op("pe", lambda e, g=g: e.matmul(pr_[:, 0:1], ROT[:, g, :], zl[g][:], start=True, stop=True), reads=[ROT.b, zl[g].b], writes=[pr_.b])
                        act(lambda e, g=g: e.copy(zi[g][:], pr_[:, 0:1]), [pr_], [zi[g]])
                        dve(lambda e, g=g: e.tensor_tensor_scan(out=z_[:], data0=rho[:, g:g + 1].to_broadcast([128, S5C]), data1=m_[:], initial=zi[g][:, 0:1],
                                                              op0=ALU.mult, op1=ALU.add), [rho, m_, zi[g]], [z_])
                    dve(lambda e, g=g: e.tensor_copy(zl[g][:], z_[:, S5C - 1:S5C]), [z_], [zl[g]])
                    dve(lambda e, g=g: e.tensor_tensor(out=zc_[:], in0=z_[:], in1=C2[:, g, :], op=ALU.mult), [z_, C2], [zc_])
                    dve(lambda e, g=g: e.tensor_tensor(out=zs_[:], in0=z_[:], in1=S2[:, g, :], op=ALU.mult), [z_, S2], [zs_])
                    S.op("pe", lambda e, g=g: e.matmul(yps[:, 0:S5C], W1[:, g, :], zc_[:], start=(g == 0), stop=False), reads=[W1.b, zc_.b], writes=[yps.b])
                    S.op("pe", lambda e, g=g: e.matmul(yps[:, 0:S5C], W2[:, g, :], zs_[:], start=False, stop=(g == GPC - 1)), reads=[W2.b, zs_.b], writes=[yps.b])
                yf_, yo_ = yf[nchunk % 2], yo[nchunk % 2]
                dve(lambda e: e.scalar_tensor_tensor(out=yf_[:], in0=u_[:, csl], scalar=PR[:, 4, 1:2], in1=yps[:, 0:S5C], op0=ALU.mult, op1=ALU.add),
                    [u_, PR, yps], [yf_])
                g1, g2 = gl[nchunk % 2]
                dve(lambda e: e.tensor_tensor(out=g1[:], in0=yf_[:], in1=yf_[:], op=ALU.mult), [yf_], [g1])
                dve(lambda e: e.tensor_scalar(out=g1[:], in0=g1[:], scalar1=0.044715, scalar2=1.0, op0=ALU.mult, op1=ALU.add), [g1], [g1])
                dve(lambda e: e.tensor_tensor(out=g1[:], in0=g1[:], in1=yf_[:], op=ALU.mult), [g1, yf_], [g1])
                act(lambda e: e.activation(out=g2[:], in_=g1[:], func=AF.Sigmoid, scale=float(2.0 * np.sqrt(2.0 / np.pi))), [g1], [g2])
                dve(lambda e: e.tensor_tensor(out=yo_[:], in0=yf_[:], in1=g2[:], op=ALU.mult), [yf_, g2], [yo_])
                S.dma("sp", yT[:, sb * 512 + cc * S5C: sb * 512 + (cc + 1) * S5C], yo_[:], reads=[yo_.b])
                nchunk += 1
        S.barrier()


def build_mixer(L, x_bf16, do_gdn=True, do_s5=True):
    NR = max(1, L // 1024)
    TR = min(L, 1024)
    nc = bass.Bass("TRN2", target_bir_lowering=False)
    xT = nc.dram_tensor("xT", [NR, D_MODEL, TR], BF16 if x_bf16 else F32, kind="ExternalInput").ap()
    wg = nc.dram_tensor("wg", [D_MODEL, GDN_COLS], F32, kind="ExternalInput").ap()
    convw = nc.dram_tensor("convw", [128, 36], F32, kind="ExternalInput").ap()
    hp = nc.dram_tensor("hp", [128, 16], F32, kind="ExternalInput").ap()
    cstg = nc.dram_tensor("cstg", [128, 8 * 128], F32, kind="ExternalInput").ap()
    wu = nc.dram_tensor("wu", [D_MODEL, 128], F32, kind="ExternalInput").ap()
    pr_d = nc.dram_tensor("pr", [128, 512], F32, kind="ExternalInput").ap()
    pc_d = nc.dram_tensor("pc", [128, 512], F32, kind="ExternalInput").ap()
    csts = nc.dram_tensor("csts", [128, 5 * 256], F32, kind="ExternalInput").ap()
    yT = nc.dram_tensor("yT", [512, L], BF16, kind="ExternalOutput").ap()
    with ExitStack() as outer:
        S = Sched(nc, outer)
        if do_gdn:
            emit_gdn(nc, S, L, x_bf16, xT, wg, convw, hp, cstg, yT[0:384, :])
        if do_s5:
            emit_s5(nc, S, L, x_bf16, xT, wu, pr_d, pc_d, csts, yT[384:512, :])
        S.barrier()
    return nc


def prep_mixer(c, layer, inp):
    g = prep_gdn(c, layer, inp)
    s_ = prep_s5(c, layer, inp)
    return {"wg": g["wg"], "convw": g["convw"], "hp": g["hp"], "cstg": g["cst"],
            "wu": s_["wu"], "pr": s_["pr"], "pc": s_["pc"], "csts": s_["cst"]}


TPC = 1024
NT = TPC // 128
BIG = 1.0e30


def b_consts():
    c = np.zeros((128, 4, 128), np.float32)
    c[:, 0, :] = np.eye(128)
    c[:, 1, :] = 1.0
    k = np.arange(128)
    c[:, 2, :] = (k[:, None] < k[None, :])
    c[:, 3, :] = k[None, :]
    return c.reshape(128, 512)


def emit_consts(S, C, cst):
    CS = C.sb([128, 4, 128], F32, "CS")
    CSb = C.sb([128, 3, 128], BF16, "CSb")
    S.dma("sp", CS[:].rearrange("p a b -> p (a b)"), cst, writes=[CS.b])
    for j in range(3):
        S.op("dve", lambda e, j=j: e.tensor_copy(CSb[:, j, :], CS[:, j, :]), reads=[CS.b], writes=[CSb.b])
    return CS, CSb


def emit_proj_res(S, C, PS, lhs_fn, nk, rhs_view, rhs_f32, resid, resid_bufs, H, Hbufs):
    Wn = [C.sb([128, nk, 512], BF16, "Wn") for _ in range(2)]
    xt = [C.sb([128, 512], F32, "xt") for _ in range(3)]
    ht = [C.sb([128, 512], F32, "ht") for _ in range(3)]
    banks = BankPool(PS[0:4])
    q = "pool" if rhs_f32 else "sp"
    cnt = 0
    for n in range(8):
        w = Wn[n % 2]
        for j in range(4):
            ks = slice(j * nk // 4, (j + 1) * nk // 4)
            S.dma(q, w[:, ks, :], rhs_view[:, ks, n * 512:(n + 1) * 512], writes=[w.b])
        for i in range(NT):
            ps = banks.get()
            for k in range(nk):
                ap, b = lhs_fn(k, i)
                S.op("pe", lambda e, ap=ap, k=k: e.matmul(ps[:, :], ap, w[:, k, :], start=(k == 0), stop=(k == nk - 1)),
                     reads=[b, w.b], writes=[ps.b])
            x_, h_ = xt[cnt % 3], ht[cnt % 3]
            cnt += 1
            S.dma("sp", x_[:], resid[i * 128:(i + 1) * 128, n * 512:(n + 1) * 512], reads=[resid_bufs[i]], writes=[x_.b])
            S.op("dve", lambda e: e.scalar_tensor_tensor(out=h_[:], in0=x_[:], scalar=float(DN_ALPHA), in1=ps[:, :], op0=ALU.mult, op1=ALU.add),
                 reads=[x_.b, ps.b], writes=[h_.b])
            S.dma("sp", H[i * 128:(i + 1) * 128, n * 512:(n + 1) * 512], h_[:], reads=[h_.b], writes=[Hbufs[i]])


def emit_ln_tiles(S, C, H, Hbufs, lng, lnb, out_cb, eps=LN_EPS):
    G = C.sb([128, D_MODEL], F32, "lnG")
    Bt = C.sb([128, D_MODEL], F32, "lnB")
    S.dma("sp", G[:], lng, writes=[G.b])
    S.dma("sp", Bt[:], lnb, writes=[Bt.b])
    tiles = [C.sb([128, D_MODEL], F32, "lt") for _ in range(2)]
    junk = C.sb([128, D_MODEL], BF16, "junk")
    s1 = C.sb([128, 1], F32); nm = C.sb([128, 1], F32); s2 = C.sb([128, 1], F32); rt = C.sb([128, 1], F32); rstd = C.sb([128, 1], F32)
    for i in range(NT):
        t = tiles[i % 2]
        S.dma("sp", t[:], H[i * 128:(i + 1) * 128, :], reads=[Hbufs[i]], writes=[t.b])
        S.op("dve", lambda e: e.reduce_sum(out=s1[:], in_=t[:], axis=AX.X), reads=[t.b], writes=[s1.b])
        S.op("dve", lambda e: e.tensor_scalar(out=nm[:], in0=s1[:], scalar1=-1.0 / D_MODEL, scalar2=None, op0=ALU.mult), reads=[s1.b], writes=[nm.b])
        S.op("act", lambda e: e.activation(out=junk[:], in_=t[:], func=AF.Square, bias=nm[:, 0:1], accum_out=s2[:]), reads=[t.b, nm.b], writes=[junk.b, s2.b])
        S.op("act", lambda e: e.activation(out=rt[:], in_=s2[:], func=AF.Sqrt, bias=eps, scale=1.0 / D_MODEL), reads=[s2.b], writes=[rt.b])
        S.op("dve", lambda e: e.reciprocal(rstd[:], rt[:]), reads=[rt.b], writes=[rstd.b])
        S.op("dve", lambda e: e.tensor_scalar(out=t[:], in0=t[:], scalar1=nm[:, 0:1], scalar2=rstd[:, 0:1], op0=ALU.add, op1=ALU.mult),
             reads=[t.b, nm.b, rstd.b], writes=[t.b])
        S.op("dve", lambda e: e.tensor_tensor(out=t[:], in0=t[:], in1=G[:], op=ALU.mult), reads=[t.b, G.b], writes=[t.b])
        S.op("dve", lambda e: e.tensor_tensor(out=t[:], in0=t[:], in1=Bt[:], op=ALU.add), reads=[t.b, Bt.b], writes=[t.b])
        out_cb(i, t)


def emit_transpose_tile(S, banks, src, identb, CSb, dst_fn, eng_alt=[0]):
    for g in range(4):
        ps = banks.get()
        psb = ps[:].bitcast(BF16)
        for j in range(8):
            kc = g * 8 + j
            S.op("pe", lambda e, j=j, kc=kc: e.transpose(psb[:, j * 128:(j + 1) * 128], src[:, kc * 128:(kc + 1) * 128], identb),
                 reads=[src.b, CSb.b], writes=[ps.b])
        ap, b = dst_fn(g)
        en = "act" if (eng_alt[0] % 2 == 0) else "dve"
        eng_alt[0] += 1
        if en == "act":
            S.op("act", lambda e: e.copy(ap, psb[:, 0:1024].rearrange("p (a b) -> p a b", a=8)), reads=[ps.b], writes=[b])
        else:
            S.op("dve", lambda e: e.tensor_copy(ap, psb[:, 0:1024].rearrange("p (a b) -> p a b", a=8)), reads=[ps.b], writes=[b])


def build_t0():
    nc = bass.Bass("TRN2", target_bir_lowering=False)
    x = nc.dram_tensor("x", [TPC, D_MODEL], F32, kind="ExternalInput").ap()
    cst = nc.dram_tensor("cst", [128, 512], F32, kind="ExternalInput").ap()
    xT = nc.dram_tensor("xTo", [D_MODEL, TPC], BF16, kind="ExternalOutput").ap()
    with ExitStack() as outer:
        S = Sched(nc, outer)
        C = Ctx(nc, outer, S)
        PS = C.psum_banks(8)
        CS, CSb = emit_consts(S, C, cst)
        xTs = C.sb([128, KC, TPC], BF16, "xTs")
        xb = [C.sb([128, D_MODEL], BF16, "xb") for _ in range(2)]
        banks = BankPool(PS)
        for i in range(NT):
            S.dma("pool", xb[i % 2][:], x[i * 128:(i + 1) * 128, :], writes=[xb[i % 2].b])
            emit_transpose_tile(S, banks, xb[i % 2], CSb[:, 0, :], CSb,
                                lambda g, i=i: (xTs[:, g * 8:(g + 1) * 8, i * 128:(i + 1) * 128], xTs.b))
        S.dma("sp", xT.rearrange("(kc p) t -> p kc t", p=128), xTs[:], reads=[xTs.b])
        S.barrier()
    return nc


def build_b1():
    nc = bass.Bass("TRN2", target_bir_lowering=False)
    ymix = nc.dram_tensor("ymix", [D_MODEL, TPC], BF16, kind="ExternalInput").ap()
    wo = nc.dram_tensor("wo", [D_MODEL, D_MODEL], F32, kind="ExternalInput").ap()
    wglu = nc.dram_tensor("wglu", [S5_WIDTH, S5_WIDTH], F32, kind="ExternalInput").ap()
    x = nc.dram_tensor("x", [TPC, D_MODEL], F32, kind="ExternalInput").ap()
    lng = nc.dram_tensor("lng", [128, D_MODEL], F32, kind="ExternalInput").ap()
    lnb = nc.dram_tensor("lnb", [128, D_MODEL], F32, kind="ExternalInput").ap()
    wr = nc.dram_tensor("wr", [D_MODEL, 36], F32, kind="ExternalInput").ap()
    rb = nc.dram_tensor("rb", [128, 36], F32, kind="ExternalInput").ap()
    cst = nc.dram_tensor("cst", [128, 512], F32, kind="ExternalInput").ap()
    x1o = nc.dram_tensor("x1", [TPC, D_MODEL], F32, kind="ExternalOutput").ap()
    xg = nc.dram_tensor("xg", [N_EXPERTS, 128, KC, CAP], BF16, kind="ExternalOutput").ap()
    selw = nc.dram_tensor("selw", [128, N_EXPERTS, TPC], BF16, kind="ExternalOutput").ap()
    H = nc.dram_tensor("Hscr", [TPC, D_MODEL], F32, kind="Internal").ap()
    with ExitStack() as outer:
        S = Sched(nc, outer)
        Hb = [Buf(f"H{i}") for i in range(NT)]
        xb_ = [Buf(f"xr{i}") for i in range(NT)]
        with ExitStack() as es:
            C = Ctx(nc, es, S)
            PS = C.psum_banks(8)
            Y = C.sb([128, KC, TPC], BF16, "Y")
            y2 = C.sb([128, 8, TPC], BF16, "y2")
            Wg = C.sb([128, 8, S5_WIDTH], BF16, "Wglu")
            sig = [C.sb([128, 512], F32, "sig") for _ in range(2)]
            yv = ymix.rearrange("(k p) t -> p k t", p=128)
            for j in range(4):
                S.dma("sp", Y[:, 8 * j:8 * j + 8, :], yv[:, 8 * j:8 * j + 8, :], writes=[Y.b])
            S.dma("pool", Wg[:], wglu.rearrange("(r p) n -> p r n", p=128), writes=[Wg.b])
            gb = BankPool(PS[4:8])
            n = 0
            for jc in range(8):
                for th in range(2):
                    ps = gb.get()
                    tsl = slice(th * 512, (th + 1) * 512)
                    for r in range(8):
                        S.op("pe", lambda e, r=r: e.matmul(ps[:, :], Wg[:, r, jc * 128:(jc + 1) * 128], Y[:, 4 * r + 3, tsl], start=(r == 0), stop=(r == 7)),
                             reads=[Wg.b, Y.b], writes=[ps.b])
                    sg = sig[n % 2]
                    n += 1
                    S.op("act", lambda e: e.activation(out=sg[:], in_=ps[:, :], func=AF.Sigmoid), reads=[ps.b], writes=[sg.b])
                    S.op("dve", lambda e: e.tensor_tensor(out=y2[:, jc, tsl], in0=Y[:, 4 * jc + 3, tsl], in1=sg[:], op=ALU.mult),
                         reads=[Y.b, sg.b], writes=[y2.b])

            def lhs_fn(k, i):
                tsl = slice(i * 128, (i + 1) * 128)
                if k % 4 == 3:
                    return y2[:, k // 4, tsl], y2.b
                return Y[:, k, tsl], Y.b

            emit_proj_res(S, C, PS, lhs_fn, KC, wo.rearrange("(k p) n -> p k n", p=128), True, x, xb_, H, Hb)
            S.barrier()
        with ExitStack() as es2:
            C = Ctx(nc, es2, S)
            PS = C.psum_banks(8)
            CS, CSb = emit_consts(S, C, cst)
            identb, onesb, lstr = CSb[:, 0, :], CSb[:, 1, :], CSb[:, 2, :]
            iota = CS[:, 3, :]
            X1b = C.sb([128, NT, D_MODEL], BF16, "X1b")
            X1bb = [Buf(f"x1b{i}") for i in range(NT)]
            M1 = [C.sb([128, 32], F32, "M1") for _ in range(NT)]
            M2 = [C.sb([128, 32], F32, "M2") for _ in range(NT)]
            MAf = [C.sb([128, 32], F32, "MAf") for _ in range(NT)]
            MA = [C.sb([128, 32], BF16, "MA") for _ in range(NT)]
            CWm = [C.sb([128, 32], F32, "CWm") for _ in range(NT)]
            POS = [C.sb([128, 32], F32, "POS") for _ in range(NT)]
            x1ob = [Buf(f"x1o{i}") for i in range(NT)]
            with ExitStack() as es2a:
                Ca = Ctx(nc, es2a, S)
                Wr = Ca.sb([128, KC, 36], BF16, "Wr")
                RB = Ca.sb([128, 36], F32, "RB")
                S.dma("pool", Wr[:], wr.rearrange("(k p) n -> p k n", p=128), writes=[Wr.b])
                S.dma("sp", RB[:], rb, writes=[RB.b])
                x1T = Ca.sb([128, KC, 128], BF16, "x1T")
                lg = Ca.sb([128, 36], F32, "lg")
                sm = {k: Ca.sb([128, 1], F32, k) for k in ("gmax", "ngmax", "se", "grp", "m1", "m2", "d", "ed", "den", "w1", "w2", "cw1", "cw2")}
                ex4 = Ca.sb([128, 4], F32); maskg = Ca.sb([128, 4], F32); pen = Ca.sb([128, 4], F32)
                elm = Ca.sb([128, 32], F32); elm2 = Ca.sb([128, 32], F32)
                tb = BankPool(PS[0:6])
                lb = BankPool(PS[6:8])

                def dv(fn, reads, writes):
                    S.op("dve", fn, reads=[t.b for t in reads], writes=[t.b for t in writes])

                def ac(fn, reads, writes):
                    S.op("act", fn, reads=[t.b for t in reads], writes=[t.b for t in writes])

                def ln_cb(i, t):
                    S.dma("sp", x1o[i * 128:(i + 1) * 128, :], t[:], reads=[t.b], writes=[x1ob[i]])
                    S.op("act", lambda e: e.copy(X1b[:, i, :], t[:]), reads=[t.b], writes=[X1bb[i]])
                    v = View(lambda k: X1b[:, i, k[1]], X1bb[i])
                    emit_transpose_tile(S, tb, v, identb, CSb, lambda g: (x1T[:, g * 8:(g + 1) * 8, :], x1T.b))
                    ps = lb.get()
                    for kc in range(KC):
                        S.op("pe", lambda e, kc=kc: e.matmul(ps[:, 0:36], x1T[:, kc, :], Wr[:, kc, :], start=(kc == 0), stop=(kc == KC - 1)),
                             reads=[x1T.b, Wr.b], writes=[ps.b])
                    dv(lambda e: e.tensor_tensor(out=lg[:], in0=ps[:, 0:36], in1=RB[:], op=ALU.add), [ps, RB], [lg])
                    dv(lambda e: e.reduce_max(out=sm["gmax"][:], in_=lg[:, 0:4], axis=AX.X), [lg], [sm["gmax"]])
                    dv(lambda e: e.tensor_scalar(out=sm["ngmax"][:], in0=sm["gmax"][:], scalar1=-1.0, scalar2=None, op0=ALU.mult), [sm["gmax"]], [sm["ngmax"]])
                    ac(lambda e: e.activation(out=ex4[:], in_=lg[:, 0:4], func=AF.Exp, bias=sm["ngmax"][:, 0:1], accum_out=sm["se"][:]), [lg, sm["ngmax"]], [ex4, sm["se"]])
                    dv(lambda e: e.reciprocal(sm["grp"][:], sm["se"][:]), [sm["se"]], [sm["grp"]])
                    dv(lambda e: e.tensor_scalar(out=maskg[:], in0=lg[:, 0:4], scalar1=sm["gmax"][:, 0:1], scalar2=None, op0=ALU.is_equal), [lg, sm["gmax"]], [maskg])
                    dv(lambda e: e.tensor_scalar(out=pen[:], in0=maskg[:], scalar1=-1.0, scalar2=BIG, op0=ALU.add, op1=ALU.mult), [maskg], [pen])
                    for g in range(4):
                        dv(lambda e, g=g: e.tensor_scalar(out=elm[:, 8 * g:8 * g + 8], in0=lg[:, 4 + 8 * g:12 + 8 * g], scalar1=pen[:, g:g + 1], scalar2=None, op0=ALU.add),
                           [lg, pen], [elm])
                    dv(lambda e: e.reduce_max(out=sm["m1"][:], in_=elm[:], axis=AX.X), [elm], [sm["m1"]])
                    dv(lambda e: e.tensor_scalar(out=M1[i][:], in0=elm[:], scalar1=sm["m1"][:, 0:1], scalar2=None, op0=ALU.is_equal), [elm, sm["m1"]], [M1[i]])
                    dv(lambda e: e.scalar_tensor_tensor(out=elm2[:], in0=M1[i][:], scalar=-BIG, in1=elm[:], op0=ALU.mult, op1=ALU.add), [M1[i], elm], [elm2])
                    dv(lambda e: e.reduce_max(out=sm["m2"][:], in_=elm2[:], axis=AX.X), [elm2], [sm["m2"]])
                    dv(lambda e: e.tensor_scalar(out=M2[i][:], in0=elm2[:], scalar1=sm["m2"][:, 0:1], scalar2=None, op0=ALU.is_equal), [elm2, sm["m2"]], [M2[i]])
                    dv(lambda e: e.tensor_tensor(out=sm["d"][:], in0=sm["m2"][:], in1=sm["m1"][:], op=ALU.subtract), [sm["m2"], sm["m1"]], [sm["d"]])
                    ac(lambda e: e.activation(out=sm["ed"][:], in_=sm["d"][:], func=AF.Exp), [sm["d"]], [sm["ed"]])
                    dv(lambda e: e.tensor_scalar(out=sm["den"][:], in0=sm["ed"][:], scalar1=1.0, scalar2=None, op0=ALU.add), [sm["ed"]], [sm["den"]])
                    dv(lambda e: e.reciprocal(sm["w1"][:], sm["den"][:]), [sm["den"]], [sm["w1"]])
                    dv(lambda e: e.tensor_tensor(out=sm["w2"][:], in0=sm["ed"][:], in1=sm["w1"][:], op=ALU.mult), [sm["ed"], sm["w1"]], [sm["w2"]])
                    dv(lambda e: e.tensor_tensor(out=sm["cw1"][:], in0=sm["w1"][:], in1=sm["grp"][:], op=ALU.mult), [sm["w1"], sm["grp"]], [sm["cw1"]])
                    dv(lambda e: e.tensor_tensor(out=sm["cw2"][:], in0=sm["w2"][:], in1=sm["grp"][:], op=ALU.mult), [sm["w2"], sm["grp"]], [sm["cw2"]])
                    dv(lambda e: e.tensor_tensor(out=MAf[i][:], in0=M1[i][:], in1=M2[i][:], op=ALU.add), [M1[i], M2[i]], [MAf[i]])
                    dv(lambda e: e.tensor_copy(MA[i][:], MAf[i][:]), [MAf[i]], [MA[i]])
                    dv(lambda e: e.tensor_scalar(out=CWm[i][:], in0=M1[i][:], scalar1=sm["cw1"][:, 0:1], scalar2=None, op0=ALU.mult), [M1[i], sm["cw1"]], [CWm[i]])
                    dv(lambda e: e.scalar_tensor_tensor(out=CWm[i][:], in0=M2[i][:], scalar=sm["cw2"][:, 0:1], in1=CWm[i][:], op0=ALU.mult, op1=ALU.add),
                       [M2[i], sm["cw2"], CWm[i]], [CWm[i]])

                emit_ln_tiles(S, Ca, H, Hb, lng, lnb, ln_cb)
                for i in range(NT):
                    ps = lb.get()
                    for i2 in range(i):
                        S.op("pe", lambda e, i2=i2: e.matmul(ps[:, 0:32], onesb, MA[i2][:], start=(i2 == 0), stop=False), reads=[CSb.b, MA[i2].b], writes=[ps.b])
                    S.op("pe", lambda e: e.matmul(ps[:, 0:32], lstr, MA[i][:], start=(i == 0), stop=True), reads=[CSb.b, MA[i].b], writes=[ps.b])
                    S.op("act", lambda e: e.copy(POS[i][:], ps[:, 0:32]), reads=[ps.b], writes=[POS[i].b])
                S.barrier()
            with ExitStack() as es2b:
                Cb = Ctx(nc, es2b, S)
                SELW = Cb.sb([128, N_EXPERTS, TPC], BF16, "SELW")
                Sel = [[Cb.sb([128, CAP], BF16, "Sel") for _ in range(NT)] for _ in range(2)]
                Swt = [Cb.sb([128, CAP], BF16, "Swt") for _ in range(4)]
                XG = [Cb.sb([128, KC, CAP], BF16, "XG") for _ in range(2)]
                gbk = BankPool(PS[0:5])
                sbk = BankPool(PS[5:8])
                nsw = 0
                for ex in range(N_EXPERTS):
                    sel = Sel[ex % 2]
                    ps_t = sbk.get()
                    ps_tb = ps_t[:].bitcast(BF16)
                    for i in range(NT):
                        S.op("dve", lambda e, i=i: e.tensor_scalar(out=sel[i][:], in0=iota, scalar1=POS[i][:, ex:ex + 1], scalar2=MAf[i][:, ex:ex + 1],
                                                                   op0=ALU.is_equal, op1=ALU.mult), reads=[CS.b, POS[i].b, MAf[i].b], writes=[sel[i].b])
                        sw = Swt[nsw % 4]
                        nsw += 1
                        S.op("dve", lambda e, i=i, sw=sw: e.tensor_scalar(out=sw[:], in0=iota, scalar1=POS[i][:, ex:ex + 1], scalar2=CWm[i][:, ex:ex + 1],
                                                                          op0=ALU.is_equal, op1=ALU.mult), reads=[CS.b, POS[i].b, CWm[i].b], writes=[sw.b])
                        S.op("pe", lambda e, i=i, sw=sw: e.transpose(ps_tb[:, i * 128:(i + 1) * 128], sw[:], identb), reads=[sw.b, CSb.b], writes=[ps_t.b])
                    S.op("act", lambda e: e.copy(SELW[:, ex, :], ps_tb[:, 0:1024]), reads=[ps_t.b], writes=[SELW.b])
                    xg_ = XG[ex % 2]
                    for g in range(8):
                        ps = gbk.get()
                        for j in range(4):
                            kc = g * 4 + j
                            for i in range(NT):
                                S.op("pe", lambda e, i=i, j=j, kc=kc: e.matmul(ps[:, j * 128:(j + 1) * 128], X1b[:, i, kc * 128:(kc + 1) * 128], sel[i][:],
                                                                            start=(i == 0), stop=(i == NT - 1)),
                                     reads=[X1bb[i], sel[i].b], writes=[ps.b])
                        if g % 2 == 0:
                            S.op("act", lambda e, g=g: e.copy(xg_[:, 4 * g:4 * g + 4, :], ps[:, :].rearrange("p (a b) -> p a b", a=4)), reads=[ps.b], writes=[xg_.b])
                        else:
                            S.op("dve", lambda e, g=g: e.tensor_copy(xg_[:, 4 * g:4 * g + 4, :], ps[:, :].rearrange("p (a b) -> p a b", a=4)), reads=[ps.b], writes=[xg_.b])
                    S.dma("sp", xg[ex], xg_[:], reads=[xg_.b])
                S.dma("sp", selw, SELW[:], reads=[SELW.b])
                S.barrier()
        S.barrier()
    return nc


EPC = N_EXPERTS // NCORES
SLOTS = NCORES * CAP


def build_b2():
    nc = bass.Bass("TRN2", target_bir_lowering=False)
    xg = nc.dram_tensor("xg", [EPC, 128, KC, SLOTS], BF16, kind="ExternalInput").ap()
    wg_ = nc.dram_tensor("wg", [EPC, D_MODEL, D_EXPERT], F32, kind="ExternalInput").ap()
    wu_ = nc.dram_tensor("wu", [EPC, D_MODEL, D_EXPERT], F32, kind="ExternalInput").ap()
    wd_ = nc.dram_tensor("wd", [EPC, D_EXPERT, D_MODEL], F32, kind="ExternalInput").ap()
    cst = nc.dram_tensor("cst", [128, 512], F32, kind="ExternalInput").ap()
    yo = nc.dram_tensor("yo", [EPC, SLOTS, D_MODEL], BF16, kind="ExternalOutput").ap()
    NST = SLOTS // 128
    with ExitStack() as outer:
        S = Sched(nc, outer)
        C = Ctx(nc, outer, S)
        PS = C.psum_banks(8)
        CS, CSb = emit_consts(S, C, cst)
        identb = CSb[:, 0, :]
        Wg = C.sb([128, KC, D_EXPERT], BF16, "Wg")
        Wu = C.sb([128, KC, D_EXPERT], BF16, "Wu")
        Wd = C.sb([128, 4, D_MODEL], BF16, "Wd")
        X = C.sb([128, KC, SLOTS], BF16, "X")
        hidT = C.sb([128, 4, SLOTS], BF16, "hidT")
        sg = [C.sb([128, 512], F32, "sg") for _ in range(2)]
        hid = [C.sb([128, 512], BF16, "hid") for _ in range(2)]
        Yt = [C.sb([128, D_MODEL], BF16, "Yt") for _ in range(2)]
        gub = BankPool(PS[0:4])
        tbk = BankPool(PS[4:5])
        dbk = BankPool(PS[5:8])
        ny = 0
        for ex in range(EPC):
            wgv = wg_[ex].rearrange("(k p) n -> p k n", p=128)
            wuv = wu_[ex].rearrange("(k p) n -> p k n", p=128)
            wdv = wd_[ex].rearrange("(m p) n -> p m n", p=128)
            for j in range(4):
                S.dma("pool", Wg[:, 8 * j:8 * j + 8, :], wgv[:, 8 * j:8 * j + 8, :], writes=[Wg.b])
            for j in range(4):
                S.dma("pool", Wu[:, 8 * j:8 * j + 8, :], wuv[:, 8 * j:8 * j + 8, :], writes=[Wu.b])
            for j in range(4):
                S.dma("sp", X[:, 8 * j:8 * j + 8, :], xg[ex][:, 8 * j:8 * j + 8, :], writes=[X.b])
            for j in range(4):
                S.dma("pool", Wd[:, j, :], wdv[:, j, :], writes=[Wd.b])
            for st in range(NST):
                ssl = slice(st * 128, (st + 1) * 128)
                pg = gub.get()
                pu = gub.get()
                for kc in range(KC):
                    S.op("pe", lambda e, kc=kc: e.matmul(pg[:, :], X[:, kc, ssl], Wg[:, kc, :], start=(kc == 0), stop=(kc == KC - 1)), reads=[X.b, Wg.b], writes=[pg.b])
                for kc in range(KC):
                    S.op("pe", lambda e, kc=kc: e.matmul(pu[:, :], X[:, kc, ssl], Wu[:, kc, :], start=(kc == 0), stop=(kc == KC - 1)), reads=[X.b, Wu.b], writes=[pu.b])
                s_, h_ = sg[st % 2], hid[st % 2]
                S.op("act", lambda e: e.activation(out=s_[:], in_=pg[:, :], func=AF.Silu), reads=[pg.b], writes=[s_.b])
                S.op("dve", lambda e: e.tensor_tensor(out=h_[:], in0=s_[:], in1=pu[:, :], op=ALU.mult), reads=[s_.b, pu.b], writes=[h_.b])
                pt = tbk.get()
                ptb = pt[:].bitcast(BF16)
                for mc in range(4):
                    S.op("pe", lambda e, mc=mc: e.transpose(ptb[:, mc * 128:(mc + 1) * 128], h_[:, mc * 128:(mc + 1) * 128], identb), reads=[h_.b, CSb.b], writes=[pt.b])
                S.op("act", lambda e: e.copy(hidT[:, :, ssl], ptb[:, 0:512].rearrange("p (a b) -> p a b", a=4)), reads=[pt.b], writes=[hidT.b])
            for st in range(NST):
                ssl = slice(st * 128, (st + 1) * 128)
                y_ = Yt[ny % 2]
                ny += 1
                for n in range(8):
                    pd = dbk.get()
                    for mc in range(4):
                        S.op("pe", lambda e, mc=mc: e.matmul(pd[:, :], hidT[:, mc, ssl], Wd[:, mc, n * 512:(n + 1) * 512], start=(mc == 0), stop=(mc == 3)),
                             reads=[hidT.b, Wd.b], writes=[pd.b])
                    if n % 2 == 0:
                        S.op("act", lambda e: e.copy(y_[:, n * 512:(n + 1) * 512], pd[:, :]), reads=[pd.b], writes=[y_.b])
                    else:
                        S.op("dve", lambda e: e.tensor_copy(y_[:, n * 512:(n + 1) * 512], pd[:, :]), reads=[pd.b], writes=[y_.b])
                S.dma("sp", yo[ex, ssl, :], y_[:], reads=[y_.b])
        S.barrier()
    return nc


def build_b3():
    nc = bass.Bass("TRN2", target_bir_lowering=False)
    yin = nc.dram_tensor("yin", [N_EXPERTS * CAP, D_MODEL], BF16, kind="ExternalInput").ap()
    selw = nc.dram_tensor("selw", [128, N_EXPERTS, TPC], BF16, kind="ExternalInput").ap()
    x1 = nc.dram_tensor("x1", [TPC, D_MODEL], F32, kind="ExternalInput").ap()
    lng = nc.dram_tensor("lng", [128, D_MODEL], F32, kind="ExternalInput").ap()
    lnb = nc.dram_tensor("lnb", [128, D_MODEL], F32, kind="ExternalInput").ap()
    cst = nc.dram_tensor("cst", [128, 512], F32, kind="ExternalInput").ap()
    x2 = nc.dram_tensor("x2", [TPC, D_MODEL], F32, kind="ExternalOutput").ap()
    xTo = nc.dram_tensor("xTo", [D_MODEL, TPC], BF16, kind="ExternalOutput").ap()
    H = nc.dram_tensor("Hscr", [TPC, D_MODEL], F32, kind="Internal").ap()
    with ExitStack() as outer:
        S = Sched(nc, outer)
        Hb = [Buf(f"H{i}") for i in range(NT)]
        xb_ = [Buf(f"xr{i}") for i in range(NT)]
        with ExitStack() as es:
            C = Ctx(nc, es, S)
            PS = C.psum_banks(8)
            SW = C.sb([128, N_EXPERTS, TPC], BF16, "SW")
            for j in range(4):
                S.dma("sp", SW[:, 8 * j:8 * j + 8, :], selw[:, 8 * j:8 * j + 8, :], writes=[SW.b])
            emit_proj_res(S, C, PS, lambda k, i: (SW[:, k, i * 128:(i + 1) * 128], SW.b), N_EXPERTS,
                          yin.rearrange("(e s) d -> s e d", s=128), False, x1, xb_, H, Hb)
            S.barrier()
        with ExitStack() as es2:
            C = Ctx(nc, es2, S)
            PS = C.psum_banks(8)
            CS, CSb = emit_consts(S, C, cst)
            xTs = C.sb([128, KC, TPC], BF16, "xTs")
            xb = [C.sb([128, D_MODEL], BF16, "xb") for _ in range(2)]
            banks = BankPool(PS)
            x2b = [Buf(f"x2{i}") for i in range(NT)]

            def cb(i, t):
                S.dma("sp", x2[i * 128:(i + 1) * 128, :], t[:], reads=[t.b], writes=[x2b[i]])
                b_ = xb[i % 2]
                S.op("act", lambda e: e.copy(b_[:], t[:]), reads=[t.b], writes=[b_.b])
                emit_transpose_tile(S, banks, b_, CSb[:, 0, :], CSb, lambda g: (xTs[:, g * 8:(g + 1) * 8, i * 128:(i + 1) * 128], xTs.b))

            emit_ln_tiles(S, C, H, Hb, lng, lnb, cb)
            S.dma("sp", xTo.rearrange("(kc p) t -> p kc t", p=128), xTs[:], reads=[xTs.b])
            S.barrier()
        S.barrier()
    return nc


_NC_CACHE = {}
_DBG = None


def _get_nc(name, fn):
    if name not in _NC_CACHE:
        _NC_CACHE[name] = fn()
    return _NC_CACHE[name]


def _run(nc, maps):
    res = run_bass_kernel_spmd(nc, maps, core_ids=list(range(len(maps))))
    return res.results


def mix_perm():
    p = []
    for r in range(NCORES):
        p.append(np.arange(384 * r, 384 * r + 384))
        p.append(GDN_WIDTH + np.arange(128 * r, 128 * r + 128))
    return np.concatenate(p)


def prep_b1_weights(layer, inp):
    wo = np.ascontiguousarray(inp["w_out"][layer][mix_perm(), :])
    wglu = np.ascontiguousarray(inp["s5_w_glu"][layer])
    wr = np.concatenate([inp["router_group_w"][layer]] + [inp["router_expert_w"][layer][g] for g in range(4)], axis=1)
    rbv = np.concatenate([inp["router_group_b"][layer], inp["router_expert_b"][layer].reshape(-1)])
    return {"wo": wo, "wglu": wglu, "wr": np.ascontiguousarray(wr.astype(np.float32)),
            "rb": np.ascontiguousarray(np.broadcast_to(rbv[None, :], (128, 36))).astype(np.float32),
            "lng": np.ascontiguousarray(np.broadcast_to(inp["ln1_g"][layer][None, :], (128, D_MODEL))),
            "lnb": np.ascontiguousarray(np.broadcast_to(inp["ln1_b"][layer][None, :], (128, D_MODEL))),
            "cst": b_consts()}


def run_layer(layer, inp, xT_all, xres, ncores=NCORES, L=SEQ):
    ncm = _get_nc(("mixer", L), lambda: build_mixer(L, True))
    maps = []
    for c in range(NCORES):
        m = prep_mixer(c, layer, inp)
        m["xT"] = xT_all
        maps.append(m)
    res = _run(ncm, maps)
    ymix_all = np.concatenate([np.asarray(res[c]["yT"]) for c in range(NCORES)], axis=0)
    del res, maps
    ntc = L // TPC
    w1 = prep_b1_weights(layer, inp)
    maps = []
    for c in range(ntc):
        m = dict(w1)
        m["ymix"] = np.ascontiguousarray(ymix_all[:, c * TPC:(c + 1) * TPC])
        m["x"] = xres[c]
        maps.append(m)
    res = _run(_get_nc("b1", build_b1), maps)
    x1 = [np.asarray(res[c]["x1"]) for c in range(ntc)]
    if _DBG is not None:
        _DBG["ymix"] = ymix_all
        _DBG["x1"] = x1
    xg = [np.asarray(res[c]["xg"]) for c in range(ntc)]
    selw = [np.asarray(res[c]["selw"]) for c in range(ntc)]
    del res, maps
    maps = []
    for c2 in range(NCORES):
        xin = np.zeros((EPC, 128, KC, SLOTS), ml_dtypes.bfloat16)
        for c in range(ntc):
            xin[:, :, :, c * CAP:(c + 1) * CAP] = xg[c][EPC * c2:EPC * c2 + EPC]
        maps.append({"xg": xin,
                     "wg": np.ascontiguousarray(inp["expert_w_gate"][layer][EPC * c2:EPC * c2 + EPC]),
                     "wu": np.ascontiguousarray(inp["expert_w_up"][layer][EPC * c2:EPC * c2 + EPC]),
                     "wd": np.ascontiguousarray(inp["expert_w_down"][layer][EPC * c2:EPC * c2 + EPC]),
                     "cst": b_consts()})
    del xg
    res = _run(_get_nc("b2", build_b2), maps)
    yo = [np.asarray(res[c2]["yo"]) for c2 in range(NCORES)]
    del res, maps
    lng = np.ascontiguousarray(np.broadcast_to(inp["ln2_g"][layer][None, :], (128, D_MODEL)))
    lnb = np.ascontiguousarray(np.broadcast_to(inp["ln2_b"][layer][None, :], (128, D_MODEL)))
    maps = []
    for c in range(ntc):
        yin = np.concatenate([yo[e // EPC][e % EPC, c * CAP:(c + 1) * CAP, :] for e in range(N_EXPERTS)], axis=0)
        maps.append({"yin": np.ascontiguousarray(yin), "selw": selw[c], "x1": x1[c], "lng": lng, "lnb": lnb, "cst": b_consts()})
    res = _run(_get_nc("b3", build_b3), maps)
    x2 = [np.asarray(res[c]["x2"]) for c in range(ntc)]
    xTn = [np.asarray(res[c]["xTo"]) for c in range(ntc)]
    return x2, xTn


def kernel(**inputs):
    inp = {k: np.asarray(v) for k, v in inputs.items()}
    x = inp["x"][0]
    xres = [np.ascontiguousarray(x[c * TPC:(c + 1) * TPC]) for c in range(NCORES)]
    res = _run(_get_nc("t0", build_t0), [{"x": xres[c], "cst": b_consts()} for c in range(NCORES)])
    xT_all = np.stack([np.asarray(res[c]["xTo"]) for c in range(NCORES)], axis=0)
    for layer in range(DEPTH):
        xres, xTn = run_layer(layer, inp, xT_all, xres)
        xT_all = np.stack(xTn, axis=0)
    return np.concatenate(xres, axis=0)[None].astype(np.float32)
```

```python
import numpy as np
import ml_dtypes
from contextlib import ExitStack
import concourse.bass as bass
import concourse.mybir as mybir
from concourse.bass_utils import run_bass_kernel_spmd

F32 = mybir.dt.float32
BF16 = mybir.dt.bfloat16
I32 = mybir.dt.int32
AF = mybir.ActivationFunctionType
ALU = mybir.AluOpType
AX = mybir.AxisListType

D_MODEL = 4096
SEQ = 8192
DEPTH = 2
NCORES = 8
KC = D_MODEL // 128
GDN_HEADS = 24
HPC = 3
GDN_WIDTH = 3072
S5_WIDTH = 1024
S5_STATE = 64
GPC = 8
CH = 64
N_EXPERTS = 32
D_EXPERT = 512
DN_ALPHA = (2 * DEPTH) ** 0.25
LN_EPS = 1e-5
RMS_EPS = 1e-6
L2_EPS = 1e-6
CAP = 128
TWO_PI = float(2 * np.pi)
MAGIC = 12582912.0


class Ev:
    __slots__ = ("sem", "sid", "val", "eng")

    def __init__(self, sem, sid, val, eng):
        self.sem, self.sid, self.val, self.eng = sem, sid, val, eng


class Buf:
    def __init__(self, name, excl=False):
        self.name = name
        self.w = None
        self.r = {}
        self.excl = excl


class Sched:
    ROT = 8000
    NSLOT = 12

    def __init__(self, nc, es):
        self.nc, self.es = nc, es
        self.engs = dict(pe=nc.tensor, act=nc.scalar, dve=nc.vector, pool=nc.gpsimd, sp=nc.sync)
        self.nsem = 0
        self.cur = {}
        for e in self.engs:
            self.cur[e] = [self._newsem(e), 0]
        self.known = {e: {} for e in self.engs}
        self.slots = {}
        self.slot_i = {}
        self.nops = 0
        self.prev = {}

    def _newsem(self, tag):
        self.nsem += 1
        s = self.es.enter_context(self.nc.semaphore(f"s{self.nsem}_{tag}"))
        return (s, self.nsem)

    def _wait(self, e, ev):
        if ev is None:
            return
        if e == "pe" and ev.eng == "pe":
            return
        k = self.known[e]
        if k.get(ev.sid, -1) >= ev.val:
            return
        self.engs[e].wait_ge(ev.sem, ev.val)
        k[ev.sid] = ev.val

    def _deps(self, e, reads, writes):
        for b in reads:
            self._wait(e, b.w)
            if b.excl:
                for ev in list(b.r.values()):
                    self._wait(e, ev)
        for b in writes:
            self._wait(e, b.w)
            for ev in list(b.r.values()):
                self._wait(e, ev)

    def _commit(self, ev, reads, writes):
        for b in reads:
            if b.excl:
                b.w = ev
                b.r = {}
            else:
                b.r[ev.sid] = ev
        for b in writes:
            b.w = ev
            b.r = {}

    def op(self, e, fn, reads=(), writes=()):
        self._deps(e, reads, writes)
        ins = fn(self.engs[e])
        (sem, sid), cnt = self.cur[e]
        cnt += 1
        ins.then_inc(sem, 1)
        ev = Ev(sem, sid, cnt, e)
        self.cur[e][1] = cnt
        if cnt >= self.ROT:
            self.prev[e] = ev
            self.cur[e] = [self._newsem(e), 0]
        self._commit(ev, reads, writes)
        self.nops += 1
        return ev

    def dma(self, q, out, in_, reads=(), writes=(), **kw):
        if q not in self.slots:
            self.slots[q] = [[self._newsem("d" + q), 0, None] for _ in range(self.NSLOT)]
            self.slot_i[q] = 0
        i = self.slot_i[q]
        self.slot_i[q] = (i + 1) % self.NSLOT
        slot = self.slots[q][i]
        self._wait(q, slot[2])
        self._deps(q, reads, writes)
        ins = self.engs[q].dma_start(out=out, in_=in_, **kw)
        slot[1] += 16
        (sem, sid) = slot[0]
        ins.then_inc(sem, 16)
        ev = Ev(sem, sid, slot[1], "dma")
        slot[2] = ev
        self._commit(ev, reads, writes)
        self.nops += 1
        return ev

    def dma_ins(self, q, mk, reads=(), writes=()):
        if q not in self.slots:
            self.slots[q] = [[self._newsem("d" + q), 0, None] for _ in range(self.NSLOT)]
            self.slot_i[q] = 0
        i = self.slot_i[q]
        self.slot_i[q] = (i + 1) % self.NSLOT
        slot = self.slots[q][i]
        self._wait(q, slot[2])
        self._deps(q, reads, writes)
        ins = mk(self.engs[q])
        slot[1] += 16
        (sem, sid) = slot[0]
        ins.then_inc(sem, 16)
        ev = Ev(sem, sid, slot[1], "dma")
        slot[2] = ev
        self._commit(ev, reads, writes)
        return ev

    def barrier(self):
        evs = []
        for e in self.engs:
            (sem, sid), cnt = self.cur[e]
            if cnt > 0:
                evs.append(Ev(sem, sid, cnt, e))
            elif e in self.prev:
                evs.append(self.prev[e])
        for q in self.slots:
            for slot in self.slots[q]:
                if slot[2] is not None:
                    evs.append(slot[2])
        for e in self.engs:
            for ev in evs:
                self._wait(e, ev)

    def finish(self, bufs):
        for b in bufs:
            self._wait("sp", b.w)


class T:
    def __init__(self, t, name, excl=False):
        self.t = t
        self.b = Buf(name, excl)

    def __getitem__(self, k):
        return self.t[k]


_uid = [0]


class Ctx:
    def __init__(self, nc, es, S=None):
        self.nc, self.es = nc, es
        self.S = S if S is not None else Sched(nc, es)

    @property
    def n(self):
        return _uid[0]

    @n.setter
    def n(self, v):
        _uid[0] = v

    def sb(self, shape, dt=F32, name=None):
        self.n += 1
        nm = f"{name or 't'}_{self.n}"
        return T(self.es.enter_context(self.nc.sbuf_tensor(nm, list(shape), dt)), nm)

    def psum_banks(self, n=8):
        out = []
        for i in range(n):
            self.n += 1
            nm = f"ps{i}_{self.n}"
            out.append(T(self.es.enter_context(self.nc.psum_tensor(nm, [128, 512], F32)), nm, excl=True))
        return out


class View:
    def __init__(self, fn, b):
        self.fn, self.b = fn, b

    def __getitem__(self, k):
        return self.fn(k)


class BankPool:
    def __init__(self, banks):
        self.banks = list(banks)
        self.i = 0

    def get(self):
        b = self.banks[self.i]
        self.i = (self.i + 1) % len(self.banks)
        return b


GDN_COLS = 4 * HPC * 128 + 2 * HPC
NEG = -30000.0


def gdn_consts():
    c = np.zeros((128, 8, 128), np.float32)
    c[:, 0, :] = np.eye(128)
    i = np.arange(64)
    U = (i[:, None] <= i[None, :]).astype(np.float32)
    c[:64, 1, :64] = U
    c[:64, 2, :64] = -U
    c[:64, 3, :64] = (i[:, None] > i[None, :]).astype(np.float32)
    c[:, 4, :] = 1.0
    c[:64, 5, :64] = np.where(i[:, None] > i[None, :], 0.0, NEG)
    c[:64, 6, :64] = np.where(i[None, :] >= i[:, None], 0.0, NEG)
    return c.reshape(128, 8 * 128)


def emit_gdn(nc, S, L, x_bf16, xT, wg, convw, hp, cst, yT):
    NR = max(1, L // 1024)
    TR = min(L, 1024)
    NSB = L // 512
    xq = "sp" if x_bf16 else "pool"

    with ExitStack() as es:
        C = Ctx(nc, es, S)
        W = C.sb([128, KC, GDN_COLS], BF16, "W")
        XT = [C.sb([128, 8, 512], BF16, "XT") for _ in range(4)]
        CW = C.sb([128, 36], F32, "CW")
        HP = C.sb([128, 16], F32, "HP")
        CS = C.sb([128, 8, 128], F32, "CS")
        CSb = C.sb([128, 2, 128], BF16, "CSb")
        EAL = C.sb([128, 3], F32, "EAL")
        halo = C.sb([128, 9, 3], F32, "halo")
        raw = [C.sb([128, 515], F32, "raw") for _ in range(2)]
        cv = [C.sb([128, 512], F32, "cv") for _ in range(2)]
        actb = [C.sb([128, 512], F32, "actb") for _ in range(2)]
        sqb = [C.sb([128, 512], BF16, "sqb") for _ in range(2)]
        rnb = [C.sb([128, 512], F32, "rnb") for _ in range(2)]
        qn = [C.sb([128, 512], BF16, "qn") for _ in range(HPC)]
        kn = [C.sb([128, 512], BF16, "kn") for _ in range(HPC)]
        vT = [C.sb([128, 512], BF16, "vT") for _ in range(HPC)]
        zs = [C.sb([128, 512], F32, "zs") for _ in range(HPC)]
        abT = C.sb([8, 512], F32, "abT")
        yo = [C.sb([128, 512], BF16, "yo") for _ in range(HPC)]
        Sst = [C.sb([128, 128], F32, "S") for _ in range(HPC)]
        Sb = [C.sb([128, 128], BF16, "Sb") for _ in range(HPC)]
        PS = C.psum_banks(8)
        proj_banks = BankPool(PS[0:2])
        wk_banks = BankPool(PS[2:8])

        ident = CS[:, 0, :]
        identb = CSb[:, 0, :]
        onesb = CSb[:, 1, :]

        wv = wg.rearrange("(kc p) n -> p kc n", p=128)
        for j in range(8):
            S.dma("pool", W[:, 4 * j:4 * j + 4, :], wv[:, 4 * j:4 * j + 4, :], writes=[W.b])
        S.dma("sp", CW[:], convw, writes=[CW.b])
        S.dma("sp", HP[:], hp, writes=[HP.b])
        S.dma("sp", CS[:].rearrange("p a b -> p (a b)"), cst, writes=[CS.b])
        S.op("dve", lambda e: e.tensor_copy(CSb[:, 0, :], CS[:, 0, :]), reads=[CS.b], writes=[CSb.b])
        S.op("dve", lambda e: e.tensor_copy(CSb[:, 1, :], CS[:, 4, :]), reads=[CS.b], writes=[CSb.b])
        S.op("act", lambda e: e.activation(out=EAL[:], in_=HP[:, 0:3], func=AF.Exp), reads=[HP.b], writes=[EAL.b])
        S.op("dve", lambda e: e.memset(halo[:].rearrange("p a b -> p (a b)"), 0.0), writes=[halo.b])
        for h in range(HPC):
            S.op("dve", lambda e, h=h: e.memset(Sst[h][:], 0.0), writes=[Sst[h].b])
            S.op("dve", lambda e, h=h: e.memset(Sb[h][:], 0.0), writes=[Sb[h].b])

        xv = xT.rearrange("r (kc p) t -> r p kc t", p=128)

        def small(shape, dt=F32, name="w"):
            return C.sb(shape, dt, name)

        NCG = 2
        NU = NCG * HPC
        GS = [dict(
            x1=small([64, 3]), ex=small([64, 3]), sp=small([64, 3]), g=small([64, 3]), beta=small([64, 3]),
            edec=small([64, 3]), edlm=small([64, 3]), edl=small([128, 3]), bedec=small([64, 3]),
        ) for _ in range(NCG)]
        HW = []
        for u in range(NU):
            HW.append(dict(
                Gb=small([64, 64]), gs=small([64, 64]), gT=small([64, 64]),
                kbe=small([64, 128], BF16), ktail=small([64, 128], BF16),
                bv=small([64, 128], BF16), N=small([64, 64]),
                P=[small([64, 64]), small([64, 64])], Q=[small([64, 64]), small([64, 64])],
                X=small([64, 64]), Xb=small([64, 64], BF16), qkT=small([64, 64], BF16),
                nwT=small([128, 64], BF16), Lm=small([64, 128]), qd=small([128, 64], BF16),
                vn=small([64, 128], BF16), ssq=small([64, 1]), rt=small([64, 1]), rstd=small([64, 1]),
                junk=small([64, 128], BF16),
            ))

        def proj_tile(c0, m):
            ps = proj_banks.get()
            for kc in range(KC):
                S.op("pe", lambda e, kc=kc: e.matmul(ps[0:m, :], W[:, kc, c0:c0 + m], XT[kc // 8][:, kc % 8, :],
                                                     start=(kc == 0), stop=(kc == KC - 1)),
                     reads=[W.b, XT[kc // 8].b], writes=[ps.b])
            return ps

        for sb in range(NSB):
            r = (sb * 512) // TR
            t0 = (sb * 512) % TR
            for j in range(4):
                S.dma(xq, XT[j][:], xv[r, :, 8 * j:8 * j + 8, t0:t0 + 512], writes=[XT[j].b])
            rot = 0
            for kind, base in (("k", 384), ("q", 0), ("v", 768)):
                for h in range(HPC):
                    ct = {"q": 0, "k": 3, "v": 6}[kind] + h
                    ps = proj_tile(base + 128 * h, 128)
                    rw, cvb, ab_, sq_, rn_ = raw[rot], cv[rot], actb[rot], sqb[rot], rnb[rot]
                    rot ^= 1
                    S.op("act", lambda e: e.copy(rw[:, 3:515], ps[:, :]), reads=[ps.b], writes=[rw.b])
                    S.op("dve", lambda e: e.tensor_copy(rw[:, 0:3], halo[:, ct, :]), reads=[halo.b], writes=[rw.b])
                    S.op("dve", lambda e: e.tensor_scalar(out=cvb[:], in0=rw[:, 0:512], scalar1=CW[:, 4 * ct:4 * ct + 1],
                                                          scalar2=None, op0=ALU.mult), reads=[rw.b, CW.b], writes=[cvb.b])
                    for j in range(1, 4):
                        S.op("dve", lambda e, j=j: e.scalar_tensor_tensor(out=cvb[:], in0=rw[:, j:j + 512],
                                                                          scalar=CW[:, 4 * ct + j:4 * ct + j + 1], in1=cvb[:],
                                                                          op0=ALU.mult, op1=ALU.add),
                             reads=[rw.b, CW.b, cvb.b], writes=[cvb.b])
                    S.op("dve", lambda e: e.tensor_copy(halo[:, ct, :], rw[:, 512:515]), reads=[rw.b], writes=[halo.b])
                    if kind == "v":
                        S.op("act", lambda e: e.activation(out=vT[h][:], in_=cvb[:], func=AF.Silu), reads=[cvb.b], writes=[vT[h].b])
                        continue
                    S.op("act", lambda e: e.activation(out=ab_[:], in_=cvb[:], func=AF.Silu), reads=[cvb.b], writes=[ab_.b])
                    S.op("act", lambda e: e.activation(out=sq_[:], in_=ab_[:], func=AF.Square), reads=[ab_.b], writes=[sq_.b])
                    ps2 = proj_banks.get()
                    S.op("pe", lambda e: e.matmul(ps2[:, :], onesb, sq_[:], start=True, stop=True), reads=[CSb.b, sq_.b], writes=[ps2.b])
                    S.op("act", lambda e: e.activation(out=rn_[:], in_=ps2[:, :], func=AF.Sqrt, bias=L2_EPS), reads=[ps2.b], writes=[rn_.b])
                    S.op("dve", lambda e: e.reciprocal(rn_[:], rn_[:]), reads=[rn_.b], writes=[rn_.b])
                    dst = qn[h] if kind == "q" else kn[h]
                    sc = 128.0 ** -0.5 if kind == "q" else 1.0
                    S.op("dve", lambda e: e.scalar_tensor_tensor(out=dst[:], in0=ab_[:], scalar=sc, in1=rn_[:], op0=ALU.mult, op1=ALU.mult),
                         reads=[ab_.b, rn_.b], writes=[dst.b])
            for h in range(HPC):
                ps = proj_tile(1152 + 128 * h, 128)
                S.op("act", lambda e: e.activation(out=zs[h][:], in_=ps[:, :], func=AF.Silu), reads=[ps.b], writes=[zs[h].b])
            ps = proj_tile(1536, 6)
            S.op("act", lambda e: e.copy(abT[0:6, :], ps[0:6, :]), reads=[ps.b], writes=[abT.b])

            U_ = CS[0:64, 1, 0:64]
            nU_ = CS[0:64, 2, 0:64]
            for cg in range(8 // NCG):
                units = []
                for ci in range(NCG):
                    c = cg * NCG + ci
                    for h in range(HPC):
                        u = ci * HPC + h
                        units.append(dict(h=h, cs=slice(64 * c, 64 * c + 64), w=HW[u], G=GS[ci], ps=PS[2 + u]))
                gun = [dict(cs=slice(64 * (cg * NCG + ci), 64 * (cg * NCG + ci) + 64), G=GS[ci], ps=PS[2 + ci * HPC]) for ci in range(NCG)]
                gate_steps = [
                    lambda g: S.op("pe", lambda e: e.transpose(g["ps"][0:64, 0:6], abT[0:6, g["cs"]], ident[0:6, 0:6]), reads=[abT.b, CS.b], writes=[g["ps"].b]),
                    lambda g: S.op("dve", lambda e: e.tensor_tensor(out=g["G"]["x1"][:], in0=g["ps"][0:64, 0:3], in1=HP[0:64, 3:6], op=ALU.add),
                                   reads=[g["ps"].b, HP.b], writes=[g["G"]["x1"].b]),
                    lambda g: S.op("act", lambda e: e.activation(out=g["G"]["beta"][:], in_=g["ps"][0:64, 3:6], func=AF.Sigmoid), reads=[g["ps"].b], writes=[g["G"]["beta"].b]),
                    lambda g: S.op("act", lambda e: e.activation(out=g["G"]["ex"][:], in_=g["G"]["x1"][:], func=AF.Exp), reads=[g["G"]["x1"].b], writes=[g["G"]["ex"].b]),
                    lambda g: S.op("act", lambda e: e.activation(out=g["G"]["sp"][:], in_=g["G"]["ex"][:], func=AF.Ln, bias=1.0), reads=[g["G"]["ex"].b], writes=[g["G"]["sp"].b]),
                    lambda g: S.op("dve", lambda e: e.scalar_tensor_tensor(out=g["G"]["g"][:], in0=g["G"]["sp"][:], scalar=-1.0, in1=EAL[0:64, :], op0=ALU.mult, op1=ALU.mult),
                                   reads=[g["G"]["sp"].b, EAL.b], writes=[g["G"]["g"].b]),
                    lambda g: S.op("pe", lambda e: e.matmul(g["ps"][0:64, 8:11], CS[0:64, 1, 0:64], g["G"]["g"][:], start=True, stop=True), reads=[CS.b, g["G"]["g"].b], writes=[g["ps"].b]),
                    lambda g: S.op("pe", lambda e: e.matmul(g["ps"][0:64, 16:19], CS[0:64, 3, 0:64], g["G"]["g"][:], start=True, stop=True), reads=[CS.b, g["G"]["g"].b], writes=[g["ps"].b]),
                    lambda g: S.op("pe", lambda e: e.matmul(g["ps"][:, 24:27], CS[0:64, 4, :], g["G"]["g"][:], start=True, stop=True), reads=[CS.b, g["G"]["g"].b], writes=[g["ps"].b]),
                    lambda g: S.op("act", lambda e: e.activation(out=g["G"]["edec"][:], in_=g["ps"][0:64, 8:11], func=AF.Exp), reads=[g["ps"].b], writes=[g["G"]["edec"].b]),
                    lambda g: S.op("act", lambda e: e.activation(out=g["G"]["edlm"][:], in_=g["ps"][0:64, 16:19], func=AF.Exp), reads=[g["ps"].b], writes=[g["G"]["edlm"].b]),
                    lambda g: S.op("act", lambda e: e.activation(out=g["G"]["edl"][:], in_=g["ps"][:, 24:27], func=AF.Exp), reads=[g["ps"].b], writes=[g["G"]["edl"].b]),
                    lambda g: S.op("dve", lambda e: e.tensor_tensor(out=g["G"]["bedec"][:], in0=g["G"]["beta"][:], in1=g["G"]["edec"][:], op=ALU.mult),
                                   reads=[g["G"]["beta"].b, g["G"]["edec"].b], writes=[g["G"]["bedec"].b]),
                ]
                for st in gate_steps:
                    for g in gun:
                        st(g)

                def psb(u):
                    return u["ps"][:].bitcast(BF16)

                s1 = [
                    lambda u: S.op("dve", lambda e: e.tensor_scalar(out=u["w"]["Gb"][:], in0=CS[0:64, 4, 0:64], scalar1=u["G"]["g"][:, u["h"]:u["h"] + 1], scalar2=None, op0=ALU.mult),
                                   reads=[CS.b, u["G"]["g"].b], writes=[u["w"]["Gb"].b]),
                    lambda u: S.op("pe", lambda e: e.matmul(u["ps"][0:64, 0:64], U_, u["w"]["Gb"][:], start=True, stop=False), reads=[CS.b, u["w"]["Gb"].b], writes=[u["ps"].b]),
                    lambda u: S.op("pe", lambda e: e.matmul(u["ps"][0:64, 0:64], u["w"]["Gb"][:], nU_, start=False, stop=True), reads=[CS.b, u["w"]["Gb"].b], writes=[u["ps"].b]),
                    lambda u: S.op("pe", lambda e: e.matmul(u["ps"][0:64, 64:128], u["w"]["Gb"][:], U_, start=True, stop=False), reads=[CS.b, u["w"]["Gb"].b], writes=[u["ps"].b]),
                    lambda u: S.op("pe", lambda e: e.matmul(u["ps"][0:64, 64:128], nU_, u["w"]["Gb"][:], start=False, stop=True), reads=[CS.b, u["w"]["Gb"].b], writes=[u["ps"].b]),
                    lambda u: S.op("pe", lambda e: e.transpose(psb(u)[0:64, 256:384], kn[u["h"]][:, u["cs"]], identb), reads=[kn[u["h"]].b, CSb.b], writes=[u["ps"].b]),
                    lambda u: S.op("pe", lambda e: e.transpose(psb(u)[0:64, 384:512], vT[u["h"]][:, u["cs"]], identb), reads=[vT[u["h"]].b, CSb.b], writes=[u["ps"].b]),
                    lambda u: S.op("pe", lambda e: e.matmul(u["ps"][0:64, 256:320], kn[u["h"]][:, u["cs"]], kn[u["h"]][:, u["cs"]], start=True, stop=True), reads=[kn[u["h"]].b], writes=[u["ps"].b]),
                    lambda u: S.op("pe", lambda e: e.matmul(u["ps"][0:64, 320:384], kn[u["h"]][:, u["cs"]], qn[u["h"]][:, u["cs"]], start=True, stop=True), reads=[kn[u["h"]].b, qn[u["h"]].b], writes=[u["ps"].b]),
                    lambda u: S.op("dve", lambda e: e.tensor_tensor(out=u["w"]["gs"][:], in0=u["ps"][0:64, 0:64], in1=CS[0:64, 5, 0:64], op=ALU.add),
                                   reads=[u["ps"].b, CS.b], writes=[u["w"]["gs"].b]),
                    lambda u: S.op("dve", lambda e: e.tensor_tensor(out=u["w"]["gT"][:], in0=u["ps"][0:64, 64:128], in1=CS[0:64, 6, 0:64], op=ALU.add),
                                   reads=[u["ps"].b, CS.b], writes=[u["w"]["gT"].b]),
                    lambda u: S.op("act", lambda e: e.activation(out=u["w"]["gs"][:], in_=u["w"]["gs"][:], func=AF.Exp), reads=[u["w"]["gs"].b], writes=[u["w"]["gs"].b]),
                    lambda u: S.op("act", lambda e: e.activation(out=u["w"]["gT"][:], in_=u["w"]["gT"][:], func=AF.Exp), reads=[u["w"]["gT"].b], writes=[u["w"]["gT"].b]),
                    lambda u: S.op("dve", lambda e: e.tensor_scalar(out=u["w"]["kbe"][:], in0=psb(u)[0:64, 256:384], scalar1=u["G"]["bedec"][:, u["h"]:u["h"] + 1], scalar2=None, op0=ALU.mult),
                                   reads=[u["ps"].b, u["G"]["bedec"].b], writes=[u["w"]["kbe"].b]),
                    lambda u: S.op("dve", lambda e: e.tensor_scalar(out=u["w"]["ktail"][:], in0=psb(u)[0:64, 256:384], scalar1=u["G"]["edlm"][:, u["h"]:u["h"] + 1], scalar2=None, op0=ALU.mult),
                                   reads=[u["ps"].b, u["G"]["edlm"].b], writes=[u["w"]["ktail"].b]),
                    lambda u: S.op("dve", lambda e: e.tensor_scalar(out=u["w"]["bv"][:], in0=psb(u)[0:64, 384:512], scalar1=u["G"]["beta"][:, u["h"]:u["h"] + 1], scalar2=None, op0=ALU.mult),
                                   reads=[u["ps"].b, u["G"]["beta"].b], writes=[u["w"]["bv"].b]),
                    lambda u: S.op("dve", lambda e: e.scalar_tensor_tensor(out=u["w"]["N"][:], in0=u["ps"][0:64, 256:320], scalar=u["G"]["beta"][:, u["h"]:u["h"] + 1], in1=u["w"]["gs"][:],
                                                                         op0=ALU.mult, op1=ALU.mult),
                                   reads=[u["ps"].b, u["G"]["beta"].b, u["w"]["gs"].b], writes=[u["w"]["N"].b]),
                    lambda u: S.op("dve", lambda e: e.tensor_tensor(out=u["w"]["qkT"][:], in0=u["ps"][0:64, 320:384], in1=u["w"]["gT"][:], op=ALU.mult),
                                   reads=[u["ps"].b, u["w"]["gT"].b], writes=[u["w"]["qkT"].b]),
                    lambda u: S.op("dve", lambda e: e.tensor_scalar(out=u["w"]["Lm"][:], in0=CS[0:64, 4, :], scalar1=u["G"]["edec"][:, u["h"]:u["h"] + 1], scalar2=None, op0=ALU.mult),
                                   reads=[CS.b, u["G"]["edec"].b], writes=[u["w"]["Lm"].b]),
                    lambda u: S.op("pe", lambda e: e.matmul(u["ps"][:, 384:448], u["w"]["Lm"][:], ident[0:64, 0:64], start=True, stop=True), reads=[u["w"]["Lm"].b, CS.b], writes=[u["ps"].b]),
                    lambda u: S.op("pe", lambda e: e.transpose(u["ps"][0:64, 448:512], u["w"]["N"][:], ident[0:64, 0:64]), reads=[u["w"]["N"].b, CS.b], writes=[u["ps"].b]),
                    lambda u: S.op("dve", lambda e: e.tensor_tensor(out=u["w"]["qd"][:], in0=qn[u["h"]][:, u["cs"]], in1=u["ps"][:, 384:448], op=ALU.mult),
                                   reads=[qn[u["h"]].b, u["ps"].b], writes=[u["w"]["qd"].b]),
                    lambda u: S.op("act", lambda e: e.copy(u["w"]["P"][0][:], u["ps"][0:64, 448:512]), reads=[u["ps"].b], writes=[u["w"]["P"][0].b]),
                    lambda u: S.op("dve", lambda e: e.tensor_tensor(out=u["w"]["X"][:], in0=ident[0:64, 0:64], in1=u["ps"][0:64, 448:512], op=ALU.subtract),
                                   reads=[u["ps"].b, CS.b], writes=[u["w"]["X"].b]),
                ]
                for st in s1:
                    for u in units:
                        st(u)
                for lvl in range(1, 6):
                    ci_ = (lvl - 1) % 2
                    for u in units:
                        w = u["w"]
                        u["Pc"] = w["P"][ci_]
                        u["Qc"] = w["N"] if lvl == 1 else w["Q"][ci_]
                        u["Pn"] = w["P"][1 - ci_]
                        u["Qn"] = w["Q"][1 - ci_]
                    s2 = []
                    if lvl < 5:
                        s2.append(lambda u: S.op("pe", lambda e: e.matmul(u["ps"][0:64, 0:64], u["Qc"][:], u["Pc"][:], start=True, stop=True), reads=[u["Pc"].b, u["Qc"].b], writes=[u["ps"].b]))
                    s2.append(lambda u: S.op("pe", lambda e: e.matmul(u["ps"][0:64, 64:128], u["Pc"][:], u["Qc"][:], start=True, stop=True), reads=[u["Pc"].b, u["Qc"].b], writes=[u["ps"].b]))
                    if lvl < 5:
                        s2.append(lambda u: S.op("act", lambda e: e.copy(u["Pn"][:], u["ps"][0:64, 0:64]), reads=[u["ps"].b], writes=[u["Pn"].b]))
                    s2.append(lambda u: S.op("dve", lambda e: e.tensor_copy(u["Qn"][:], u["ps"][0:64, 64:128]), reads=[u["ps"].b], writes=[u["Qn"].b]))
                    s2.append(lambda u: S.op("pe", lambda e: e.matmul(u["ps"][0:64, 128:192], u["Qn"][:], u["w"]["X"][:], start=True, stop=True), reads=[u["Qn"].b, u["w"]["X"].b], writes=[u["ps"].b]))
                    s2.append(lambda u: S.op("dve", lambda e: e.tensor_tensor(out=u["w"]["X"][:], in0=u["w"]["X"][:], in1=u["ps"][0:64, 128:192], op=ALU.add),
                                             reads=[u["ps"].b, u["w"]["X"].b], writes=[u["w"]["X"].b]))
                    for st in s2:
                        for u in units:
                            st(u)
                s3a = [
                    lambda u: S.op("act", lambda e: e.copy(u["w"]["Xb"][:], u["w"]["X"][:]), reads=[u["w"]["X"].b], writes=[u["w"]["Xb"].b]),
                    lambda u: S.op("pe", lambda e: e.matmul(u["ps"][:, 0:64], u["w"]["kbe"][:], u["w"]["Xb"][:], start=True, stop=True), reads=[u["w"]["kbe"].b, u["w"]["Xb"].b], writes=[u["ps"].b]),
                    lambda u: S.op("act", lambda e: e.mul(u["w"]["nwT"][:], u["ps"][:, 0:64], -1.0), reads=[u["ps"].b], writes=[u["w"]["nwT"].b]),
                ]
                for st in s3a:
                    for u in units:
                        st(u)
                s3b = [
                    lambda u: S.op("pe", lambda e: e.matmul(u["ps"][0:64, 128:256], u["w"]["Xb"][:], u["w"]["bv"][:], start=True, stop=False), reads=[u["w"]["Xb"].b, u["w"]["bv"].b], writes=[u["ps"].b]),
                    lambda u: S.op("pe", lambda e: e.matmul(u["ps"][0:64, 128:256], u["w"]["nwT"][:], Sb[u["h"]][:], start=False, stop=True), reads=[u["w"]["nwT"].b, Sb[u["h"]].b], writes=[u["ps"].b]),
                    lambda u: S.op("act", lambda e: e.copy(u["w"]["vn"][:], u["ps"][0:64, 128:256]), reads=[u["ps"].b], writes=[u["w"]["vn"].b]),
                    lambda u: S.op("pe", lambda e: e.matmul(u["ps"][0:64, 256:384], u["w"]["qd"][:], Sb[u["h"]][:], start=True, stop=False), reads=[u["w"]["qd"].b, Sb[u["h"]].b], writes=[u["ps"].b]),
                    lambda u: S.op("pe", lambda e: e.matmul(u["ps"][0:64, 256:384], u["w"]["qkT"][:], u["w"]["vn"][:], start=False, stop=True), reads=[u["w"]["qkT"].b, u["w"]["vn"].b], writes=[u["ps"].b]),
                    lambda u: S.op("pe", lambda e: e.matmul(u["ps"][:, 384:512], u["w"]["ktail"][:], u["w"]["vn"][:], start=True, stop=True), reads=[u["w"]["ktail"].b, u["w"]["vn"].b], writes=[u["ps"].b]),
                    lambda u: S.op("dve", lambda e: e.scalar_tensor_tensor(out=Sst[u["h"]][:], in0=Sst[u["h"]][:], scalar=u["G"]["edl"][:, u["h"]:u["h"] + 1], in1=u["ps"][:, 384:512],
                                                                         op0=ALU.mult, op1=ALU.add),
                                   reads=[Sst[u["h"]].b, u["G"]["edl"].b, u["ps"].b], writes=[Sst[u["h"]].b]),
                    lambda u: S.op("act", lambda e: e.copy(Sb[u["h"]][:], Sst[u["h"]][:]), reads=[Sst[u["h"]].b], writes=[Sb[u["h"]].b]),
                ]
                s3c = [
                    lambda u: S.op("act", lambda e: e.activation(out=u["w"]["junk"][:], in_=u["ps"][0:64, 256:384], func=AF.Square, accum_out=u["w"]["ssq"][:]),
                                   reads=[u["ps"].b], writes=[u["w"]["junk"].b, u["w"]["ssq"].b]),
                    lambda u: S.op("act", lambda e: e.activation(out=u["w"]["rt"][:], in_=u["w"]["ssq"][:], func=AF.Sqrt, bias=RMS_EPS, scale=1.0 / 128.0),
                                   reads=[u["w"]["ssq"].b], writes=[u["w"]["rt"].b]),
                    lambda u: S.op("dve", lambda e: e.reciprocal(u["w"]["rstd"][:], u["w"]["rt"][:]), reads=[u["w"]["rt"].b], writes=[u["w"]["rstd"].b]),
                    lambda u: S.op("dve", lambda e: e.tensor_scalar(out=u["w"]["Lm"][:], in0=u["ps"][0:64, 256:384], scalar1=u["w"]["rstd"][:, 0:1], scalar2=None, op0=ALU.mult),
                                   reads=[u["ps"].b, u["w"]["rstd"].b], writes=[u["w"]["Lm"].b]),
                    lambda u: S.op("pe", lambda e: e.transpose(u["ps"][:, 0:64], u["w"]["Lm"][:], ident[0:64, 0:64]), reads=[u["w"]["Lm"].b, CS.b], writes=[u["ps"].b]),
                    lambda u: S.op("dve", lambda e: e.scalar_tensor_tensor(out=yo[u["h"]][:, u["cs"]], in0=u["ps"][:, 0:64], scalar=HP[:, 8:9], in1=zs[u["h"]][:, u["cs"]],
                                                                         op0=ALU.mult, op1=ALU.mult),
                                   reads=[u["ps"].b, HP.b, zs[u["h"]].b], writes=[yo[u["h"]].b]),
                ]
                for ci in range(NCG):
                    us = units[ci * HPC:(ci + 1) * HPC]
                    for st in s3b:
                        for u in us:
                            st(u)
                for st in s3c:
                    for u in units:
                        st(u)

            for h in range(HPC):
                S.dma("sp", yT[128 * h:128 * h + 128, sb * 512:sb * 512 + 512], yo[h][:], reads=[yo[h].b])
        S.barrier()


def x_to_xT(x2d):
    L = x2d.shape[0]
    TR = min(L, 1024)
    return np.ascontiguousarray(x2d.reshape(L // TR, TR, D_MODEL).transpose(0, 2, 1))


def prep_gdn(c, layer, inp):
    hs = [HPC * c + i for i in range(HPC)]
    w_in = inp["w_in"][layer]
    cols = []
    for blk in range(4):
        for h in hs:
            cols.append(np.arange(blk * GDN_WIDTH + h * 128, blk * GDN_WIDTH + (h + 1) * 128))
    cols.append(np.array([4 * GDN_WIDTH + h for h in hs]))
    cols.append(np.array([4 * GDN_WIDTH + GDN_HEADS + h for h in hs]))
    cols = np.concatenate(cols)
    wg = np.ascontiguousarray(w_in[:, cols])
    cw = inp["gdn_conv_w"][layer]
    convw = np.zeros((128, 36), np.float32)
    for blk in range(3):
        for i, h in enumerate(hs):
            ct = blk * 3 + i
            ch = blk * GDN_WIDTH + h * 128 + np.arange(128)
            convw[:, 4 * ct:4 * ct + 4] = cw[:, ch].T
    hp = np.zeros((128, 16), np.float32)
    hp[:, 0:3] = inp["gdn_a_log"][layer][hs][None, :]
    hp[:, 3:6] = inp["gdn_dt_bias"][layer][hs][None, :]
    hp[:, 8] = inp["gdn_norm_w"][layer]
    return {"wg": wg, "convw": convw, "hp": hp, "cst": gdn_consts()}


S5C = 256


def s5_consts():
    c = np.zeros((128, 5, 256), np.float32)
    c[:, 0, :128] = np.eye(128)
    k = np.arange(128)
    sw = np.zeros((128, 128), np.float32)
    sw[k, (k + 64) % 128] = 1.0
    c[:, 1, :128] = sw
    c[:, 2, :] = np.arange(256)[None, :]
    g = np.arange(128) // 16
    c[:, 3, :8] = (g[:, None] == np.arange(8)[None, :])
    c[:64, 3, 8] = 1.0
    c[64:, 3, 8] = -1.0
    c[:, 3, 9] = -1.0
    return c.reshape(128, 5 * 256)


def prep_s5(c, layer, inp):
    gs = np.arange(GPC * c, GPC * c + GPC)
    w_in = inp["w_in"][layer]
    c_u = 4 * GDN_WIDTH + 2 * GDN_HEADS
    wu = np.ascontiguousarray(w_in[:, c_u + 128 * c:c_u + 128 * c + 128])
    lre = inp["s5_lambda_re"][layer][gs]
    lim = inp["s5_lambda_im"][layer][gs]
    ldt = inp["s5_log_dt"][layer][gs]
    bre = inp["s5_b_re"][layer][gs]
    bim = inp["s5_b_im"][layer][gs]
    cre = inp["s5_c_re"][layer][gs]
    cim = inp["s5_c_im"][layer][gs]
    pr = np.zeros((128, 8, 64), np.float32)
    pr[:, 0, :] = np.repeat(lre, 16, axis=0)
    pr[:, 1, :] = np.repeat(lim, 16, axis=0)
    pr[:, 2, :] = bre.transpose(0, 2, 1).reshape(128, 64)
    pr[:, 3, :] = bim.transpose(0, 2, 1).reshape(128, 64)
    pr[:, 4, 0] = np.repeat(ldt, 16)
    pr[:, 4, 1] = inp["s5_d"][layer][128 * c:128 * c + 128]
    pc = np.zeros((128, 4, 128), np.float32)
    cTre = cre.transpose(2, 0, 1).reshape(64, 128)
    cTim = cim.transpose(2, 0, 1).reshape(64, 128)
    pc[:64, 0, :] = cTre
    pc[64:, 0, :] = cTim
    pc[:64, 1, :] = cTim
    pc[64:, 1, :] = cTre
    pc[:, 2, 0:8] = np.tile(lre.T, (2, 1))
    pc[:, 2, 8:16] = np.tile(lim.T, (2, 1))
    pc[:, 2, 16:24] = ldt[None, :]
    return {"wu": wu, "pr": pr.reshape(128, 512), "pc": pc.reshape(128, 512), "cst": s5_consts()}


def emit_s5(nc, S, L, x_bf16, xT, wu, pr_d, pc_d, cst, yT):
    NR = max(1, L // 1024)
    TR = min(L, 1024)
    NSB = L // 512
    xq = "sp" if x_bf16 else "pool"

    with ExitStack() as es:
        C = Ctx(nc, es, S)
        W = C.sb([128, KC, 128], BF16, "W")
        XT = [C.sb([128, 8, 512], BF16, "XT") for _ in range(8)]
        PR = C.sb([128, 8, 64], F32, "PR")
        PC = C.sb([128, 4, 128], F32, "PC")
        CS = C.sb([128, 5, 256], F32, "CS")
        PS = C.psum_banks(8)
        ident = CS[:, 0, 0:128]
        swap = CS[:, 1, 0:128]
        iota = CS[:, 2, :]

        wv = wu.rearrange("(kc p) n -> p kc n", p=128)
        S.dma("pool", W[:], wv, writes=[W.b])
        S.dma("sp", PR[:].rearrange("p a b -> p (a b)"), pr_d, writes=[PR.b])
        S.dma("sp", PC[:].rearrange("p a b -> p (a b)"), pc_d, writes=[PC.b])
        S.dma("sp", CS[:].rearrange("p a b -> p (a b)"), cst, writes=[CS.b])

        n_tmp = [0]

        def tmp(shape, dt=F32):
            n_tmp[0] += 1
            return C.sb(shape, dt, "tmp")

        def dve(fn, reads, writes):
            return S.op("dve", fn, reads=[t.b for t in reads], writes=[t.b for t in writes])

        def act(fn, reads, writes):
            return S.op("act", fn, reads=[t.b for t in reads], writes=[t.b for t in writes])

        sin_tmp = {}

        def sin_of(dst, ang, shift):
            key = tuple(dst.shape_)
            if key not in sin_tmp:
                sin_tmp[key] = (tmp(list(key)), tmp(list(key)))
            k, r = sin_tmp[key]
            dve(lambda e: e.tensor_scalar(out=k[:], in0=ang[:], scalar1=shift, scalar2=1.0 / TWO_PI, op0=ALU.add, op1=ALU.mult), [ang], [k])
            dve(lambda e: e.tensor_scalar(out=k[:], in0=k[:], scalar1=MAGIC, scalar2=MAGIC, op0=ALU.add, op1=ALU.subtract), [k], [k])
            dve(lambda e: e.scalar_tensor_tensor(out=r[:], in0=k[:], scalar=-TWO_PI, in1=ang[:], op0=ALU.mult, op1=ALU.add), [k, ang], [r])
            dve(lambda e: e.tensor_scalar(out=r[:], in0=r[:], scalar1=shift, scalar2=3.1415925, op0=ALU.add, op1=ALU.min), [r], [r])
            dve(lambda e: e.tensor_scalar(out=r[:], in0=r[:], scalar1=-3.1415925, scalar2=None, op0=ALU.max), [r], [r])
            act(lambda e: e.activation(out=dst[:], in_=r[:], func=AF.Sin), [r], [dst])

        def dst_shape(t):
            return t.shape_

        def mk(shape, dt=F32):
            t = tmp(shape, dt)
            t.shape_ = shape
            return t

        dtc = mk([128, 1])
        act(lambda e: e.activation(out=dtc[:], in_=PR[:, 4, 0:1], func=AF.Exp), [PR], [dtc])
        lrd = mk([128, 64]); lid = mk([128, 64]); mag = mk([128, 64]); sn = mk([128, 64]); cs_ = mk([128, 64])
        dve(lambda e: e.tensor_scalar(out=lrd[:], in0=PR[:, 0, :], scalar1=dtc[:, 0:1], scalar2=None, op0=ALU.mult), [PR, dtc], [lrd])
        dve(lambda e: e.tensor_scalar(out=lid[:], in0=PR[:, 1, :], scalar1=dtc[:, 0:1], scalar2=None, op0=ALU.mult), [PR, dtc], [lid])
        act(lambda e: e.activation(out=mag[:], in_=lrd[:], func=AF.Exp), [lrd], [mag])
        sin_of(sn, lid, 0.0)
        sin_of(cs_, lid, float(np.pi / 2))
        nr = mk([128, 64]); ni = mk([128, 64]); den = mk([128, 64]); t1 = mk([128, 64]); t2 = mk([128, 64])
        cre = mk([128, 64]); cim = mk([128, 64])
        dve(lambda e: e.tensor_tensor(out=nr[:], in0=mag[:], in1=cs_[:], op=ALU.mult), [mag, cs_], [nr])
        dve(lambda e: e.tensor_scalar(out=nr[:], in0=nr[:], scalar1=-1.0, scalar2=None, op0=ALU.add), [nr], [nr])
        dve(lambda e: e.tensor_tensor(out=ni[:], in0=mag[:], in1=sn[:], op=ALU.mult), [mag, sn], [ni])
        dve(lambda e: e.tensor_tensor(out=den[:], in0=PR[:, 0, :], in1=PR[:, 0, :], op=ALU.mult), [PR], [den])
        dve(lambda e: e.tensor_tensor(out=t1[:], in0=PR[:, 1, :], in1=PR[:, 1, :], op=ALU.mult), [PR], [t1])
        dve(lambda e: e.tensor_tensor(out=den[:], in0=den[:], in1=t1[:], op=ALU.add), [den, t1], [den])
        dve(lambda e: e.reciprocal(den[:], den[:]), [den], [den])
        dve(lambda e: e.tensor_tensor(out=t1[:], in0=nr[:], in1=PR[:, 0, :], op=ALU.mult), [nr, PR], [t1])
        dve(lambda e: e.tensor_tensor(out=t2[:], in0=ni[:], in1=PR[:, 1, :], op=ALU.mult), [ni, PR], [t2])
        dve(lambda e: e.tensor_tensor(out=t1[:], in0=t1[:], in1=t2[:], op=ALU.add), [t1, t2], [t1])
        dve(lambda e: e.tensor_tensor(out=cre[:], in0=t1[:], in1=den[:], op=ALU.mult), [t1, den], [cre])
        dve(lambda e: e.tensor_tensor(out=t1[:], in0=ni[:], in1=PR[:, 0, :], op=ALU.mult), [ni, PR], [t1])
        dve(lambda e: e.tensor_tensor(out=t2[:], in0=nr[:], in1=PR[:, 1, :], op=ALU.mult), [nr, PR], [t2])
        dve(lambda e: e.tensor_tensor(out=t1[:], in0=t1[:], in1=t2[:], op=ALU.subtract), [t1, t2], [t1])
        dve(lambda e: e.tensor_tensor(out=cim[:], in0=t1[:], in1=den[:], op=ALU.mult), [t1, den], [cim])
        BB1 = mk([128, 128]); BB2 = mk([128, 128])
        dve(lambda e: e.tensor_tensor(out=t1[:], in0=cre[:], in1=PR[:, 2, :], op=ALU.mult), [cre, PR], [t1])
        dve(lambda e: e.tensor_tensor(out=t2[:], in0=cim[:], in1=PR[:, 3, :], op=ALU.mult), [cim, PR], [t2])
        dve(lambda e: e.tensor_tensor(out=BB1[:, 0:64], in0=t1[:], in1=t2[:], op=ALU.subtract), [t1, t2], [BB1])
        dve(lambda e: e.tensor_tensor(out=t1[:], in0=cre[:], in1=PR[:, 3, :], op=ALU.mult), [cre, PR], [t1])
        dve(lambda e: e.tensor_tensor(out=t2[:], in0=cim[:], in1=PR[:, 2, :], op=ALU.mult), [cim, PR], [t2])
        dve(lambda e: e.tensor_tensor(out=BB1[:, 64:128], in0=t1[:], in1=t2[:], op=ALU.add), [t1, t2], [BB1])
        dve(lambda e: e.tensor_copy(BB2[:, 0:64], BB1[:, 64:128]), [BB1], [BB2])
        dve(lambda e: e.tensor_scalar(out=BB2[:, 64:128], in0=BB1[:, 0:64], scalar1=-1.0, scalar2=None, op0=ALU.mult), [BB1], [BB2])
        Bm1 = mk([128, GPC, 128], BF16); Bm2 = mk([128, GPC, 128], BF16)
        for g in range(GPC):
            dve(lambda e, g=g: e.tensor_scalar(out=Bm1[:, g, :], in0=BB1[:], scalar1=CS[:, 3, g:g + 1], scalar2=None, op0=ALU.mult), [BB1, CS], [Bm1])
            dve(lambda e, g=g: e.tensor_scalar(out=Bm2[:, g, :], in0=BB2[:], scalar1=CS[:, 3, g:g + 1], scalar2=None, op0=ALU.mult), [BB2, CS], [Bm2])
        W1 = mk([128, GPC, 128], BF16); W2 = mk([128, GPC, 128], BF16)
        dve(lambda e: e.memset(W1[:].rearrange("p a b -> p (a b)"), 0.0), [], [W1])
        dve(lambda e: e.memset(W2[:].rearrange("p a b -> p (a b)"), 0.0), [], [W2])
        for g in range(GPC):
            sl = slice(16 * g, 16 * g + 16)
            dve(lambda e, g=g, sl=sl: e.tensor_scalar(out=W1[:, g, sl], in0=PC[:, 0, sl], scalar1=CS[:, 3, 8:9], scalar2=None, op0=ALU.mult), [PC, CS], [W1])
            dve(lambda e, g=g, sl=sl: e.tensor_scalar(out=W2[:, g, sl], in0=PC[:, 1, sl], scalar1=CS[:, 3, 9:10], scalar2=None, op0=ALU.mult), [PC, CS], [W2])
        dt2 = mk([128, 8]); th = mk([128, 8]); rho = mk([128, 8]); lr2 = mk([128, 8])
        act(lambda e: e.activation(out=dt2[:], in_=PC[:, 2, 16:24], func=AF.Exp), [PC], [dt2])
        dve(lambda e: e.tensor_tensor(out=th[:], in0=PC[:, 2, 8:16], in1=dt2[:], op=ALU.mult), [PC, dt2], [th])
        dve(lambda e: e.tensor_tensor(out=lr2[:], in0=PC[:, 2, 0:8], in1=dt2[:], op=ALU.mult), [PC, dt2], [lr2])
        act(lambda e: e.activation(out=rho[:], in_=lr2[:], func=AF.Exp), [lr2], [rho])
        C2 = mk([128, GPC, S5C]); S2 = mk([128, GPC, S5C])
        ang = mk([128, S5C]); sg = mk([128, S5C]); cg = mk([128, S5C])
        for g in range(GPC):
            dve(lambda e, g=g: e.tensor_scalar(out=ang[:], in0=iota, scalar1=th[:, g:g + 1], scalar2=None, op0=ALU.mult), [CS, th], [ang])
            sin_of(sg, ang, 0.0)
            sin_of(cg, ang, float(np.pi / 2))
            dve(lambda e, g=g, sg=sg: e.tensor_copy(S2[:, g, :], sg[:]), [sg], [S2])
            dve(lambda e, g=g, cg=cg: e.tensor_copy(C2[:, g, :], cg[:]), [cg], [C2])
        angc = mk([128, 8]); crr = mk([128, 8]); srr = mk([128, 8])
        dve(lambda e: e.tensor_scalar(out=angc[:], in0=th[:], scalar1=float(S5C), scalar2=None, op0=ALU.mult), [th], [angc])
        sin_of(srr, angc, 0.0)
        sin_of(crr, angc, float(np.pi / 2))
        dve(lambda e: e.tensor_scalar(out=srr[:], in0=srr[:], scalar1=CS[:, 3, 8:9], scalar2=None, op0=ALU.mult), [srr, CS], [srr])
        ROT = mk([128, GPC, 128])
        for g in range(GPC):
            dve(lambda e, g=g: e.tensor_scalar(out=ROT[:, g, :], in0=ident, scalar1=crr[:, g:g + 1], scalar2=None, op0=ALU.mult), [CS, crr], [ROT])
            dve(lambda e, g=g: e.scalar_tensor_tensor(out=ROT[:, g, :], in0=swap, scalar=srr[:, g:g + 1], in1=ROT[:, g, :], op0=ALU.mult, op1=ALU.add),
                [CS, srr, ROT], [ROT])

        uT = [mk([128, 512]) for _ in range(2)]
        uTb = [mk([128, 512], BF16) for _ in range(2)]
        mbuf = [mk([128, S5C]) for _ in range(2)]
        tbuf = [mk([128, S5C]) for _ in range(2)]
        zeta = [mk([128, S5C]) for _ in range(2)]
        Zc = [mk([128, S5C], BF16) for _ in range(2)]
        Zs = [mk([128, S5C], BF16) for _ in range(2)]
        zl = [mk([128, 1]) for _ in range(GPC)]
        zi = [mk([128, 1]) for _ in range(GPC)]
        yf = [mk([128, S5C]) for _ in range(2)]
        yo = [mk([128, S5C], BF16) for _ in range(2)]
        gl = [(mk([128, S5C]), mk([128, S5C])) for _ in range(2)]
        proj_banks = BankPool(PS[0:2])
        p_banks = BankPool(PS[2:6])
        y_banks = BankPool(PS[6:8])
        xv = xT.rearrange("r (kc p) t -> r p kc t", p=128)
        rot = 0
        nchunk = 0
        for sb in range(NSB):
            r = (sb * 512) // TR
            t0 = (sb * 512) % TR
            xs = XT[4 * (sb % 2):4 * (sb % 2) + 4]
            for j in range(4):
                S.dma(xq, xs[j][:], xv[r, :, 8 * j:8 * j + 8, t0:t0 + 512], writes=[xs[j].b])
            ps = proj_banks.get()
            for kc in range(KC):
                S.op("pe", lambda e, kc=kc: e.matmul(ps[:, :], W[:, kc, :], xs[kc // 8][:, kc % 8, :], start=(kc == 0), stop=(kc == KC - 1)),
                     reads=[W.b, xs[kc // 8].b], writes=[ps.b])
            u_, ub_ = uT[sb % 2], uTb[sb % 2]
            act(lambda e: e.copy(u_[:], ps[:, :]), [ps], [u_])
            dve(lambda e: e.tensor_copy(ub_[:], ps[:, :]), [ps], [ub_])
            for cc in range(512 // S5C):
                csl = slice(cc * S5C, (cc + 1) * S5C)
                yps = y_banks.get()
                for g in range(GPC):
                    pb = p_banks.get()
                    m_, t_, z_, zc_, zs_ = mbuf[rot], tbuf[rot], zeta[rot], Zc[rot], Zs[rot]
                    rot ^= 1
                    S.op("pe", lambda e, g=g: e.matmul(pb[:, 0:S5C], Bm1[:, g, :], ub_[:, csl], start=True, stop=True), reads=[Bm1.b, ub_.b], writes=[pb.b])
                    S.op("pe", lambda e, g=g: e.matmul(pb[:, S5C:2 * S5C], Bm2[:, g, :], ub_[:, csl], start=True, stop=True), reads=[Bm2.b, ub_.b], writes=[pb.b])
                    dve(lambda e, g=g: e.tensor_tensor(out=m_[:], in0=pb[:, 0:S5C], in1=C2[:, g, :], op=ALU.mult), [pb, C2], [m_])
                    dve(lambda e, g=g: e.tensor_tensor(out=t_[:], in0=pb[:, S5C:2 * S5C], in1=S2[:, g, :], op=ALU.mult), [pb, S2], [t_])
                    dve(lambda e: e.tensor_tensor(out=m_[:], in0=m_[:], in1=t_[:], op=ALU.add), [m_, t_], [m_])
                    if nchunk == 0:
                        dve(lambda e, g=g: e.tensor_tensor_scan(out=z_[:], data0=rho[:, g:g + 1].to_broadcast([128, S5C]), data1=m_[:], initial=0.0,
                                                              op0=ALU.mult, op1=ALU.add), [rho, m_], [z_])
                    else:
                        pr_ = p_banks.get()
                        S.op("pe", lambda e, g=g: e.matmul(pr_[:, 0:1], ROT[:, g, :], zl[g][:], start=True, stop=True), reads=[ROT.b, zl[g].b], writes=[pr_.b])
                        act(lambda e, g=g: e.copy(zi[g][:], pr_[:, 0:1]), [pr_], [zi[g]])
                        dve(lambda e, g=g: e.tensor_tensor_scan(out=z_[:], data0=rho[:, g:g + 1].to_broadcast([128, S5C]), data1=m_[:], initial=zi[g][:, 0:1],
                                                              op0=ALU.mult, op1=ALU.add), [rho, m_, zi[g]], [z_])
                    dve(lambda e, g=g: e.tensor_copy(zl[g][:], z_[:, S5C - 1:S5C]), [z_], [zl[g]])
                    dve(lambda e, g=g: e.tensor_tensor(out=zc_[:], in0=z_[:], in1=C2[:, g, :], op=ALU.mult), [z_, C2], [zc_])
                    dve(lambda e, g=g: e.tensor_tensor(out=zs_[:], in0=z_[:], in1=S2[:, g, :], op=ALU.mult), [z_, S2], [zs_])
                    S.op("pe", lambda e, g=g: e.matmul(yps[:, 0:S5C], W1[:, g, :], zc_[:], start=(g == 0), stop=False), reads=[W1.b, zc_.b], writes=[yps.b])
                    S.op("pe", lambda e, g=g: e.matmul(yps[:, 0:S5C], W2[:, g, :], zs_[:], start=False, stop=(g == GPC - 1)), reads=[W2.b, zs_.b], writes=[yps.b])
                yf_, yo_ = yf[nchunk % 2], yo[nchunk % 2]
                dve(lambda e: e.scalar_tensor_tensor(out=yf_[:], in0=u_[:, csl], scalar=PR[:, 4, 1:2], in1=yps[:, 0:S5C], op0=ALU.mult, op1=ALU.add),
                    [u_, PR, yps], [yf_])
                g1, g2 = gl[nchunk % 2]
                dve(lambda e: e.tensor_tensor(out=g1[:], in0=yf_[:], in1=yf_[:], op=ALU.mult), [yf_], [g1])
                dve(lambda e: e.tensor_scalar(out=g1[:], in0=g1[:], scalar1=0.044715, scalar2=1.0, op0=ALU.mult, op1=ALU.add), [g1], [g1])
                dve(lambda e: e.tensor_tensor(out=g1[:], in0=g1[:], in1=yf_[:], op=ALU.mult), [g1, yf_], [g1])
                act(lambda e: e.activation(out=g2[:], in_=g1[:], func=AF.Sigmoid, scale=float(2.0 * np.sqrt(2.0 / np.pi))), [g1], [g2])
                dve(lambda e: e.tensor_tensor(out=yo_[:], in0=yf_[:], in1=g2[:], op=ALU.mult), [yf_, g2], [yo_])
                S.dma("sp", yT[:, sb * 512 + cc * S5C: sb * 512 + (cc + 1) * S5C], yo_[:], reads=[yo_.b])
                nchunk += 1
        S.barrier()


def build_mixer(L, x_bf16, do_gdn=True, do_s5=True):
    NR = max(1, L // 1024)
    TR = min(L, 1024)
    nc = bass.Bass("TRN2", target_bir_lowering=False)
    xT = nc.dram_tensor("xT", [NR, D_MODEL, TR], BF16 if x_bf16 else F32, kind="ExternalInput").ap()
    wg = nc.dram_tensor("wg", [D_MODEL, GDN_COLS], F32, kind="ExternalInput").ap()
    convw = nc.dram_tensor("convw", [128, 36], F32, kind="ExternalInput").ap()
    hp = nc.dram_tensor("hp", [128, 16], F32, kind="ExternalInput").ap()
    cstg = nc.dram_tensor("cstg", [128, 8 * 128], F32, kind="ExternalInput").ap()
    wu = nc.dram_tensor("wu", [D_MODEL, 128], F32, kind="ExternalInput").ap()
    pr_d = nc.dram_tensor("pr", [128, 512], F32, kind="ExternalInput").ap()
    pc_d = nc.dram_tensor("pc", [128, 512], F32, kind="ExternalInput").ap()
    csts = nc.dram_tensor("csts", [128, 5 * 256], F32, kind="ExternalInput").ap()
    yT = nc.dram_tensor("yT", [512, L], BF16, kind="ExternalOutput").ap()
    with ExitStack() as outer:
        S = Sched(nc, outer)
        if do_gdn:
            emit_gdn(nc, S, L, x_bf16, xT, wg, convw, hp, cstg, yT[0:384, :])
        if do_s5:
            emit_s5(nc, S, L, x_bf16, xT, wu, pr_d, pc_d, csts, yT[384:512, :])
        S.barrier()
    return nc


def prep_mixer(c, layer, inp):
    g = prep_gdn(c, layer, inp)
    s_ = prep_s5(c, layer, inp)
    return {"wg": g["wg"], "convw": g["convw"], "hp": g["hp"], "cstg": g["cst"],
            "wu": s_["wu"], "pr": s_["pr"], "pc": s_["pc"], "csts": s_["cst"]}


TPC = 1024
NT = TPC // 128
BIG = 1.0e30


def b_consts():
    c = np.zeros((128, 4, 128), np.float32)
    c[:, 0, :] = np.eye(128)
    c[:, 1, :] = 1.0
    k = np.arange(128)
    c[:, 2, :] = (k[:, None] < k[None, :])
    c[:, 3, :] = k[None, :]
    return c.reshape(128, 512)


def emit_consts(S, C, cst):
    CS = C.sb([128, 4, 128], F32, "CS")
    CSb = C.sb([128, 3, 128], BF16, "CSb")
    S.dma("sp", CS[:].rearrange("p a b -> p (a b)"), cst, writes=[CS.b])
    for j in range(3):
        S.op("dve", lambda e, j=j: e.tensor_copy(CSb[:, j, :], CS[:, j, :]), reads=[CS.b], writes=[CSb.b])
    return CS, CSb


def emit_proj_res(S, C, PS, lhs_fn, nk, rhs_view, rhs_f32, resid, resid_bufs, H, Hbufs):
    Wn = [C.sb([128, nk, 512], BF16, "Wn") for _ in range(2)]
    xt = [C.sb([128, 512], F32, "xt") for _ in range(3)]
    ht = [C.sb([128, 512], F32, "ht") for _ in range(3)]
    banks = BankPool(PS[0:4])
    q = "pool" if rhs_f32 else "sp"
    cnt = 0
    for n in range(8):
        w = Wn[n % 2]
        for j in range(4):
            ks = slice(j * nk // 4, (j + 1) * nk // 4)
            S.dma(q, w[:, ks, :], rhs_view[:, ks, n * 512:(n + 1) * 512], writes=[w.b])
        for i in range(NT):
            ps = banks.get()
            for k in range(nk):
                ap, b = lhs_fn(k, i)
                S.op("pe", lambda e, ap=ap, k=k: e.matmul(ps[:, :], ap, w[:, k, :], start=(k == 0), stop=(k == nk - 1)),
                     reads=[b, w.b], writes=[ps.b])
            x_, h_ = xt[cnt % 3], ht[cnt % 3]
            cnt += 1
            S.dma("sp", x_[:], resid[i * 128:(i + 1) * 128, n * 512:(n + 1) * 512], reads=[resid_bufs[i]], writes=[x_.b])
            S.op("dve", lambda e: e.scalar_tensor_tensor(out=h_[:], in0=x_[:], scalar=float(DN_ALPHA), in1=ps[:, :], op0=ALU.mult, op1=ALU.add),
                 reads=[x_.b, ps.b], writes=[h_.b])
            S.dma("sp", H[i * 128:(i + 1) * 128, n * 512:(n + 1) * 512], h_[:], reads=[h_.b], writes=[Hbufs[i]])


def emit_ln_tiles(S, C, H, Hbufs, lng, lnb, out_cb, eps=LN_EPS):
    G = C.sb([128, D_MODEL], F32, "lnG")
    Bt = C.sb([128, D_MODEL], F32, "lnB")
    S.dma("sp", G[:], lng, writes=[G.b])
    S.dma("sp", Bt[:], lnb, writes=[Bt.b])
    tiles = [C.sb([128, D_MODEL], F32, "lt") for _ in range(2)]
    junk = C.sb([128, D_MODEL], BF16, "junk")
    s1 = C.sb([128, 1], F32); nm = C.sb([128, 1], F32); s2 = C.sb([128, 1], F32); rt = C.sb([128, 1], F32); rstd = C.sb([128, 1], F32)
    for i in range(NT):
        t = tiles[i % 2]
        S.dma("sp", t[:], H[i * 128:(i + 1) * 128, :], reads=[Hbufs[i]], writes=[t.b])
        S.op("dve", lambda e: e.reduce_sum(out=s1[:], in_=t[:], axis=AX.X), reads=[t.b], writes=[s1.b])
        S.op("dve", lambda e: e.tensor_scalar(out=nm[:], in0=s1[:], scalar1=-1.0 / D_MODEL, scalar2=None, op0=ALU.mult), reads=[s1.b], writes=[nm.b])
        S.op("act", lambda e: e.activation(out=junk[:], in_=t[:], func=AF.Square, bias=nm[:, 0:1], accum_out=s2[:]), reads=[t.b, nm.b], writes=[junk.b, s2.b])
        S.op("act", lambda e: e.activation(out=rt[:], in_=s2[:], func=AF.Sqrt, bias=eps, scale=1.0 / D_MODEL), reads=[s2.b], writes=[rt.b])
        S.op("dve", lambda e: e.reciprocal(rstd[:], rt[:]), reads=[rt.b], writes=[rstd.b])
        S.op("dve", lambda e: e.tensor_scalar(out=t[:], in0=t[:], scalar1=nm[:, 0:1], scalar2=rstd[:, 0:1], op0=ALU.add, op1=ALU.mult),
             reads=[t.b, nm.b, rstd.b], writes=[t.b])
        S.op("dve", lambda e: e.tensor_tensor(out=t[:], in0=t[:], in1=G[:], op=ALU.mult), reads=[t.b, G.b], writes=[t.b])
        S.op("dve", lambda e: e.tensor_tensor(out=t[:], in0=t[:], in1=Bt[:], op=ALU.add), reads=[t.b, Bt.b], writes=[t.b])
        out_cb(i, t)


def emit_transpose_tile(S, banks, src, identb, CSb, dst_fn, eng_alt=[0]):
    for g in range(4):
        ps = banks.get()
        psb = ps[:].bitcast(BF16)
        for j in range(8):
            kc = g * 8 + j
            S.op("pe", lambda e, j=j, kc=kc: e.transpose(psb[:, j * 128:(j + 1) * 128], src[:, kc * 128:(kc + 1) * 128], identb),
                 reads=[src.b, CSb.b], writes=[ps.b])
        ap, b = dst_fn(g)
        en = "act" if (eng_alt[0] % 2 == 0) else "dve"
        eng_alt[0] += 1
        if en == "act":
            S.op("act", lambda e: e.copy(ap, psb[:, 0:1024].rearrange("p (a b) -> p a b", a=8)), reads=[ps.b], writes=[b])
        else:
            S.op("dve", lambda e: e.tensor_copy(ap, psb[:, 0:1024].rearrange("p (a b) -> p a b", a=8)), reads=[ps.b], writes=[b])


def build_t0():
    nc = bass.Bass("TRN2", target_bir_lowering=False)
    x = nc.dram_tensor("x", [TPC, D_MODEL], F32, kind="ExternalInput").ap()
    cst = nc.dram_tensor("cst", [128, 512], F32, kind="ExternalInput").ap()
    xT = nc.dram_tensor("xTo", [D_MODEL, TPC], BF16, kind="ExternalOutput").ap()
    with ExitStack() as outer:
        S = Sched(nc, outer)
        C = Ctx(nc, outer, S)
        PS = C.psum_banks(8)
        CS, CSb = emit_consts(S, C, cst)
        xTs = C.sb([128, KC, TPC], BF16, "xTs")
        xb = [C.sb([128, D_MODEL], BF16, "xb") for _ in range(2)]
        banks = BankPool(PS)
        for i in range(NT):
            S.dma("pool", xb[i % 2][:], x[i * 128:(i + 1) * 128, :], writes=[xb[i % 2].b])
            emit_transpose_tile(S, banks, xb[i % 2], CSb[:, 0, :], CSb,
                                lambda g, i=i: (xTs[:, g * 8:(g + 1) * 8, i * 128:(i + 1) * 128], xTs.b))
        S.dma("sp", xT.rearrange("(kc p) t -> p kc t", p=128), xTs[:], reads=[xTs.b])
        S.barrier()
    return nc


def build_b1():
    nc = bass.Bass("TRN2", target_bir_lowering=False)
    ymix = nc.dram_tensor("ymix", [D_MODEL, TPC], BF16, kind="ExternalInput").ap()
    wo = nc.dram_tensor("wo", [D_MODEL, D_MODEL], F32, kind="ExternalInput").ap()
    wglu = nc.dram_tensor("wglu", [S5_WIDTH, S5_WIDTH], F32, kind="ExternalInput").ap()
    x = nc.dram_tensor("x", [TPC, D_MODEL], F32, kind="ExternalInput").ap()
    lng = nc.dram_tensor("lng", [128, D_MODEL], F32, kind="ExternalInput").ap()
    lnb = nc.dram_tensor("lnb", [128, D_MODEL], F32, kind="ExternalInput").ap()
    wr = nc.dram_tensor("wr", [D_MODEL, 36], F32, kind="ExternalInput").ap()
    rb = nc.dram_tensor("rb", [128, 36], F32, kind="ExternalInput").ap()
    cst = nc.dram_tensor("cst", [128, 512], F32, kind="ExternalInput").ap()
    x1o = nc.dram_tensor("x1", [TPC, D_MODEL], F32, kind="ExternalOutput").ap()
    xg = nc.dram_tensor("xg", [N_EXPERTS, 128, KC, CAP], BF16, kind="ExternalOutput").ap()
    selw = nc.dram_tensor("selw", [128, N_EXPERTS, TPC], BF16, kind="ExternalOutput").ap()
    H = nc.dram_tensor("Hscr", [TPC, D_MODEL], F32, kind="Internal").ap()
    with ExitStack() as outer:
        S = Sched(nc, outer)
        Hb = [Buf(f"H{i}") for i in range(NT)]
        xb_ = [Buf(f"xr{i}") for i in range(NT)]
        with ExitStack() as es:
            C = Ctx(nc, es, S)
            PS = C.psum_banks(8)
            Y = C.sb([128, KC, TPC], BF16, "Y")
            y2 = C.sb([128, 8, TPC], BF16, "y2")
            Wg = C.sb([128, 8, S5_WIDTH], BF16, "Wglu")
            sig = [C.sb([128, 512], F32, "sig") for _ in range(2)]
            yv = ymix.rearrange("(k p) t -> p k t", p=128)
            for j in range(4):
                S.dma("sp", Y[:, 8 * j:8 * j + 8, :], yv[:, 8 * j:8 * j + 8, :], writes=[Y.b])
            S.dma("pool", Wg[:], wglu.rearrange("(r p) n -> p r n", p=128), writes=[Wg.b])
            gb = BankPool(PS[4:8])
            n = 0
            for jc in range(8):
                for th in range(2):
                    ps = gb.get()
                    tsl = slice(th * 512, (th + 1) * 512)
                    for r in range(8):
                        S.op("pe", lambda e, r=r: e.matmul(ps[:, :], Wg[:, r, jc * 128:(jc + 1) * 128], Y[:, 4 * r + 3, tsl], start=(r == 0), stop=(r == 7)),
                             reads=[Wg.b, Y.b], writes=[ps.b])
                    sg = sig[n % 2]
                    n += 1
                    S.op("act", lambda e: e.activation(out=sg[:], in_=ps[:, :], func=AF.Sigmoid), reads=[ps.b], writes=[sg.b])
                    S.op("dve", lambda e: e.tensor_tensor(out=y2[:, jc, tsl], in0=Y[:, 4 * jc + 3, tsl], in1=sg[:], op=ALU.mult),
                         reads=[Y.b, sg.b], writes=[y2.b])

            def lhs_fn(k, i):
                tsl = slice(i * 128, (i + 1) * 128)
                if k % 4 == 3:
                    return y2[:, k // 4, tsl], y2.b
                return Y[:, k, tsl], Y.b

            emit_proj_res(S, C, PS, lhs_fn, KC, wo.rearrange("(k p) n -> p k n", p=128), True, x, xb_, H, Hb)
            S.barrier()
        with ExitStack() as es2:
            C = Ctx(nc, es2, S)
            PS = C.psum_banks(8)
            CS, CSb = emit_consts(S, C, cst)
            identb, onesb, lstr = CSb[:, 0, :], CSb[:, 1, :], CSb[:, 2, :]
            iota = CS[:, 3, :]
            X1b = C.sb([128, NT, D_MODEL], BF16, "X1b")
            X1bb = [Buf(f"x1b{i}") for i in range(NT)]
            M1 = [C.sb([128, 32], F32, "M1") for _ in range(NT)]
            M2 = [C.sb([128, 32], F32, "M2") for _ in range(NT)]
            MAf = [C.sb([128, 32], F32, "MAf") for _ in range(NT)]
            MA = [C.sb([128, 32], BF16, "MA") for _ in range(NT)]
            CWm = [C.sb([128, 32], F32, "CWm") for _ in range(NT)]
            POS = [C.sb([128, 32], F32, "POS") for _ in range(NT)]
            x1ob = [Buf(f"x1o{i}") for i in range(NT)]
            with ExitStack() as es2a:
                Ca = Ctx(nc, es2a, S)
                Wr = Ca.sb([128, KC, 36], BF16, "Wr")
                RB = Ca.sb([128, 36], F32, "RB")
                S.dma("pool", Wr[:], wr.rearrange("(k p) n -> p k n", p=128), writes=[Wr.b])
                S.dma("sp", RB[:], rb, writes=[RB.b])
                x1T = Ca.sb([128, KC, 128], BF16, "x1T")
                lg = Ca.sb([128, 36], F32, "lg")
                sm = {k: Ca.sb([128, 1], F32, k) for k in ("gmax", "ngmax", "se", "grp", "m1", "m2", "d", "ed", "den", "w1", "w2", "cw1", "cw2")}
                ex4 = Ca.sb([128, 4], F32); maskg = Ca.sb([128, 4], F32); pen = Ca.sb([128, 4], F32)
                elm = Ca.sb([128, 32], F32); elm2 = Ca.sb([128, 32], F32)
                tb = BankPool(PS[0:6])
                lb = BankPool(PS[6:8])

                def dv(fn, reads, writes):
                    S.op("dve", fn, reads=[t.b for t in reads], writes=[t.b for t in writes])

                def ac(fn, reads, writes):
                    S.op("act", fn, reads=[t.b for t in reads], writes=[t.b for t in writes])

                def ln_cb(i, t):
                    S.dma("sp", x1o[i * 128:(i + 1) * 128, :], t[:], reads=[t.b], writes=[x1ob[i]])
                    S.op("act", lambda e: e.copy(X1b[:, i, :], t[:]), reads=[t.b], writes=[X1bb[i]])
                    v = View(lambda k: X1b[:, i, k[1]], X1bb[i])
                    emit_transpose_tile(S, tb, v, identb, CSb, lambda g: (x1T[:, g * 8:(g + 1) * 8, :], x1T.b))
                    ps = lb.get()
                    for kc in range(KC):
                        S.op("pe", lambda e, kc=kc: e.matmul(ps[:, 0:36], x1T[:, kc, :], Wr[:, kc, :], start=(kc == 0), stop=(kc == KC - 1)),
                             reads=[x1T.b, Wr.b], writes=[ps.b])
                    dv(lambda e: e.tensor_tensor(out=lg[:], in0=ps[:, 0:36], in1=RB[:], op=ALU.add), [ps, RB], [lg])
                    dv(lambda e: e.reduce_max(out=sm["gmax"][:], in_=lg[:, 0:4], axis=AX.X), [lg], [sm["gmax"]])
                    dv(lambda e: e.tensor_scalar(out=sm["ngmax"][:], in0=sm["gmax"][:], scalar1=-1.0, scalar2=None, op0=ALU.mult), [sm["gmax"]], [sm["ngmax"]])
                    ac(lambda e: e.activation(out=ex4[:], in_=lg[:, 0:4], func=AF.Exp, bias=sm["ngmax"][:, 0:1], accum_out=sm["se"][:]), [lg, sm["ngmax"]], [ex4, sm["se"]])
                    dv(lambda e: e.reciprocal(sm["grp"][:], sm["se"][:]), [sm["se"]], [sm["grp"]])
                    dv(lambda e: e.tensor_scalar(out=maskg[:], in0=lg[:, 0:4], scalar1=sm["gmax"][:, 0:1], scalar2=None, op0=ALU.is_equal), [lg, sm["gmax"]], [maskg])
                    dv(lambda e: e.tensor_scalar(out=pen[:], in0=maskg[:], scalar1=-1.0, scalar2=BIG, op0=ALU.add, op1=ALU.mult), [maskg], [pen])
                    for g in range(4):
                        dv(lambda e, g=g: e.tensor_scalar(out=elm[:, 8 * g:8 * g + 8], in0=lg[:, 4 + 8 * g:12 + 8 * g], scalar1=pen[:, g:g + 1], scalar2=None, op0=ALU.add),
                           [lg, pen], [elm])
                    dv(lambda e: e.reduce_max(out=sm["m1"][:], in_=elm[:], axis=AX.X), [elm], [sm["m1"]])
                    dv(lambda e: e.tensor_scalar(out=M1[i][:], in0=elm[:], scalar1=sm["m1"][:, 0:1], scalar2=None, op0=ALU.is_equal), [elm, sm["m1"]], [M1[i]])
                    dv(lambda e: e.scalar_tensor_tensor(out=elm2[:], in0=M1[i][:], scalar=-BIG, in1=elm[:], op0=ALU.mult, op1=ALU.add), [M1[i], elm], [elm2])
                    dv(lambda e: e.reduce_max(out=sm["m2"][:], in_=elm2[:], axis=AX.X), [elm2], [sm["m2"]])
                    dv(lambda e: e.tensor_scalar(out=M2[i][:], in0=elm2[:], scalar1=sm["m2"][:, 0:1], scalar2=None, op0=ALU.is_equal), [elm2, sm["m2"]], [M2[i]])
                    dv(lambda e: e.tensor_tensor(out=sm["d"][:], in0=sm["m2"][:], in1=sm["m1"][:], op=ALU.subtract), [sm["m2"], sm["m1"]], [sm["d"]])
                    ac(lambda e: e.activation(out=sm["ed"][:], in_=sm["d"][:], func=AF.Exp), [sm["d"]], [sm["ed"]])
                    dv(lambda e: e.tensor_scalar(out=sm["den"][:], in0=sm["ed"][:], scalar1=1.0, scalar2=None, op0=ALU.add), [sm["ed"]], [sm["den"]])
                    dv(lambda e: e.reciprocal(sm["w1"][:], sm["den"][:]), [sm["den"]], [sm["w1"]])
                    dv(lambda e: e.tensor_tensor(out=sm["w2"][:], in0=sm["ed"][:], in1=sm["w1"][:], op=ALU.mult), [sm["ed"], sm["w1"]], [sm["w2"]])
                    dv(lambda e: e.tensor_tensor(out=sm["cw1"][:], in0=sm["w1"][:], in1=sm["grp"][:], op=ALU.mult), [sm["w1"], sm["grp"]], [sm["cw1"]])
                    dv(lambda e: e.tensor_tensor(out=sm["cw2"][:], in0=sm["w2"][:], in1=sm["grp"][:], op=ALU.mult), [sm["w2"], sm["grp"]], [sm["cw2"]])
                    dv(lambda e: e.tensor_tensor(out=MAf[i][:], in0=M1[i][:], in1=M2[i][:], op=ALU.add), [M1[i], M2[i]], [MAf[i]])
                    dv(lambda e: e.tensor_copy(MA[i][:], MAf[i][:]), [MAf[i]], [MA[i]])
                    dv(lambda e: e.tensor_scalar(out=CWm[i][:], in0=M1[i][:], scalar1=sm["cw1"][:, 0:1], scalar2=None, op0=ALU.mult), [M1[i], sm["cw1"]], [CWm[i]])
                    dv(lambda e: e.scalar_tensor_tensor(out=CWm[i][:], in0=M2[i][:], scalar=sm["cw2"][:, 0:1], in1=CWm[i][:], op0=ALU.mult, op1=ALU.add),
                       [M2[i], sm["cw2"], CWm[i]], [CWm[i]])

                emit_ln_tiles(S, Ca, H, Hb, lng, lnb, ln_cb)
                for i in range(NT):
                    ps = lb.get()
                    for i2 in range(i):
                        S.op("pe", lambda e, i2=i2: e.matmul(ps[:, 0:32], onesb, MA[i2][:], start=(i2 == 0), stop=False), reads=[CSb.b, MA[i2].b], writes=[ps.b])
                    S.op("pe", lambda e: e.matmul(ps[:, 0:32], lstr, MA[i][:], start=(i == 0), stop=True), reads=[CSb.b, MA[i].b], writes=[ps.b])
                    S.op("act", lambda e: e.copy(POS[i][:], ps[:, 0:32]), reads=[ps.b], writes=[POS[i].b])
                S.barrier()
            with ExitStack() as es2b:
                Cb = Ctx(nc, es2b, S)
                SELW = Cb.sb([128, N_EXPERTS, TPC], BF16, "SELW")
                Sel = [[Cb.sb([128, CAP], BF16, "Sel") for _ in range(NT)] for _ in range(2)]
                Swt = [Cb.sb([128, CAP], BF16, "Swt") for _ in range(4)]
                XG = [Cb.sb([128, KC, CAP], BF16, "XG") for _ in range(2)]
                gbk = BankPool(PS[0:5])
                sbk = BankPool(PS[5:8])
                nsw = 0
                for ex in range(N_EXPERTS):
                    sel = Sel[ex % 2]
                    ps_t = sbk.get()
                    ps_tb = ps_t[:].bitcast(BF16)
                    for i in range(NT):
                        S.op("dve", lambda e, i=i: e.tensor_scalar(out=sel[i][:], in0=iota, scalar1=POS[i][:, ex:ex + 1], scalar2=MAf[i][:, ex:ex + 1],
                                                                   op0=ALU.is_equal, op1=ALU.mult), reads=[CS.b, POS[i].b, MAf[i].b], writes=[sel[i].b])
                        sw = Swt[nsw % 4]
                        nsw += 1
                        S.op("dve", lambda e, i=i, sw=sw: e.tensor_scalar(out=sw[:], in0=iota, scalar1=POS[i][:, ex:ex + 1], scalar2=CWm[i][:, ex:ex + 1],
                                                                          op0=ALU.is_equal, op1=ALU.mult), reads=[CS.b, POS[i].b, CWm[i].b], writes=[sw.b])
                        S.op("pe", lambda e, i=i, sw=sw: e.transpose(ps_tb[:, i * 128:(i + 1) * 128], sw[:], identb), reads=[sw.b, CSb.b], writes=[ps_t.b])
                    S.op("act", lambda e: e.copy(SELW[:, ex, :], ps_tb[:, 0:1024]), reads=[ps_t.b], writes=[SELW.b])
                    xg_ = XG[ex % 2]
                    for g in range(8):
                        ps = gbk.get()
                        for j in range(4):
                            kc = g * 4 + j
                            for i in range(NT):
                                S.op("pe", lambda e, i=i, j=j, kc=kc: e.matmul(ps[:, j * 128:(j + 1) * 128], X1b[:, i, kc * 128:(kc + 1) * 128], sel[i][:],
                                                                            start=(i == 0), stop=(i == NT - 1)),
                                     reads=[X1bb[i], sel[i].b], writes=[ps.b])
                        if g % 2 == 0:
                            S.op("act", lambda e, g=g: e.copy(xg_[:, 4 * g:4 * g + 4, :], ps[:, :].rearrange("p (a b) -> p a b", a=4)), reads=[ps.b], writes=[xg_.b])
                        else:
                            S.op("dve", lambda e, g=g: e.tensor_copy(xg_[:, 4 * g:4 * g + 4, :], ps[:, :].rearrange("p (a b) -> p a b", a=4)), reads=[ps.b], writes=[xg_.b])
                    S.dma("sp", xg[ex], xg_[:], reads=[xg_.b])
                S.dma("sp", selw, SELW[:], reads=[SELW.b])
                S.barrier()
        S.barrier()
    return nc


EPC = N_EXPERTS // NCORES
SLOTS = NCORES * CAP


def build_b2():
    nc = bass.Bass("TRN2", target_bir_lowering=False)
    xg = nc.dram_tensor("xg", [EPC, 128, KC, SLOTS], BF16, kind="ExternalInput").ap()
    wg_ = nc.dram_tensor("wg", [EPC, D_MODEL, D_EXPERT], F32, kind="ExternalInput").ap()
    wu_ = nc.dram_tensor("wu", [EPC, D_MODEL, D_EXPERT], F32, kind="ExternalInput").ap()
    wd_ = nc.dram_tensor("wd", [EPC, D_EXPERT, D_MODEL], F32, kind="ExternalInput").ap()
    cst = nc.dram_tensor("cst", [128, 512], F32, kind="ExternalInput").ap()
    yo = nc.dram_tensor("yo", [EPC, SLOTS, D_MODEL], BF16, kind="ExternalOutput").ap()
    NST = SLOTS // 128
    with ExitStack() as outer:
        S = Sched(nc, outer)
        C = Ctx(nc, outer, S)
        PS = C.psum_banks(8)
        CS, CSb = emit_consts(S, C, cst)
        identb = CSb[:, 0, :]
        Wg = C.sb([128, KC, D_EXPERT], BF16, "Wg")
        Wu = C.sb([128, KC, D_EXPERT], BF16, "Wu")
        Wd = C.sb([128, 4, D_MODEL], BF16, "Wd")
        X = C.sb([128, KC, SLOTS], BF16, "X")
        hidT = C.sb([128, 4, SLOTS], BF16, "hidT")
        sg = [C.sb([128, 512], F32, "sg") for _ in range(2)]
        hid = [C.sb([128, 512], BF16, "hid") for _ in range(2)]
        Yt = [C.sb([128, D_MODEL], BF16, "Yt") for _ in range(2)]
        gub = BankPool(PS[0:4])
        tbk = BankPool(PS[4:5])
        dbk = BankPool(PS[5:8])
        ny = 0
        for ex in range(EPC):
            wgv = wg_[ex].rearrange("(k p) n -> p k n", p=128)
            wuv = wu_[ex].rearrange("(k p) n -> p k n", p=128)
            wdv = wd_[ex].rearrange("(m p) n -> p m n", p=128)
            for j in range(4):
                S.dma("pool", Wg[:, 8 * j:8 * j + 8, :], wgv[:, 8 * j:8 * j + 8, :], writes=[Wg.b])
            for j in range(4):
                S.dma("pool", Wu[:, 8 * j:8 * j + 8, :], wuv[:, 8 * j:8 * j + 8, :], writes=[Wu.b])
            for j in range(4):
                S.dma("sp", X[:, 8 * j:8 * j + 8, :], xg[ex][:, 8 * j:8 * j + 8, :], writes=[X.b])
            for j in range(4):
                S.dma("pool", Wd[:, j, :], wdv[:, j, :], writes=[Wd.b])
            for st in range(NST):
                ssl = slice(st * 128, (st + 1) * 128)
                pg = gub.get()
                pu = gub.get()
                for kc in range(KC):
                    S.op("pe", lambda e, kc=kc: e.matmul(pg[:, :], X[:, kc, ssl], Wg[:, kc, :], start=(kc == 0), stop=(kc == KC - 1)), reads=[X.b, Wg.b], writes=[pg.b])
                for kc in range(KC):
                    S.op("pe", lambda e, kc=kc: e.matmul(pu[:, :], X[:, kc, ssl], Wu[:, kc, :], start=(kc == 0), stop=(kc == KC - 1)), reads=[X.b, Wu.b], writes=[pu.b])
                s_, h_ = sg[st % 2], hid[st % 2]
                S.op("act", lambda e: e.activation(out=s_[:], in_=pg[:, :], func=AF.Silu), reads=[pg.b], writes=[s_.b])
                S.op("dve", lambda e: e.tensor_tensor(out=h_[:], in0=s_[:], in1=pu[:, :], op=ALU.mult), reads=[s_.b, pu.b], writes=[h_.b])
                pt = tbk.get()
                ptb = pt[:].bitcast(BF16)
                for mc in range(4):
                    S.op("pe", lambda e, mc=mc: e.transpose(ptb[:, mc * 128:(mc + 1) * 128], h_[:, mc * 128:(mc + 1) * 128], identb), reads=[h_.b, CSb.b], writes=[pt.b])
                S.op("act", lambda e: e.copy(hidT[:, :, ssl], ptb[:, 0:512].rearrange("p (a b) -> p a b", a=4)), reads=[pt.b], writes=[hidT.b])
            for st in range(NST):
                ssl = slice(st * 128, (st + 1) * 128)
                y_ = Yt[ny % 2]
                ny += 1
                for n in range(8):
                    pd = dbk.get()
                    for mc in range(4):
                        S.op("pe", lambda e, mc=mc: e.matmul(pd[:, :], hidT[:, mc, ssl], Wd[:, mc, n * 512:(n + 1) * 512], start=(mc == 0), stop=(mc == 3)),
                             reads=[hidT.b, Wd.b], writes=[pd.b])
                    if n % 2 == 0:
                        S.op("act", lambda e: e.copy(y_[:, n * 512:(n + 1) * 512], pd[:, :]), reads=[pd.b], writes=[y_.b])
                    else:
                        S.op("dve", lambda e: e.tensor_copy(y_[:, n * 512:(n + 1) * 512], pd[:, :]), reads=[pd.b], writes=[y_.b])
                S.dma("sp", yo[ex, ssl, :], y_[:], reads=[y_.b])
        S.barrier()
    return nc


def build_b3():
    nc = bass.Bass("TRN2", target_bir_lowering=False)
    yin = nc.dram_tensor("yin", [N_EXPERTS * CAP, D_MODEL], BF16, kind="ExternalInput").ap()
    selw = nc.dram_tensor("selw", [128, N_EXPERTS, TPC], BF16, kind="ExternalInput").ap()
    x1 = nc.dram_tensor("x1", [TPC, D_MODEL], F32, kind="ExternalInput").ap()
    lng = nc.dram_tensor("lng", [128, D_MODEL], F32, kind="ExternalInput").ap()
    lnb = nc.dram_tensor("lnb", [128, D_MODEL], F32, kind="ExternalInput").ap()
    cst = nc.dram_tensor("cst", [128, 512], F32, kind="ExternalInput").ap()
    x2 = nc.dram_tensor("x2", [TPC, D_MODEL], F32, kind="ExternalOutput").ap()
    xTo = nc.dram_tensor("xTo", [D_MODEL, TPC], BF16, kind="ExternalOutput").ap()
    H = nc.dram_tensor("Hscr", [TPC, D_MODEL], F32, kind="Internal").ap()
    with ExitStack() as outer:
        S = Sched(nc, outer)
        Hb = [Buf(f"H{i}") for i in range(NT)]
        xb_ = [Buf(f"xr{i}") for i in range(NT)]
        with ExitStack() as es:
            C = Ctx(nc, es, S)
            PS = C.psum_banks(8)
            SW = C.sb([128, N_EXPERTS, TPC], BF16, "SW")
            for j in range(4):
                S.dma("sp", SW[:, 8 * j:8 * j + 8, :], selw[:, 8 * j:8 * j + 8, :], writes=[SW.b])
            emit_proj_res(S, C, PS, lambda k, i: (SW[:, k, i * 128:(i + 1) * 128], SW.b), N_EXPERTS,
                          yin.rearrange("(e s) d -> s e d", s=128), False, x1, xb_, H, Hb)
            S.barrier()
        with ExitStack() as es2:
            C = Ctx(nc, es2, S)
            PS = C.psum_banks(8)
            CS, CSb = emit_consts(S, C, cst)
            xTs = C.sb([128, KC, TPC], BF16, "xTs")
            xb = [C.sb([128, D_MODEL], BF16, "xb") for _ in range(2)]
            banks = BankPool(PS)
            x2b = [Buf(f"x2{i}") for i in range(NT)]

            def cb(i, t):
                S.dma("sp", x2[i * 128:(i + 1) * 128, :], t[:], reads=[t.b], writes=[x2b[i]])
                b_ = xb[i % 2]
                S.op("act", lambda e: e.copy(b_[:], t[:]), reads=[t.b], writes=[b_.b])
                emit_transpose_tile(S, banks, b_, CSb[:, 0, :], CSb, lambda g: (xTs[:, g * 8:(g + 1) * 8, i * 128:(i + 1) * 128], xTs.b))

            emit_ln_tiles(S, C, H, Hb, lng, lnb, cb)
            S.dma("sp", xTo.rearrange("(kc p) t -> p kc t", p=128), xTs[:], reads=[xTs.b])
            S.barrier()
        S.barrier()
    return nc


_NC_CACHE = {}
_DBG = None


def _get_nc(name, fn):
    if name not in _NC_CACHE:
        _NC_CACHE[name] = fn()
    return _NC_CACHE[name]


def _run(nc, maps):
    res = run_bass_kernel_spmd(nc, maps, core_ids=list(range(len(maps))))
    return res.results


def mix_perm():
    p = []
    for r in range(NCORES):
        p.append(np.arange(384 * r, 384 * r + 384))
        p.append(GDN_WIDTH + np.arange(128 * r, 128 * r + 128))
    return np.concatenate(p)


def prep_b1_weights(layer, inp):
    wo = np.ascontiguousarray(inp["w_out"][layer][mix_perm(), :])
    wglu = np.ascontiguousarray(inp["s5_w_glu"][layer])
    wr = np.concatenate([inp["router_group_w"][layer]] + [inp["router_expert_w"][layer][g] for g in range(4)], axis=1)
    rbv = np.concatenate([inp["router_group_b"][layer], inp["router_expert_b"][layer].reshape(-1)])
    return {"wo": wo, "wglu": wglu, "wr": np.ascontiguousarray(wr.astype(np.float32)),
            "rb": np.ascontiguousarray(np.broadcast_to(rbv[None, :], (128, 36))).astype(np.float32),
            "lng": np.ascontiguousarray(np.broadcast_to(inp["ln1_g"][layer][None, :], (128, D_MODEL))),
            "lnb": np.ascontiguousarray(np.broadcast_to(inp["ln1_b"][layer][None, :], (128, D_MODEL))),
            "cst": b_consts()}


def run_layer(layer, inp, xT_all, xres, ncores=NCORES, L=SEQ):
    ncm = _get_nc(("mixer", L), lambda: build_mixer(L, True))
    maps = []
    for c in range(NCORES):
        m = prep_mixer(c, layer, inp)
        m["xT"] = xT_all
        maps.append(m)
    res = _run(ncm, maps)
    ymix_all = np.concatenate([np.asarray(res[c]["yT"]) for c in range(NCORES)], axis=0)
    del res, maps
    ntc = L // TPC
    w1 = prep_b1_weights(layer, inp)
    maps = []
    for c in range(ntc):
        m = dict(w1)
        m["ymix"] = np.ascontiguousarray(ymix_all[:, c * TPC:(c + 1) * TPC])
        m["x"] = xres[c]
        maps.append(m)
    res = _run(_get_nc("b1", build_b1), maps)
    x1 = [np.asarray(res[c]["x1"]) for c in range(ntc)]
    if _DBG is not None:
        _DBG["ymix"] = ymix_all
        _DBG["x1"] = x1
    xg = [np.asarray(res[c]["xg"]) for c in range(ntc)]
    selw = [np.asarray(res[c]["selw"]) for c in range(ntc)]
    del res, maps
    maps = []
    for c2 in range(NCORES):
        xin = np.zeros((EPC, 128, KC, SLOTS), ml_dtypes.bfloat16)
        for c in range(ntc):
            xin[:, :, :, c * CAP:(c + 1) * CAP] = xg[c][EPC * c2:EPC * c2 + EPC]
        maps.append({"xg": xin,
                     "wg": np.ascontiguousarray(inp["expert_w_gate"][layer][EPC * c2:EPC * c2 + EPC]),
                     "wu": np.ascontiguousarray(inp["expert_w_up"][layer][EPC * c2:EPC * c2 + EPC]),
                     "wd": np.ascontiguousarray(inp["expert_w_down"][layer][EPC * c2:EPC * c2 + EPC]),
                     "cst": b_consts()})
    del xg
    res = _run(_get_nc("b2", build_b2), maps)
    yo = [np.asarray(res[c2]["yo"]) for c2 in range(NCORES)]
    del res, maps
    lng = np.ascontiguousarray(np.broadcast_to(inp["ln2_g"][layer][None, :], (128, D_MODEL)))
    lnb = np.ascontiguousarray(np.broadcast_to(inp["ln2_b"][layer][None, :], (128, D_MODEL)))
    maps = []
    for c in range(ntc):
        yin = np.concatenate([yo[e // EPC][e % EPC, c * CAP:(c + 1) * CAP, :] for e in range(N_EXPERTS)], axis=0)
        maps.append({"yin": np.ascontiguousarray(yin), "selw": selw[c], "x1": x1[c], "lng": lng, "lnb": lnb, "cst": b_consts()})
    res = _run(_get_nc("b3", build_b3), maps)
    x2 = [np.asarray(res[c]["x2"]) for c in range(ntc)]
    xTn = [np.asarray(res[c]["xTo"]) for c in range(ntc)]
    return x2, xTn


def kernel(**inputs):
    inp = {k: np.asarray(v) for k, v in inputs.items()}
    x = inp["x"][0]
    xres = [np.ascontiguousarray(x[c * TPC:(c + 1) * TPC]) for c in range(NCORES)]
    res = _run(_get_nc("t0", build_t0), [{"x": xres[c], "cst": b_consts()} for c in range(NCORES)])
    xT_all = np.stack([np.asarray(res[c]["xTo"]) for c in range(NCORES)], axis=0)
    for layer in range(DEPTH):
        xres, xTn = run_layer(layer, inp, xT_all, xres)
        xT_all = np.stack(xTn, axis=0)
    return np.concatenate(xres, axis=0)[None].astype(np.float32)
```

```python
import numpy as np
import ml_dtypes
from contextlib import ExitStack
import concourse.bass as bass
import concourse.mybir as mybir
from concourse.bass_utils import run_bass_kernel_spmd

F32 = mybir.dt.float32
BF16 = mybir.dt.bfloat16
I32 = mybir.dt.int32
AF = mybir.ActivationFunctionType
ALU = mybir.AluOpType
AX = mybir.AxisListType

D_MODEL = 4096
SEQ = 8192
DEPTH = 2
NCORES = 8
KC = D_MODEL // 128
GDN_HEADS = 24
HPC = 3
GDN_WIDTH = 3072
S5_WIDTH = 1024
S5_STATE = 64
GPC = 8
CH = 64
N_EXPERTS = 32
D_EXPERT = 512
DN_ALPHA = (2 * DEPTH) ** 0.25
LN_EPS = 1e-5
RMS_EPS = 1e-6
L2_EPS = 1e-6
CAP = 128
TWO_PI = float(2 * np.pi)
MAGIC = 12582912.0


class Ev:
    __slots__ = ("sem", "sid", "val", "eng")

    def __init__(self, sem, sid, val, eng):
        self.sem, self.sid, self.val, self.eng = sem, sid, val, eng


class Buf:
    def __init__(self, name, excl=False):
        self.name = name
        self.w = None
        self.r = {}
        self.excl = excl


class Sched:
    ROT = 8000
    NSLOT = 12

    def __init__(self, nc, es):
        self.nc, self.es = nc, es
        self.engs = dict(pe=nc.tensor, act=nc.scalar, dve=nc.vector, pool=nc.gpsimd, sp=nc.sync)
        self.nsem = 0
        self.cur = {}
        for e in self.engs:
            self.cur[e] = [self._newsem(e), 0]
        self.known = {e: {} for e in self.engs}
        self.slots = {}
        self.slot_i = {}
        self.nops = 0
        self.prev = {}

    def _newsem(self, tag):
        self.nsem += 1
        s = self.es.enter_context(self.nc.semaphore(f"s{self.nsem}_{tag}"))
        return (s, self.nsem)

    def _wait(self, e, ev):
        if ev is None:
            return
        if e == "pe" and ev.eng == "pe":
            return
        k = self.known[e]
        if k.get(ev.sid, -1) >= ev.val:
            return
        self.engs[e].wait_ge(ev.sem, ev.val)
        k[ev.sid] = ev.val

    def _deps(self, e, reads, writes):
        for b in reads:
            self._wait(e, b.w)
            if b.excl:
                for ev in list(b.r.values()):
                    self._wait(e, ev)
        for b in writes:
            self._wait(e, b.w)
            for ev in list(b.r.values()):
                self._wait(e, ev)

    def _commit(self, ev, reads, writes):
        for b in reads:
            if b.excl:
                b.w = ev
                b.r = {}
            else:
                b.r[ev.sid] = ev
        for b in writes:
            b.w = ev
            b.r = {}

    def op(self, e, fn, reads=(), writes=()):
        self._deps(e, reads, writes)
        ins = fn(self.engs[e])
        (sem, sid), cnt = self.cur[e]
        cnt += 1
        ins.then_inc(sem, 1)
        ev = Ev(sem, sid, cnt, e)
        self.cur[e][1] = cnt
        if cnt >= self.ROT:
            self.prev[e] = ev
            self.cur[e] = [self._newsem(e), 0]
        self._commit(ev, reads, writes)
        self.nops += 1
        return ev

    def dma(self, q, out, in_, reads=(), writes=(), **kw):
        if q not in self.slots:
            self.slots[q] = [[self._newsem("d" + q), 0, None] for _ in range(self.NSLOT)]
            self.slot_i[q] = 0
        i = self.slot_i[q]
        self.slot_i[q] = (i + 1) % self.NSLOT
        slot = self.slots[q][i]
        self._wait(q, slot[2])
        self._deps(q, reads, writes)
        ins = self.engs[q].dma_start(out=out, in_=in_, **kw)
        slot[1] += 16
        (sem, sid) = slot[0]
        ins.then_inc(sem, 16)
        ev = Ev(sem, sid, slot[1], "dma")
        slot[2] = ev
        self._commit(ev, reads, writes)
        self.nops += 1
        return ev

    def dma_ins(self, q, mk, reads=(), writes=()):
        if q not in self.slots:
            self.slots[q] = [[self._newsem("d" + q), 0, None] for _ in range(self.NSLOT)]
            self.slot_i[q] = 0
        i = self.slot_i[q]
        self.slot_i[q] = (i + 1) % self.NSLOT
        slot = self.slots[q][i]
        self._wait(q, slot[2])
        self._deps(q, reads, writes)
        ins = mk(self.engs[q])
        slot[1] += 16
        (sem, sid) = slot[0]
        ins.then_inc(sem, 16)
        ev = Ev(sem, sid, slot[1], "dma")
        slot[2] = ev
        self._commit(ev, reads, writes)
        return ev

    def barrier(self):
        evs = []
        for e in self.engs:
            (sem, sid), cnt = self.cur[e]
            if cnt > 0:
                evs.append(Ev(sem, sid, cnt, e))
            elif e in self.prev:
                evs.append(self.prev[e])
        for q in self.slots:
            for slot in self.slots[q]:
                if slot[2] is not None:
                    evs.append(slot[2])
        for e in self.engs:
            for ev in evs:
                self._wait(e, ev)

    def finish(self, bufs):
        for b in bufs:
            self._wait("sp", b.w)


class T:
    def __init__(self, t, name, excl=False):
        self.t = t
        self.b = Buf(name, excl)

    def __getitem__(self, k):
        return self.t[k]


_uid = [0]


class Ctx:
    def __init__(self, nc, es, S=None):
        self.nc, self.es = nc, es
        self.S = S if S is not None else Sched(nc, es)

    @property
    def n(self):
        return _uid[0]

    @n.setter
    def n(self, v):
        _uid[0] = v

    def sb(self, shape, dt=F32, name=None):
        self.n += 1
        nm = f"{name or 't'}_{self.n}"
        return T(self.es.enter_context(self.nc.sbuf_tensor(nm, list(shape), dt)), nm)

    def psum_banks(self, n=8):
        out = []
        for i in range(n):
            self.n += 1
            nm = f"ps{i}_{self.n}"
            out.append(T(self.es.enter_context(self.nc.psum_tensor(nm, [128, 512], F32)), nm, excl=True))
        return out


class View:
    def __init__(self, fn, b):
        self.fn, self.b = fn, b

    def __getitem__(self, k):
        return self.fn(k)


class BankPool:
    def __init__(self, banks):
        self.banks = list(banks)
        self.i = 0

    def get(self):
        b = self.banks[self.i]
        self.i = (self.i + 1) % len(self.banks)
        return b


GDN_COLS = 4 * HPC * 128 + 2 * HPC
NEG = -30000.0


def gdn_consts():
    c = np.zeros((128, 8, 128), np.float32)
    c[:, 0, :] = np.eye(128)
    i = np.arange(64)
    U = (i[:, None] <= i[None, :]).astype(np.float32)
    c[:64, 1, :64] = U
    c[:64, 2, :64] = -U
    c[:64, 3, :64] = (i[:, None] > i[None, :]).astype(np.float32)
    c[:, 4, :] = 1.0
    c[:64, 5, :64] = np.where(i[:, None] > i[None, :], 0.0, NEG)
    c[:64, 6, :64] = np.where(i[None, :] >= i[:, None], 0.0, NEG)
    return c.reshape(128, 8 * 128)


def emit_gdn(nc, S, L, x_bf16, xT, wg, convw, hp, cst, yT):
    NR = max(1, L // 1024)
    TR = min(L, 1024)
    NSB = L // 512
    xq = "sp" if x_bf16 else "pool"

    with ExitStack() as es:
        C = Ctx(nc, es, S)
        W = C.sb([128, KC, GDN_COLS], BF16, "W")
        XT = [C.sb([128, 8, 512], BF16, "XT") for _ in range(4)]
        CW = C.sb([128, 36], F32, "CW")
        HP = C.sb([128, 64], F32, "HP")
        CS = C.sb([128, 8, 128], F32, "CS")
        CSb = C.sb([128, 2, 128], BF16, "CSb")
        EAL = C.sb([128, 24], F32, "EAL")
        halo = C.sb([128, 9, 3], F32, "halo")
        raw = [C.sb([128, 515], F32, "raw") for _ in range(1)]
        cv = [C.sb([128, 512], F32, "cv") for _ in range(1)]
        actb = [C.sb([128, 512], F32, "actb") for _ in range(1)]
        sqb = [C.sb([128, 512], BF16, "sqb") for _ in range(1)]
        rnb = [C.sb([128, 512], F32, "rnb") for _ in range(1)]
        ACTS = [dict(qn=[C.sb([128, 512], BF16, "qn") for _ in range(HPC)],
                     kn=[C.sb([128, 512], BF16, "kn") for _ in range(HPC)],
                     vT=[C.sb([128, 512], BF16, "vT") for _ in range(HPC)],
                     zs=[C.sb([128, 512], BF16, "zs") for _ in range(HPC)],
                     abT=C.sb([8, 512], F32, "abT")) for _ in range(2)]
        yo = [C.sb([128, 512], BF16, "yo") for _ in range(HPC)]
        Sst = [C.sb([128, 128], F32, "S") for _ in range(HPC)]
        Sb = [C.sb([128, 128], BF16, "Sb") for _ in range(HPC)]
        PS = C.psum_banks(8)
        proj_banks = BankPool(PS[0:2])
        wk_banks = BankPool(PS[2:8])

        ident = CS[:, 0, :]
        identb = CSb[:, 0, :]
        onesb = CSb[:, 1, :]

        wv = wg.rearrange("(kc p) n -> p kc n", p=128)
        for j in range(8):
            S.dma("pool", W[:, 4 * j:4 * j + 4, :], wv[:, 4 * j:4 * j + 4, :], writes=[W.b])
        S.dma("sp", CW[:], convw, writes=[CW.b])
        S.dma("sp", HP[:], hp, writes=[HP.b])
        S.dma("sp", CS[:].rearrange("p a b -> p (a b)"), cst, writes=[CS.b])
        S.op("dve", lambda e: e.tensor_copy(CSb[:, 0, :], CS[:, 0, :]), reads=[CS.b], writes=[CSb.b])
        S.op("dve", lambda e: e.tensor_copy(CSb[:, 1, :], CS[:, 4, :]), reads=[CS.b], writes=[CSb.b])
        S.op("act", lambda e: e.activation(out=EAL[:], in_=HP[:, 40:64], func=AF.Exp), reads=[HP.b], writes=[EAL.b])
        S.op("dve", lambda e: e.memset(halo[:].rearrange("p a b -> p (a b)"), 0.0), writes=[halo.b])
        for h in range(HPC):
            S.op("dve", lambda e, h=h: e.memset(Sst[h][:], 0.0), writes=[Sst[h].b])
            S.op("dve", lambda e, h=h: e.memset(Sb[h][:], 0.0), writes=[Sb[h].b])

        xv = xT.rearrange("r (kc p) t -> r p kc t", p=128)

        def small(shape, dt=F32, name="w"):
            return C.sb(shape, dt, name)

        NCG = 2
        NU = NCG * HPC
        GG = dict(
            x1=small([64, 24]), ex=small([64, 24]), sp=small([64, 24]), g=small([64, 24]), beta=small([64, 24]),
            edec=small([64, 24]), edlm=small([64, 24]), edl=small([128, 24]), bedec=small([64, 24]),
        )
        HW = []
        for u in range(NU):
            HW.append(dict(
                Gb=small([64, 64]), gs=small([64, 64]), gT=small([64, 64]),
                kbe=small([64, 128], BF16), ktail=small([64, 128], BF16),
                bv=small([64, 128], BF16), N=small([64, 64]),
                P=[small([64, 64]), small([64, 64])], Q=[small([64, 64]), small([64, 64])],
                X=small([64, 64]), Xb=small([64, 64], BF16), qkT=small([64, 64], BF16),
                nwT=small([128, 64], BF16), Lm=small([64, 128]), qd=small([128, 64], BF16),
                vn=small([64, 128], BF16), ssq=small([64, 1]), rt=small([64, 1]), rstd=small([64, 1]),
                junk=small([64, 128], BF16),
            ))

        def proj_gen(sbn):
            A = ACTS[sbn % 2]
            r = (sbn * 512) // TR
            t0 = (sbn * 512) % TR
            for j in range(4):
                S.dma(xq, XT[j][:], xv[r, :, 8 * j:8 * j + 8, t0:t0 + 512], writes=[XT[j].b])
            yield

            def ptile(c0, m):
                ps = proj_banks.get()
                for kc in range(KC):
                    S.op("pe", lambda e, kc=kc: e.matmul(ps[0:m, :], W[:, kc, c0:c0 + m], XT[kc // 8][:, kc % 8, :],
                                                         start=(kc == 0), stop=(kc == KC - 1)),
                         reads=[W.b, XT[kc // 8].b], writes=[ps.b])
                    if kc % 2 == 1:
                        yield
                return ps

            for kind, base in (("k", 384), ("q", 0), ("v", 768)):
                for h in range(HPC):
                    ct = {"q": 0, "k": 3, "v": 6}[kind] + h
                    ps = yield from ptile(base + 128 * h, 128)
                    rw, cvb, ab_, sq_, rn_ = raw[0], cv[0], actb[0], sqb[0], rnb[0]
                    S.op("act", lambda e: e.copy(rw[:, 3:515], ps[:, :]), reads=[ps.b], writes=[rw.b])
                    S.op("dve", lambda e: e.tensor_copy(rw[:, 0:3], halo[:, ct, :]), reads=[halo.b], writes=[rw.b])
                    yield
                    S.op("dve", lambda e: e.tensor_scalar(out=cvb[:], in0=rw[:, 0:512], scalar1=CW[:, 4 * ct:4 * ct + 1],
                                                          scalar2=None, op0=ALU.mult), reads=[rw.b, CW.b], writes=[cvb.b])
                    for j in range(1, 4):
                        S.op("dve", lambda e, j=j: e.scalar_tensor_tensor(out=cvb[:], in0=rw[:, j:j + 512],
                                                                          scalar=CW[:, 4 * ct + j:4 * ct + j + 1], in1=cvb[:],
                                                                          op0=ALU.mult, op1=ALU.add),
                             reads=[rw.b, CW.b, cvb.b], writes=[cvb.b])
                        yield
                    S.op("dve", lambda e: e.tensor_copy(halo[:, ct, :], rw[:, 512:515]), reads=[rw.b], writes=[halo.b])
                    if kind == "v":
                        S.op("act", lambda e: e.activation(out=A["vT"][h][:], in_=cvb[:], func=AF.Silu), reads=[cvb.b], writes=[A["vT"][h].b])
                        yield
                        continue
                    S.op("act", lambda e: e.activation(out=ab_[:], in_=cvb[:], func=AF.Silu), reads=[cvb.b], writes=[ab_.b])
                    yield
                    S.op("act", lambda e: e.activation(out=sq_[:], in_=ab_[:], func=AF.Square), reads=[ab_.b], writes=[sq_.b])
                    yield
                    ps2 = proj_banks.get()
                    S.op("pe", lambda e: e.matmul(ps2[:, :], onesb, sq_[:], start=True, stop=True), reads=[CSb.b, sq_.b], writes=[ps2.b])
                    S.op("act", lambda e: e.activation(out=rn_[:], in_=ps2[:, :], func=AF.Sqrt, bias=L2_EPS), reads=[ps2.b], writes=[rn_.b])
                    yield
                    S.op("dve", lambda e: e.reciprocal(rn_[:], rn_[:]), reads=[rn_.b], writes=[rn_.b])
                    dst = A["qn"][h] if kind == "q" else A["kn"][h]
                    sc = 128.0 ** -0.5 if kind == "q" else 1.0
                    S.op("dve", lambda e: e.scalar_tensor_tensor(out=dst[:], in0=ab_[:], scalar=sc, in1=rn_[:], op0=ALU.mult, op1=ALU.mult),
                         reads=[ab_.b, rn_.b], writes=[dst.b])
                    yield
            for h in range(HPC):
                ps = yield from ptile(1152 + 128 * h, 128)
                S.op("act", lambda e: e.activation(out=A["zs"][h][:], in_=ps[:, :], func=AF.Silu), reads=[ps.b], writes=[A["zs"][h].b])
                yield
            ps = yield from ptile(1536, 6)
            S.op("act", lambda e: e.copy(A["abT"][0:6, :], ps[0:6, :]), reads=[ps.b], writes=[A["abT"].b])
            yield

        gen_state = [None]

        def tick():
            g = gen_state[0]
            if g is not None:
                try:
                    next(g)
                except StopIteration:
                    gen_state[0] = None

        def drain():
            while gen_state[0] is not None:
                tick()

        gen_state[0] = proj_gen(0)
        drain()
        for sb in range(NSB):
            A_ = ACTS[sb % 2]
            qn, kn, vT, zs, abT = A_["qn"], A_["kn"], A_["vT"], A_["zs"], A_["abT"]
            if sb + 1 < NSB:
                gen_state[0] = proj_gen(sb + 1)

            U_ = CS[0:64, 1, 0:64]
            nU_ = CS[0:64, 2, 0:64]
            gps = PS[2]
            for c in range(8):
                S.op("pe", lambda e, c=c: e.transpose(gps[0:64, 6 * c:6 * c + 6], abT[0:6, 64 * c:64 * c + 64], ident[0:6, 0:6]), reads=[abT.b, CS.b], writes=[gps.b])
            g3 = gps[0:64, 0:48].rearrange("p (c k) -> p c k", k=6)
            S.op("dve", lambda e: e.tensor_tensor(out=GG["x1"][:].rearrange("p (c h) -> p c h", h=3), in0=g3[:, :, 0:3],
                                                  in1=HP[0:64, 16:40].rearrange("p (c h) -> p c h", h=3), op=ALU.add),
                 reads=[gps.b, HP.b], writes=[GG["x1"].b])
            S.op("act", lambda e: e.activation(out=GG["beta"][:].rearrange("p (c h) -> p c h", h=3), in_=g3[:, :, 3:6], func=AF.Sigmoid), reads=[gps.b], writes=[GG["beta"].b])
            S.op("act", lambda e: e.activation(out=GG["ex"][:], in_=GG["x1"][:], func=AF.Exp), reads=[GG["x1"].b], writes=[GG["ex"].b])
            S.op("act", lambda e: e.activation(out=GG["sp"][:], in_=GG["ex"][:], func=AF.Ln, bias=1.0), reads=[GG["ex"].b], writes=[GG["sp"].b])
            S.op("dve", lambda e: e.scalar_tensor_tensor(out=GG["g"][:], in0=GG["sp"][:], scalar=-1.0, in1=EAL[0:64, :], op0=ALU.mult, op1=ALU.mult),
                 reads=[GG["sp"].b, EAL.b], writes=[GG["g"].b])
            S.op("pe", lambda e: e.matmul(gps[0:64, 64:88], CS[0:64, 1, 0:64], GG["g"][:], start=True, stop=True), reads=[CS.b, GG["g"].b], writes=[gps.b])
            S.op("pe", lambda e: e.matmul(gps[0:64, 96:120], CS[0:64, 3, 0:64], GG["g"][:], start=True, stop=True), reads=[CS.b, GG["g"].b], writes=[gps.b])
            S.op("pe", lambda e: e.matmul(gps[:, 128:152], CS[0:64, 4, :], GG["g"][:], start=True, stop=True), reads=[CS.b, GG["g"].b], writes=[gps.b])
            S.op("act", lambda e: e.activation(out=GG["edec"][:], in_=gps[0:64, 64:88], func=AF.Exp), reads=[gps.b], writes=[GG["edec"].b])
            S.op("act", lambda e: e.activation(out=GG["edlm"][:], in_=gps[0:64, 96:120], func=AF.Exp), reads=[gps.b], writes=[GG["edlm"].b])
            S.op("act", lambda e: e.activation(out=GG["edl"][:], in_=gps[:, 128:152], func=AF.Exp), reads=[gps.b], writes=[GG["edl"].b])
            S.op("dve", lambda e: e.tensor_tensor(out=GG["bedec"][:], in0=GG["beta"][:], in1=GG["edec"][:], op=ALU.mult),
                 reads=[GG["beta"].b, GG["edec"].b], writes=[GG["bedec"].b])
            for cg in range(8 // NCG):
                units = []
                for ci in range(NCG):
                    c = cg * NCG + ci
                    for h in range(HPC):
                        u = ci * HPC + h
                        units.append(dict(h=h, col=3 * c + h, cs=slice(64 * c, 64 * c + 64), w=HW[u], G=GG, ps=PS[2 + u]))
                def psb(u):
                    return u["ps"][:].bitcast(BF16)

                s1 = [
                    lambda u: S.op("dve", lambda e: e.tensor_scalar(out=u["w"]["Gb"][:], in0=CS[0:64, 4, 0:64], scalar1=u["G"]["g"][:, u["col"]:u["col"] + 1], scalar2=None, op0=ALU.mult),
                                   reads=[CS.b, u["G"]["g"].b], writes=[u["w"]["Gb"].b]),
                    lambda u: S.op("pe", lambda e: e.matmul(u["ps"][0:64, 0:64], U_, u["w"]["Gb"][:], start=True, stop=False), reads=[CS.b, u["w"]["Gb"].b], writes=[u["ps"].b]),
                    lambda u: S.op("pe", lambda e: e.matmul(u["ps"][0:64, 0:64], u["w"]["Gb"][:], nU_, start=False, stop=True), reads=[CS.b, u["w"]["Gb"].b], writes=[u["ps"].b]),
                    lambda u: S.op("pe", lambda e: e.matmul(u["ps"][0:64, 64:128], u["w"]["Gb"][:], U_, start=True, stop=False), reads=[CS.b, u["w"]["Gb"].b], writes=[u["ps"].b]),
                    lambda u: S.op("pe", lambda e: e.matmul(u["ps"][0:64, 64:128], nU_, u["w"]["Gb"][:], start=False, stop=True), reads=[CS.b, u["w"]["Gb"].b], writes=[u["ps"].b]),
                    lambda u: S.op("pe", lambda e: e.transpose(psb(u)[0:64, 256:384], kn[u["h"]][:, u["cs"]], identb), reads=[kn[u["h"]].b, CSb.b], writes=[u["ps"].b]),
                    lambda u: S.op("pe", lambda e: e.transpose(psb(u)[0:64, 384:512], vT[u["h"]][:, u["cs"]], identb), reads=[vT[u["h"]].b, CSb.b], writes=[u["ps"].b]),
                    lambda u: S.op("pe", lambda e: e.matmul(u["ps"][0:64, 256:320], kn[u["h"]][:, u["cs"]], kn[u["h"]][:, u["cs"]], start=True, stop=True), reads=[kn[u["h"]].b], writes=[u["ps"].b]),
                    lambda u: S.op("pe", lambda e: e.matmul(u["ps"][0:64, 320:384], kn[u["h"]][:, u["cs"]], qn[u["h"]][:, u["cs"]], start=True, stop=True), reads=[kn[u["h"]].b, qn[u["h"]].b], writes=[u["ps"].b]),
                    lambda u: S.op("dve", lambda e: e.tensor_tensor(out=u["w"]["gs"][:], in0=u["ps"][0:64, 0:64], in1=CS[0:64, 5, 0:64], op=ALU.add),
                                   reads=[u["ps"].b, CS.b], writes=[u["w"]["gs"].b]),
                    lambda u: S.op("dve", lambda e: e.tensor_tensor(out=u["w"]["gT"][:], in0=u["ps"][0:64, 64:128], in1=CS[0:64, 6, 0:64], op=ALU.add),
                                   reads=[u["ps"].b, CS.b], writes=[u["w"]["gT"].b]),
                    lambda u: S.op("act", lambda e: e.activation(out=u["w"]["gs"][:], in_=u["w"]["gs"][:], func=AF.Exp), reads=[u["w"]["gs"].b], writes=[u["w"]["gs"].b]),
                    lambda u: S.op("act", lambda e: e.activation(out=u["w"]["gT"][:], in_=u["w"]["gT"][:], func=AF.Exp), reads=[u["w"]["gT"].b], writes=[u["w"]["gT"].b]),
                    lambda u: S.op("dve", lambda e: e.tensor_scalar(out=u["w"]["kbe"][:], in0=psb(u)[0:64, 256:384], scalar1=u["G"]["bedec"][:, u["col"]:u["col"] + 1], scalar2=None, op0=ALU.mult),
                                   reads=[u["ps"].b, u["G"]["bedec"].b], writes=[u["w"]["kbe"].b]),
                    lambda u: S.op("dve", lambda e: e.tensor_scalar(out=u["w"]["ktail"][:], in0=psb(u)[0:64, 256:384], scalar1=u["G"]["edlm"][:, u["col"]:u["col"] + 1], scalar2=None, op0=ALU.mult),
                                   reads=[u["ps"].b, u["G"]["edlm"].b], writes=[u["w"]["ktail"].b]),
                    lambda u: S.op("dve", lambda e: e.tensor_scalar(out=u["w"]["bv"][:], in0=psb(u)[0:64, 384:512], scalar1=u["G"]["beta"][:, u["col"]:u["col"] + 1], scalar2=None, op0=ALU.mult),
                                   reads=[u["ps"].b, u["G"]["beta"].b], writes=[u["w"]["bv"].b]),
                    lambda u: S.op("dve", lambda e: e.scalar_tensor_tensor(out=u["w"]["N"][:], in0=u["ps"][0:64, 256:320], scalar=u["G"]["beta"][:, u["col"]:u["col"] + 1], in1=u["w"]["gs"][:],
                                                                         op0=ALU.mult, op1=ALU.mult),
                                   reads=[u["ps"].b, u["G"]["beta"].b, u["w"]["gs"].b], writes=[u["w"]["N"].b]),
                    lambda u: S.op("dve", lambda e: e.tensor_tensor(out=u["w"]["qkT"][:], in0=u["ps"][0:64, 320:384], in1=u["w"]["gT"][:], op=ALU.mult),
                                   reads=[u["ps"].b, u["w"]["gT"].b], writes=[u["w"]["qkT"].b]),
                    lambda u: S.op("dve", lambda e: e.tensor_scalar(out=u["w"]["Lm"][:], in0=CS[0:64, 4, :], scalar1=u["G"]["edec"][:, u["col"]:u["col"] + 1], scalar2=None, op0=ALU.mult),
                                   reads=[CS.b, u["G"]["edec"].b], writes=[u["w"]["Lm"].b]),
                    lambda u: S.op("pe", lambda e: e.matmul(u["ps"][:, 384:448], u["w"]["Lm"][:], ident[0:64, 0:64], start=True, stop=True), reads=[u["w"]["Lm"].b, CS.b], writes=[u["ps"].b]),
                    lambda u: S.op("pe", lambda e: e.transpose(u["ps"][0:64, 448:512], u["w"]["N"][:], ident[0:64, 0:64]), reads=[u["w"]["N"].b, CS.b], writes=[u["ps"].b]),
                    lambda u: S.op("dve", lambda e: e.tensor_tensor(out=u["w"]["qd"][:], in0=qn[u["h"]][:, u["cs"]], in1=u["ps"][:, 384:448], op=ALU.mult),
                                   reads=[qn[u["h"]].b, u["ps"].b], writes=[u["w"]["qd"].b]),
                    lambda u: S.op("act", lambda e: e.copy(u["w"]["P"][0][:], u["ps"][0:64, 448:512]), reads=[u["ps"].b], writes=[u["w"]["P"][0].b]),
                    lambda u: S.op("dve", lambda e: e.tensor_tensor(out=u["w"]["X"][:], in0=ident[0:64, 0:64], in1=u["ps"][0:64, 448:512], op=ALU.subtract),
                                   reads=[u["ps"].b, CS.b], writes=[u["w"]["X"].b]),
                ]
                for st in s1:
                    for u in units:
                        st(u)
                    tick()
                for lvl in range(1, 6):
                    ci_ = (lvl - 1) % 2
                    for u in units:
                        w = u["w"]
                        u["Pc"] = w["P"][ci_]
                        u["Qc"] = w["N"] if lvl == 1 else w["Q"][ci_]
                        u["Pn"] = w["P"][1 - ci_]
                        u["Qn"] = w["Q"][1 - ci_]
                    s2 = []
                    if lvl < 5:
                        s2.append(lambda u: S.op("pe", lambda e: e.matmul(u["ps"][0:64, 0:64], u["Qc"][:], u["Pc"][:], start=True, stop=True), reads=[u["Pc"].b, u["Qc"].b], writes=[u["ps"].b]))
                    s2.append(lambda u: S.op("pe", lambda e: e.matmul(u["ps"][0:64, 64:128], u["Pc"][:], u["Qc"][:], start=True, stop=True), reads=[u["Pc"].b, u["Qc"].b], writes=[u["ps"].b]))
                    if lvl < 5:
                        s2.append(lambda u: S.op("act", lambda e: e.copy(u["Pn"][:], u["ps"][0:64, 0:64]), reads=[u["ps"].b], writes=[u["Pn"].b]))
                    s2.append(lambda u: S.op("dve", lambda e: e.tensor_copy(u["Qn"][:], u["ps"][0:64, 64:128]), reads=[u["ps"].b], writes=[u["Qn"].b]))
                    s2.append(lambda u: S.op("pe", lambda e: e.matmul(u["ps"][0:64, 128:192], u["Qn"][:], u["w"]["X"][:], start=True, stop=True), reads=[u["Qn"].b, u["w"]["X"].b], writes=[u["ps"].b]))
                    s2.append(lambda u: S.op("dve", lambda e: e.tensor_tensor(out=u["w"]["X"][:], in0=u["w"]["X"][:], in1=u["ps"][0:64, 128:192], op=ALU.add),
                                             reads=[u["ps"].b, u["w"]["X"].b], writes=[u["w"]["X"].b]))
                    for st in s2:
                        for u in units:
                            st(u)
                        tick()
                s3a = [
                    lambda u: S.op("act", lambda e: e.copy(u["w"]["Xb"][:], u["w"]["X"][:]), reads=[u["w"]["X"].b], writes=[u["w"]["Xb"].b]),
                    lambda u: S.op("pe", lambda e: e.matmul(u["ps"][:, 0:64], u["w"]["kbe"][:], u["w"]["Xb"][:], start=True, stop=True), reads=[u["w"]["kbe"].b, u["w"]["Xb"].b], writes=[u["ps"].b]),
                    lambda u: S.op("act", lambda e: e.mul(u["w"]["nwT"][:], u["ps"][:, 0:64], -1.0), reads=[u["ps"].b], writes=[u["w"]["nwT"].b]),
                ]
                for st in s3a:
                    for u in units:
                        st(u)
                    tick()
                s3b = [
                    lambda u: S.op("pe", lambda e: e.matmul(u["ps"][0:64, 128:256], u["w"]["Xb"][:], u["w"]["bv"][:], start=True, stop=False), reads=[u["w"]["Xb"].b, u["w"]["bv"].b], writes=[u["ps"].b]),
                    lambda u: S.op("pe", lambda e: e.matmul(u["ps"][0:64, 128:256], u["w"]["nwT"][:], Sb[u["h"]][:], start=False, stop=True), reads=[u["w"]["nwT"].b, Sb[u["h"]].b], writes=[u["ps"].b]),
                    lambda u: S.op("act", lambda e: e.copy(u["w"]["vn"][:], u["ps"][0:64, 128:256]), reads=[u["ps"].b], writes=[u["w"]["vn"].b]),
                    lambda u: S.op("pe", lambda e: e.matmul(u["ps"][0:64, 256:384], u["w"]["qd"][:], Sb[u["h"]][:], start=True, stop=False), reads=[u["w"]["qd"].b, Sb[u["h"]].b], writes=[u["ps"].b]),
                    lambda u: S.op("pe", lambda e: e.matmul(u["ps"][0:64, 256:384], u["w"]["qkT"][:], u["w"]["vn"][:], start=False, stop=True), reads=[u["w"]["qkT"].b, u["w"]["vn"].b], writes=[u["ps"].b]),
                    lambda u: S.op("pe", lambda e: e.matmul(u["ps"][:, 384:512], u["w"]["ktail"][:], u["w"]["vn"][:], start=True, stop=True), reads=[u["w"]["ktail"].b, u["w"]["vn"].b], writes=[u["ps"].b]),
                    lambda u: S.op("dve", lambda e: e.scalar_tensor_tensor(out=Sst[u["h"]][:], in0=Sst[u["h"]][:], scalar=u["G"]["edl"][:, u["col"]:u["col"] + 1], in1=u["ps"][:, 384:512],
                                                                         op0=ALU.mult, op1=ALU.add),
                                   reads=[Sst[u["h"]].b, u["G"]["edl"].b, u["ps"].b], writes=[Sst[u["h"]].b]),
                    lambda u: S.op("act", lambda e: e.copy(Sb[u["h"]][:], Sst[u["h"]][:]), reads=[Sst[u["h"]].b], writes=[Sb[u["h"]].b]),
                ]
                s3c = [
                    lambda u: S.op("act", lambda e: e.activation(out=u["w"]["junk"][:], in_=u["ps"][0:64, 256:384], func=AF.Square, accum_out=u["w"]["ssq"][:]),
                                   reads=[u["ps"].b], writes=[u["w"]["junk"].b, u["w"]["ssq"].b]),
                    lambda u: S.op("act", lambda e: e.activation(out=u["w"]["rt"][:], in_=u["w"]["ssq"][:], func=AF.Sqrt, bias=RMS_EPS, scale=1.0 / 128.0),
                                   reads=[u["w"]["ssq"].b], writes=[u["w"]["rt"].b]),
                    lambda u: S.op("dve", lambda e: e.reciprocal(u["w"]["rstd"][:], u["w"]["rt"][:]), reads=[u["w"]["rt"].b], writes=[u["w"]["rstd"].b]),
                    lambda u: S.op("dve", lambda e: e.tensor_scalar(out=u["w"]["Lm"][:], in0=u["ps"][0:64, 256:384], scalar1=u["w"]["rstd"][:, 0:1], scalar2=None, op0=ALU.mult),
                                   reads=[u["ps"].b, u["w"]["rstd"].b], writes=[u["w"]["Lm"].b]),
                    lambda u: S.op("pe", lambda e: e.transpose(u["ps"][:, 0:64], u["w"]["Lm"][:], ident[0:64, 0:64]), reads=[u["w"]["Lm"].b, CS.b], writes=[u["ps"].b]),
                    lambda u: S.op("dve", lambda e: e.scalar_tensor_tensor(out=yo[u["h"]][:, u["cs"]], in0=u["ps"][:, 0:64], scalar=HP[:, 8:9], in1=zs[u["h"]][:, u["cs"]],
                                                                         op0=ALU.mult, op1=ALU.mult),
                                   reads=[u["ps"].b, HP.b, zs[u["h"]].b], writes=[yo[u["h"]].b]),
                ]
                for ci in range(NCG):
                    us = units[ci * HPC:(ci + 1) * HPC]
                    for st in s3b:
                        for u in us:
                            st(u)
                        tick()
                for st in s3c:
                    for u in units:
                        st(u)
                    tick()

            drain()
            for h in range(HPC):
                S.dma("sp", yT[128 * h:128 * h + 128, sb * 512:sb * 512 + 512], yo[h][:], reads=[yo[h].b])
        S.barrier()


def x_to_xT(x2d):
    L = x2d.shape[0]
    TR = min(L, 1024)
    return np.ascontiguousarray(x2d.reshape(L // TR, TR, D_MODEL).transpose(0, 2, 1))


def prep_gdn(c, layer, inp):
    hs = [HPC * c + i for i in range(HPC)]
    w_in = inp["w_in"][layer]
    cols = []
    for blk in range(4):
        for h in hs:
            cols.append(np.arange(blk * GDN_WIDTH + h * 128, blk * GDN_WIDTH + (h + 1) * 128))
    cols.append(np.array([4 * GDN_WIDTH + h for h in hs]))
    cols.append(np.array([4 * GDN_WIDTH + GDN_HEADS + h for h in hs]))
    cols = np.concatenate(cols)
    wg = np.ascontiguousarray(w_in[:, cols])
    cw = inp["gdn_conv_w"][layer]
    convw = np.zeros((128, 36), np.float32)
    for blk in range(3):
        for i, h in enumerate(hs):
            ct = blk * 3 + i
            ch = blk * GDN_WIDTH + h * 128 + np.arange(128)
            convw[:, 4 * ct:4 * ct + 4] = cw[:, ch].T
    hp = np.zeros((128, 64), np.float32)
    hp[:, 0:3] = inp["gdn_a_log"][layer][hs][None, :]
    hp[:, 3:6] = inp["gdn_dt_bias"][layer][hs][None, :]
    hp[:, 8] = inp["gdn_norm_w"][layer]
    hp[:, 16:40] = np.tile(inp["gdn_dt_bias"][layer][hs], 8)[None, :]
    hp[:, 40:64] = np.tile(inp["gdn_a_log"][layer][hs], 8)[None, :]
    return {"wg": wg, "convw": convw, "hp": hp, "cst": gdn_consts()}


S5C = 256


def s5_consts():
    c = np.zeros((128, 5, 256), np.float32)
    c[:, 0, :128] = np.eye(128)
    k = np.arange(128)
    sw = np.zeros((128, 128), np.float32)
    sw[k, (k + 64) % 128] = 1.0
    c[:, 1, :128] = sw
    c[:, 2, :] = np.arange(256)[None, :]
    g = np.arange(128) // 16
    c[:, 3, :8] = (g[:, None] == np.arange(8)[None, :])
    c[:64, 3, 8] = 1.0
    c[64:, 3, 8] = -1.0
    c[:, 3, 9] = -1.0
    return c.reshape(128, 5 * 256)


def prep_s5(c, layer, inp):
    gs = np.arange(GPC * c, GPC * c + GPC)
    w_in = inp["w_in"][layer]
    c_u = 4 * GDN_WIDTH + 2 * GDN_HEADS
    wu = np.ascontiguousarray(w_in[:, c_u + 128 * c:c_u + 128 * c + 128])
    lre = inp["s5_lambda_re"][layer][gs]
    lim = inp["s5_lambda_im"][layer][gs]
    ldt = inp["s5_log_dt"][layer][gs]
    bre = inp["s5_b_re"][layer][gs]
    bim = inp["s5_b_im"][layer][gs]
    cre = inp["s5_c_re"][layer][gs]
    cim = inp["s5_c_im"][layer][gs]
    pr = np.zeros((128, 8, 64), np.float32)
    pr[:, 0, :] = np.repeat(lre, 16, axis=0)
    pr[:, 1, :] = np.repeat(lim, 16, axis=0)
    pr[:, 2, :] = bre.transpose(0, 2, 1).reshape(128, 64)
    pr[:, 3, :] = bim.transpose(0, 2, 1).reshape(128, 64)
    pr[:, 4, 0] = np.repeat(ldt, 16)
    pr[:, 4, 1] = inp["s5_d"][layer][128 * c:128 * c + 128]
    pc = np.zeros((128, 4, 128), np.float32)
    cTre = cre.transpose(2, 0, 1).reshape(64, 128)
    cTim = cim.transpose(2, 0, 1).reshape(64, 128)
    pc[:64, 0, :] = cTre
    pc[64:, 0, :] = cTim
    pc[:64, 1, :] = cTim
    pc[64:, 1, :] = cTre
    pc[:, 2, 0:8] = np.tile(lre.T, (2, 1))
    pc[:, 2, 8:16] = np.tile(lim.T, (2, 1))
    pc[:, 2, 16:24] = ldt[None, :]
    return {"wu": wu, "pr": pr.reshape(128, 512), "pc": pc.reshape(128, 512), "cst": s5_consts()}


def emit_s5(nc, S, L, x_bf16, xT, wu, pr_d, pc_d, cst, yT):
    NR = max(1, L // 1024)
    TR = min(L, 1024)
    NSB = L // 512
    xq = "sp" if x_bf16 else "pool"

    with ExitStack() as es:
        C = Ctx(nc, es, S)
        W = C.sb([128, KC, 128], BF16, "W")
        XT = [C.sb([128, 8, 512], BF16, "XT") for _ in range(8)]
        PR = C.sb([128, 8, 64], F32, "PR")
        PC = C.sb([128, 4, 128], F32, "PC")
        CS = C.sb([128, 5, 256], F32, "CS")
        PS = C.psum_banks(8)
        ident = CS[:, 0, 0:128]
        swap = CS[:, 1, 0:128]
        iota = CS[:, 2, :]

        wv = wu.rearrange("(kc p) n -> p kc n", p=128)
        S.dma("pool", W[:], wv, writes=[W.b])
        S.dma("sp", PR[:].rearrange("p a b -> p (a b)"), pr_d, writes=[PR.b])
        S.dma("sp", PC[:].rearrange("p a b -> p (a b)"), pc_d, writes=[PC.b])
        S.dma("sp", CS[:].rearrange("p a b -> p (a b)"), cst, writes=[CS.b])

        n_tmp = [0]

        def tmp(shape, dt=F32):
            n_tmp[0] += 1
            return C.sb(shape, dt, "tmp")

        def dve(fn, reads, writes):
            return S.op("dve", fn, reads=[t.b for t in reads], writes=[t.b for t in writes])

        def act(fn, reads, writes):
            return S.op("act", fn, reads=[t.b for t in reads], writes=[t.b for t in writes])

        sin_tmp = {}

        def sin_of(dst, ang, shift):
            key = tuple(dst.shape_)
            if key not in sin_tmp:
                sin_tmp[key] = (tmp(list(key)), tmp(list(key)))
            k, r = sin_tmp[key]
            dve(lambda e: e.tensor_scalar(out=k[:], in0=ang[:], scalar1=shift, scalar2=1.0 / TWO_PI, op0=ALU.add, op1=ALU.mult), [ang], [k])
            dve(lambda e: e.tensor_scalar(out=k[:], in0=k[:], scalar1=MAGIC, scalar2=MAGIC, op0=ALU.add, op1=ALU.subtract), [k], [k])
            dve(lambda e: e.scalar_tensor_tensor(out=r[:], in0=k[:], scalar=-TWO_PI, in1=ang[:], op0=ALU.mult, op1=ALU.add), [k, ang], [r])
            dve(lambda e: e.tensor_scalar(out=r[:], in0=r[:], scalar1=shift, scalar2=3.1415925, op0=ALU.add, op1=ALU.min), [r], [r])
            dve(lambda e: e.tensor_scalar(out=r[:], in0=r[:], scalar1=-3.1415925, scalar2=None, op0=ALU.max), [r], [r])
            act(lambda e: e.activation(out=dst[:], in_=r[:], func=AF.Sin), [r], [dst])

        def dst_shape(t):
            return t.shape_

        def mk(shape, dt=F32):
            t = tmp(shape, dt)
            t.shape_ = shape
            return t

        dtc = mk([128, 1])
        act(lambda e: e.activation(out=dtc[:], in_=PR[:, 4, 0:1], func=AF.Exp), [PR], [dtc])
        lrd = mk([128, 64]); lid = mk([128, 64]); mag = mk([128, 64]); sn = mk([128, 64]); cs_ = mk([128, 64])
        dve(lambda e: e.tensor_scalar(out=lrd[:], in0=PR[:, 0, :], scalar1=dtc[:, 0:1], scalar2=None, op0=ALU.mult), [PR, dtc], [lrd])
        dve(lambda e: e.tensor_scalar(out=lid[:], in0=PR[:, 1, :], scalar1=dtc[:, 0:1], scalar2=None, op0=ALU.mult), [PR, dtc], [lid])
        act(lambda e: e.activation(out=mag[:], in_=lrd[:], func=AF.Exp), [lrd], [mag])
        sin_of(sn, lid, 0.0)
        sin_of(cs_, lid, float(np.pi / 2))
        nr = mk([128, 64]); ni = mk([128, 64]); den = mk([128, 64]); t1 = mk([128, 64]); t2 = mk([128, 64])
        cre = mk([128, 64]); cim = mk([128, 64])
        dve(lambda e: e.tensor_tensor(out=nr[:], in0=mag[:], in1=cs_[:], op=ALU.mult), [mag, cs_], [nr])
        dve(lambda e: e.tensor_scalar(out=nr[:], in0=nr[:], scalar1=-1.0, scalar2=None, op0=ALU.add), [nr], [nr])
        dve(lambda e: e.tensor_tensor(out=ni[:], in0=mag[:], in1=sn[:], op=ALU.mult), [mag, sn], [ni])
        dve(lambda e: e.tensor_tensor(out=den[:], in0=PR[:, 0, :], in1=PR[:, 0, :], op=ALU.mult), [PR], [den])
        dve(lambda e: e.tensor_tensor(out=t1[:], in0=PR[:, 1, :], in1=PR[:, 1, :], op=ALU.mult), [PR], [t1])
        dve(lambda e: e.tensor_tensor(out=den[:], in0=den[:], in1=t1[:], op=ALU.add), [den, t1], [den])
        dve(lambda e: e.reciprocal(den[:], den[:]), [den], [den])
        dve(lambda e: e.tensor_tensor(out=t1[:], in0=nr[:], in1=PR[:, 0, :], op=ALU.mult), [nr, PR], [t1])
        dve(lambda e: e.tensor_tensor(out=t2[:], in0=ni[:], in1=PR[:, 1, :], op=ALU.mult), [ni, PR], [t2])
        dve(lambda e: e.tensor_tensor(out=t1[:], in0=t1[:], in1=t2[:], op=ALU.add), [t1, t2], [t1])
        dve(lambda e: e.tensor_tensor(out=cre[:], in0=t1[:], in1=den[:], op=ALU.mult), [t1, den], [cre])
        dve(lambda e: e.tensor_tensor(out=t1[:], in0=ni[:], in1=PR[:, 0, :], op=ALU.mult), [ni, PR], [t1])
        dve(lambda e: e.tensor_tensor(out=t2[:], in0=nr[:], in1=PR[:, 1, :], op=ALU.mult), [nr, PR], [t2])
        dve(lambda e: e.tensor_tensor(out=t1[:], in0=t1[:], in1=t2[:], op=ALU.subtract), [t1, t2], [t1])
        dve(lambda e: e.tensor_tensor(out=cim[:], in0=t1[:], in1=den[:], op=ALU.mult), [t1, den], [cim])
        BB1 = mk([128, 128]); BB2 = mk([128, 128])
        dve(lambda e: e.tensor_tensor(out=t1[:], in0=cre[:], in1=PR[:, 2, :], op=ALU.mult), [cre, PR], [t1])
        dve(lambda e: e.tensor_tensor(out=t2[:], in0=cim[:], in1=PR[:, 3, :], op=ALU.mult), [cim, PR], [t2])
        dve(lambda e: e.tensor_tensor(out=BB1[:, 0:64], in0=t1[:], in1=t2[:], op=ALU.subtract), [t1, t2], [BB1])
        dve(lambda e: e.tensor_tensor(out=t1[:], in0=cre[:], in1=PR[:, 3, :], op=ALU.mult), [cre, PR], [t1])
        dve(lambda e: e.tensor_tensor(out=t2[:], in0=cim[:], in1=PR[:, 2, :], op=ALU.mult), [cim, PR], [t2])
        dve(lambda e: e.tensor_tensor(out=BB1[:, 64:128], in0=t1[:], in1=t2[:], op=ALU.add), [t1, t2], [BB1])
        dve(lambda e: e.tensor_copy(BB2[:, 0:64], BB1[:, 64:128]), [BB1], [BB2])
        dve(lambda e: e.tensor_scalar(out=BB2[:, 64:128], in0=BB1[:, 0:64], scalar1=-1.0, scalar2=None, op0=ALU.mult), [BB1], [BB2])
        Bm1 = mk([128, GPC, 128], BF16); Bm2 = mk([128, GPC, 128], BF16)
        for g in range(GPC):
            dve(lambda e, g=g: e.tensor_scalar(out=Bm1[:, g, :], in0=BB1[:], scalar1=CS[:, 3, g:g + 1], scalar2=None, op0=ALU.mult), [BB1, CS], [Bm1])
            dve(lambda e, g=g: e.tensor_scalar(out=Bm2[:, g, :], in0=BB2[:], scalar1=CS[:, 3, g:g + 1], scalar2=None, op0=ALU.mult), [BB2, CS], [Bm2])
        W1 = mk([128, GPC, 128], BF16); W2 = mk([128, GPC, 128], BF16)
        dve(lambda e: e.memset(W1[:].rearrange("p a b -> p (a b)"), 0.0), [], [W1])
        dve(lambda e: e.memset(W2[:].rearrange("p a b -> p (a b)"), 0.0), [], [W2])
        for g in range(GPC):
            sl = slice(16 * g, 16 * g + 16)
            dve(lambda e, g=g, sl=sl: e.tensor_scalar(out=W1[:, g, sl], in0=PC[:, 0, sl], scalar1=CS[:, 3, 8:9], scalar2=None, op0=ALU.mult), [PC, CS], [W1])
            dve(lambda e, g=g, sl=sl: e.tensor_scalar(out=W2[:, g, sl], in0=PC[:, 1, sl], scalar1=CS[:, 3, 9:10], scalar2=None, op0=ALU.mult), [PC, CS], [W2])
        dt2 = mk([128, 8]); th = mk([128, 8]); rho = mk([128, 8]); lr2 = mk([128, 8])
        act(lambda e: e.activation(out=dt2[:], in_=PC[:, 2, 16:24], func=AF.Exp), [PC], [dt2])
        dve(lambda e: e.tensor_tensor(out=th[:], in0=PC[:, 2, 8:16], in1=dt2[:], op=ALU.mult), [PC, dt2], [th])
        dve(lambda e: e.tensor_tensor(out=lr2[:], in0=PC[:, 2, 0:8], in1=dt2[:], op=ALU.mult), [PC, dt2], [lr2])
        act(lambda e: e.activation(out=rho[:], in_=lr2[:], func=AF.Exp), [lr2], [rho])
        C2 = mk([128, GPC, S5C]); S2 = mk([128, GPC, S5C])
        ang = mk([128, S5C]); sg = mk([128, S5C]); cg = mk([128, S5C])
        for g in range(GPC):
            dve(lambda e, g=g: e.tensor_scalar(out=ang[:], in0=iota, scalar1=th[:, g:g + 1], scalar2=None, op0=ALU.mult), [CS, th], [ang])
            sin_of(sg, ang, 0.0)
            sin_of(cg, ang, float(np.pi / 2))
            dve(lambda e, g=g, sg=sg: e.tensor_copy(S2[:, g, :], sg[:]), [sg], [S2])
            dve(lambda e, g=g, cg=cg: e.tensor_copy(C2[:, g, :], cg[:]), [cg], [C2])
        angc = mk([128, 8]); crr = mk([128, 8]); srr = mk([128, 8])
        dve(lambda e: e.tensor_scalar(out=angc[:], in0=th[:], scalar1=float(S5C), scalar2=None, op0=ALU.mult), [th], [angc])
        sin_of(srr, angc, 0.0)
        sin_of(crr, angc, float(np.pi / 2))
        dve(lambda e: e.tensor_scalar(out=srr[:], in0=srr[:], scalar1=CS[:, 3, 8:9], scalar2=None, op0=ALU.mult), [srr, CS], [srr])
        ROT = mk([128, GPC, 128])
        for g in range(GPC):
            dve(lambda e, g=g: e.tensor_scalar(out=ROT[:, g, :], in0=ident, scalar1=crr[:, g:g + 1], scalar2=None, op0=ALU.mult), [CS, crr], [ROT])
            dve(lambda e, g=g: e.scalar_tensor_tensor(out=ROT[:, g, :], in0=swap, scalar=srr[:, g:g + 1], in1=ROT[:, g, :], op0=ALU.mult, op1=ALU.add),
                [CS, srr, ROT], [ROT])

        uT = [mk([128, 512]) for _ in range(2)]
        uTb = [mk([128, 512], BF16) for _ in range(2)]
        mbuf = [mk([128, S5C]) for _ in range(2)]
        tbuf = [mk([128, S5C]) for _ in range(2)]
        zeta = [mk([128, S5C]) for _ in range(2)]
        Zc = [mk([128, S5C], BF16) for _ in range(2)]
        Zs = [mk([128, S5C], BF16) for _ in range(2)]
        zl = [mk([128, 1]) for _ in range(GPC)]
        zi = [mk([128, 1]) for _ in range(GPC)]
        yf = [mk([128, S5C]) for _ in range(2)]
        yo = [mk([128, S5C], BF16) for _ in range(2)]
        gl = [(mk([128, S5C]), mk([128, S5C])) for _ in range(2)]
        proj_banks = BankPool(PS[0:2])
        p_banks = BankPool(PS[2:6])
        y_banks = BankPool(PS[6:8])
        xv = xT.rearrange("r (kc p) t -> r p kc t", p=128)
        rot = 0
        nchunk = 0
        for sb in range(NSB):
            r = (sb * 512) // TR
            t0 = (sb * 512) % TR
            xs = XT[4 * (sb % 2):4 * (sb % 2) + 4]
            for j in range(4):
                S.dma(xq, xs[j][:], xv[r, :, 8 * j:8 * j + 8, t0:t0 + 512], writes=[xs[j].b])
            ps = proj_banks.get()
            for kc in range(KC):
                S.op("pe", lambda e, kc=kc: e.matmul(ps[:, :], W[:, kc, :], xs[kc // 8][:, kc % 8, :], start=(kc == 0), stop=(kc == KC - 1)),
                     reads=[W.b, xs[kc // 8].b], writes=[ps.b])
            u_, ub_ = uT[sb % 2], uTb[sb % 2]
            act(lambda e: e.copy(u_[:], ps[:, :]), [ps], [u_])
            dve(lambda e: e.tensor_copy(ub_[:], ps[:, :]), [ps], [ub_])
            for cc in range(512 // S5C):
                csl = slice(cc * S5C, (cc + 1) * S5C)
                yps = y_banks.get()
                for g in range(GPC):
                    pb = p_banks.get()
                    m_, t_, z_, zc_, zs_ = mbuf[rot], tbuf[rot], zeta[rot], Zc[rot], Zs[rot]
                    rot ^= 1
                    S.op("pe", lambda e, g=g: e.matmul(pb[:, 0:S5C], Bm1[:, g, :], ub_[:, csl], start=True, stop=True), reads=[Bm1.b, ub_.b], writes=[pb.b])
                    S.op("pe", lambda e, g=g: e.matmul(pb[:, S5C:2 * S5C], Bm2[:, g, :], ub_[:, csl], start=True, stop=True), reads=[Bm2.b, ub_.b], writes=[pb.b])
                    dve(lambda e, g=g: e.tensor_tensor(out=m_[:], in0=pb[:, 0:S5C], in1=C2[:, g, :], op=ALU.mult), [pb, C2], [m_])
                    dve(lambda e, g=g: e.tensor_tensor(out=t_[:], in0=pb[:, S5C:2 * S5C], in1=S2[:, g, :], op=ALU.mult), [pb, S2], [t_])
                    dve(lambda e: e.tensor_tensor(out=m_[:], in0=m_[:], in1=t_[:], op=ALU.add), [m_, t_], [m_])
                    if nchunk == 0:
                        dve(lambda e, g=g: e.tensor_tensor_scan(out=z_[:], data0=rho[:, g:g + 1].to_broadcast([128, S5C]), data1=m_[:], initial=0.0,
                                                              op0=ALU.mult, op1=ALU.add), [rho, m_], [z_])
                    else:
                        pr_ = p_banks.get()
                        S.op("pe", lambda e, g=g: e.matmul(pr_[:, 0:1], ROT[:, g, :], zl[g][:], start=True, stop=True), reads=[ROT.b, zl[g].b], writes=[pr_.b])
                        act(lambda e, g=g: e.copy(zi[g][:], pr_[:, 0:1]), [pr_], [zi[g]])
                        dve(lambda e, g=g: e.tensor_tensor_scan(out=z_[:], data0=rho[:, g:g + 1].to_broadcast([128, S5C]), data1=m_[:], initial=zi[g][:, 0:1],
                                                              op0=ALU.mult, op1=ALU.add), [rho, m_, zi[g]], [z_])
                    dve(lambda e, g=g: e.tensor_copy(zl[g][:], z_[:, S5C - 1:S5C]), [z_], [zl[g]])
                    dve(lambda e, g=g: e.tensor_tensor(out=zc_[:], in0=z_[:], in1=C2[:, g, :], op=ALU.mult), [z_, C2], [zc_])
                    dve(lambda e, g=g: e.tensor_tensor(out=zs_[:], in0=z_[:], in1=S2[:, g, :], op=ALU.mult), [z_, S2], [zs_])
                    S.op("pe", lambda e, g=g: e.matmul(yps[:, 0:S5C], W1[:, g, :], zc_[:], start=(g == 0), stop=False), reads=[W1.b, zc_.b], writes=[yps.b])
                    S.op("pe", lambda e, g=g: e.matmul(yps[:, 0:S5C], W2[:, g, :], zs_[:], start=False, stop=(g == GPC - 1)), reads=[W2.b, zs_.b], writes=[yps.b])
                yf_, yo_ = yf[nchunk % 2], yo[nchunk % 2]
                dve(lambda e: e.scalar_tensor_tensor(out=yf_[:], in0=u_[:, csl], scalar=PR[:, 4, 1:2], in1=yps[:, 0:S5C], op0=ALU.mult, op1=ALU.add),
                    [u_, PR, yps], [yf_])
                g1, g2 = gl[nchunk % 2]
                dve(lambda e: e.tensor_tensor(out=g1[:], in0=yf_[:], in1=yf_[:], op=ALU.mult), [yf_], [g1])
                dve(lambda e: e.tensor_scalar(out=g1[:], in0=g1[:], scalar1=0.044715, scalar2=1.0, op0=ALU.mult, op1=ALU.add), [g1], [g1])
                dve(lambda e: e.tensor_tensor(out=g1[:], in0=g1[:], in1=yf_[:], op=ALU.mult), [g1, yf_], [g1])
                act(lambda e: e.activation(out=g2[:], in_=g1[:], func=AF.Sigmoid, scale=float(2.0 * np.sqrt(2.0 / np.pi))), [g1], [g2])
                dve(lambda e: e.tensor_tensor(out=yo_[:], in0=yf_[:], in1=g2[:], op=ALU.mult), [yf_, g2], [yo_])
                S.dma("sp", yT[:, sb * 512 + cc * S5C: sb * 512 + (cc + 1) * S5C], yo_[:], reads=[yo_.b])
                nchunk += 1
        S.barrier()


def build_mixer(L, x_bf16, do_gdn=True, do_s5=True):
    NR = max(1, L // 1024)
    TR = min(L, 1024)
    nc = bass.Bass("TRN2", target_bir_lowering=False)
    xT = nc.dram_tensor("xT", [NR, D_MODEL, TR], BF16 if x_bf16 else F32, kind="ExternalInput").ap()
    wg = nc.dram_tensor("wg", [D_MODEL, GDN_COLS], F32, kind="ExternalInput").ap()
    convw = nc.dram_tensor("convw", [128, 36], F32, kind="ExternalInput").ap()
    hp = nc.dram_tensor("hp", [128, 64], F32, kind="ExternalInput").ap()
    cstg = nc.dram_tensor("cstg", [128, 8 * 128], F32, kind="ExternalInput").ap()
    wu = nc.dram_tensor("wu", [D_MODEL, 128], F32, kind="ExternalInput").ap()
    pr_d = nc.dram_tensor("pr", [128, 512], F32, kind="ExternalInput").ap()
    pc_d = nc.dram_tensor("pc", [128, 512], F32, kind="ExternalInput").ap()
    csts = nc.dram_tensor("csts", [128, 5 * 256], F32, kind="ExternalInput").ap()
    yT = nc.dram_tensor("yT", [512, L], BF16, kind="ExternalOutput").ap()
    with ExitStack() as outer:
        S = Sched(nc, outer)
        if do_gdn:
            emit_gdn(nc, S, L, x_bf16, xT, wg, convw, hp, cstg, yT[0:384, :])
        if do_s5:
            emit_s5(nc, S, L, x_bf16, xT, wu, pr_d, pc_d, csts, yT[384:512, :])
        S.barrier()
    return nc


def prep_mixer(c, layer, inp):
    g = prep_gdn(c, layer, inp)
    s_ = prep_s5(c, layer, inp)
    return {"wg": g["wg"], "convw": g["convw"], "hp": g["hp"], "cstg": g["cst"],
            "wu": s_["wu"], "pr": s_["pr"], "pc": s_["pc"], "csts": s_["cst"]}


TPC = 1024
NT = TPC // 128
BIG = 1.0e30


def b_consts():
    c = np.zeros((128, 4, 128), np.float32)
    c[:, 0, :] = np.eye(128)
    c[:, 1, :] = 1.0
    k = np.arange(128)
    c[:, 2, :] = (k[:, None] < k[None, :])
    c[:, 3, :] = k[None, :]
    return c.reshape(128, 512)


def emit_consts(S, C, cst):
    CS = C.sb([128, 4, 128], F32, "CS")
    CSb = C.sb([128, 3, 128], BF16, "CSb")
    S.dma("sp", CS[:].rearrange("p a b -> p (a b)"), cst, writes=[CS.b])
    for j in range(3):
        S.op("dve", lambda e, j=j: e.tensor_copy(CSb[:, j, :], CS[:, j, :]), reads=[CS.b], writes=[CSb.b])
    return CS, CSb


def emit_proj_res(S, C, PS, lhs_fn, nk, rhs_view, rhs_f32, resid, resid_bufs, H, Hbufs):
    Wn = [C.sb([128, nk, 512], BF16, "Wn") for _ in range(2)]
    xt = [C.sb([128, 512], F32, "xt") for _ in range(3)]
    ht = [C.sb([128, 512], F32, "ht") for _ in range(3)]
    banks = BankPool(PS[0:4])
    q = "pool" if rhs_f32 else "sp"
    cnt = 0
    for n in range(8):
        w = Wn[n % 2]
        for j in range(4):
            ks = slice(j * nk // 4, (j + 1) * nk // 4)
            S.dma(q, w[:, ks, :], rhs_view[:, ks, n * 512:(n + 1) * 512], writes=[w.b])
        for i in range(NT):
            ps = banks.get()
            for k in range(nk):
                ap, b = lhs_fn(k, i)
                S.op("pe", lambda e, ap=ap, k=k: e.matmul(ps[:, :], ap, w[:, k, :], start=(k == 0), stop=(k == nk - 1)),
                     reads=[b, w.b], writes=[ps.b])
            x_, h_ = xt[cnt % 3], ht[cnt % 3]
            cnt += 1
            S.dma("sp", x_[:], resid[i * 128:(i + 1) * 128, n * 512:(n + 1) * 512], reads=[resid_bufs[i]], writes=[x_.b])
            S.op("dve", lambda e: e.scalar_tensor_tensor(out=h_[:], in0=x_[:], scalar=float(DN_ALPHA), in1=ps[:, :], op0=ALU.mult, op1=ALU.add),
                 reads=[x_.b, ps.b], writes=[h_.b])
            S.dma("sp", H[i * 128:(i + 1) * 128, n * 512:(n + 1) * 512], h_[:], reads=[h_.b], writes=[Hbufs[i]])


def emit_ln_tiles(S, C, H, Hbufs, lng, lnb, out_cb, eps=LN_EPS):
    G = C.sb([128, D_MODEL], F32, "lnG")
    Bt = C.sb([128, D_MODEL], F32, "lnB")
    S.dma("sp", G[:], lng, writes=[G.b])
    S.dma("sp", Bt[:], lnb, writes=[Bt.b])
    tiles = [C.sb([128, D_MODEL], F32, "lt") for _ in range(2)]
    junk = C.sb([128, D_MODEL], BF16, "junk")
    s1 = C.sb([128, 1], F32); nm = C.sb([128, 1], F32); s2 = C.sb([128, 1], F32); rt = C.sb([128, 1], F32); rstd = C.sb([128, 1], F32)
    for i in range(NT):
        t = tiles[i % 2]
        S.dma("sp", t[:], H[i * 128:(i + 1) * 128, :], reads=[Hbufs[i]], writes=[t.b])
        S.op("dve", lambda e: e.reduce_sum(out=s1[:], in_=t[:], axis=AX.X), reads=[t.b], writes=[s1.b])
        S.op("dve", lambda e: e.tensor_scalar(out=nm[:], in0=s1[:], scalar1=-1.0 / D_MODEL, scalar2=None, op0=ALU.mult), reads=[s1.b], writes=[nm.b])
        S.op("act", lambda e: e.activation(out=junk[:], in_=t[:], func=AF.Square, bias=nm[:, 0:1], accum_out=s2[:]), reads=[t.b, nm.b], writes=[junk.b, s2.b])
        S.op("act", lambda e: e.activation(out=rt[:], in_=s2[:], func=AF.Sqrt, bias=eps, scale=1.0 / D_MODEL), reads=[s2.b], writes=[rt.b])
        S.op("dve", lambda e: e.reciprocal(rstd[:], rt[:]), reads=[rt.b], writes=[rstd.b])
        S.op("dve", lambda e: e.tensor_scalar(out=t[:], in0=t[:], scalar1=nm[:, 0:1], scalar2=rstd[:, 0:1], op0=ALU.add, op1=ALU.mult),
             reads=[t.b, nm.b, rstd.b], writes=[t.b])
        S.op("dve", lambda e: e.tensor_tensor(out=t[:], in0=t[:], in1=G[:], op=ALU.mult), reads=[t.b, G.b], writes=[t.b])
        S.op("dve", lambda e: e.tensor_tensor(out=t[:], in0=t[:], in1=Bt[:], op=ALU.add), reads=[t.b, Bt.b], writes=[t.b])
        out_cb(i, t)


def emit_transpose_tile(S, banks, src, identb, CSb, dst_fn, eng_alt=[0]):
    for g in range(4):
        ps = banks.get()
        psb = ps[:].bitcast(BF16)
        for j in range(8):
            kc = g * 8 + j
            S.op("pe", lambda e, j=j, kc=kc: e.transpose(psb[:, j * 128:(j + 1) * 128], src[:, kc * 128:(kc + 1) * 128], identb),
                 reads=[src.b, CSb.b], writes=[ps.b])
        ap, b = dst_fn(g)
        en = "act" if (eng_alt[0] % 2 == 0) else "dve"
        eng_alt[0] += 1
        if en == "act":
            S.op("act", lambda e: e.copy(ap, psb[:, 0:1024].rearrange("p (a b) -> p a b", a=8)), reads=[ps.b], writes=[b])
        else:
            S.op("dve", lambda e: e.tensor_copy(ap, psb[:, 0:1024].rearrange("p (a b) -> p a b", a=8)), reads=[ps.b], writes=[b])


def build_t0():
    nc = bass.Bass("TRN2", target_bir_lowering=False)
    x = nc.dram_tensor("x", [TPC, D_MODEL], F32, kind="ExternalInput").ap()
    cst = nc.dram_tensor("cst", [128, 512], F32, kind="ExternalInput").ap()
    xT = nc.dram_tensor("xTo", [D_MODEL, TPC], BF16, kind="ExternalOutput").ap()
    with ExitStack() as outer:
        S = Sched(nc, outer)
        C = Ctx(nc, outer, S)
        PS = C.psum_banks(8)
        CS, CSb = emit_consts(S, C, cst)
        xTs = C.sb([128, KC, TPC], BF16, "xTs")
        xb = [C.sb([128, D_MODEL], BF16, "xb") for _ in range(2)]
        banks = BankPool(PS)
        for i in range(NT):
            S.dma("pool", xb[i % 2][:], x[i * 128:(i + 1) * 128, :], writes=[xb[i % 2].b])
            emit_transpose_tile(S, banks, xb[i % 2], CSb[:, 0, :], CSb,
                                lambda g, i=i: (xTs[:, g * 8:(g + 1) * 8, i * 128:(i + 1) * 128], xTs.b))
        S.dma("sp", xT.rearrange("(kc p) t -> p kc t", p=128), xTs[:], reads=[xTs.b])
        S.barrier()
    return nc


def build_b1():
    nc = bass.Bass("TRN2", target_bir_lowering=False)
    ymix = nc.dram_tensor("ymix", [D_MODEL, TPC], BF16, kind="ExternalInput").ap()
    wo = nc.dram_tensor("wo", [D_MODEL, D_MODEL], F32, kind="ExternalInput").ap()
    wglu = nc.dram_tensor("wglu", [S5_WIDTH, S5_WIDTH], F32, kind="ExternalInput").ap()
    x = nc.dram_tensor("x", [TPC, D_MODEL], F32, kind="ExternalInput").ap()
    lng = nc.dram_tensor("lng", [128, D_MODEL], F32, kind="ExternalInput").ap()
    lnb = nc.dram_tensor("lnb", [128, D_MODEL], F32, kind="ExternalInput").ap()
    wr = nc.dram_tensor("wr", [D_MODEL, 36], F32, kind="ExternalInput").ap()
    rb = nc.dram_tensor("rb", [128, 36], F32, kind="ExternalInput").ap()
    cst = nc.dram_tensor("cst", [128, 512], F32, kind="ExternalInput").ap()
    x1o = nc.dram_tensor("x1", [TPC, D_MODEL], F32, kind="ExternalOutput").ap()
    xg = nc.dram_tensor("xg", [N_EXPERTS, 128, KC, CAP], BF16, kind="ExternalOutput").ap()
    selw = nc.dram_tensor("selw", [128, N_EXPERTS, TPC], BF16, kind="ExternalOutput").ap()
    H = nc.dram_tensor("Hscr", [TPC, D_MODEL], F32, kind="Internal").ap()
    with ExitStack() as outer:
        S = Sched(nc, outer)
        Hb = [Buf(f"H{i}") for i in range(NT)]
        xb_ = [Buf(f"xr{i}") for i in range(NT)]
        with ExitStack() as es:
            C = Ctx(nc, es, S)
            PS = C.psum_banks(8)
            Y = C.sb([128, KC, TPC], BF16, "Y")
            y2 = C.sb([128, 8, TPC], BF16, "y2")
            Wg = C.sb([128, 8, S5_WIDTH], BF16, "Wglu")
            sig = [C.sb([128, 512], F32, "sig") for _ in range(2)]
            yv = ymix.rearrange("(k p) t -> p k t", p=128)
            for j in range(4):
                S.dma("sp", Y[:, 8 * j:8 * j + 8, :], yv[:, 8 * j:8 * j + 8, :], writes=[Y.b])
            S.dma("pool", Wg[:], wglu.rearrange("(r p) n -> p r n", p=128), writes=[Wg.b])
            gb = BankPool(PS[4:8])
            n = 0
            for jc in range(8):
                for th in range(2):
                    ps = gb.get()
                    tsl = slice(th * 512, (th + 1) * 512)
                    for r in range(8):
                        S.op("pe", lambda e, r=r: e.matmul(ps[:, :], Wg[:, r, jc * 128:(jc + 1) * 128], Y[:, 4 * r + 3, tsl], start=(r == 0), stop=(r == 7)),
                             reads=[Wg.b, Y.b], writes=[ps.b])
                    sg = sig[n % 2]
                    n += 1
                    S.op("act", lambda e: e.activation(out=sg[:], in_=ps[:, :], func=AF.Sigmoid), reads=[ps.b], writes=[sg.b])
                    S.op("dve", lambda e: e.tensor_tensor(out=y2[:, jc, tsl], in0=Y[:, 4 * jc + 3, tsl], in1=sg[:], op=ALU.mult),
                         reads=[Y.b, sg.b], writes=[y2.b])

            def lhs_fn(k, i):
                tsl = slice(i * 128, (i + 1) * 128)
                if k % 4 == 3:
                    return y2[:, k // 4, tsl], y2.b
                return Y[:, k, tsl], Y.b

            emit_proj_res(S, C, PS, lhs_fn, KC, wo.rearrange("(k p) n -> p k n", p=128), True, x, xb_, H, Hb)
            S.barrier()
        with ExitStack() as es2:
            C = Ctx(nc, es2, S)
            PS = C.psum_banks(8)
            CS, CSb = emit_consts(S, C, cst)
            identb, onesb, lstr = CSb[:, 0, :], CSb[:, 1, :], CSb[:, 2, :]
            iota = CS[:, 3, :]
            X1b = C.sb([128, NT, D_MODEL], BF16, "X1b")
            X1bb = [Buf(f"x1b{i}") for i in range(NT)]
            M1 = [C.sb([128, 32], F32, "M1") for _ in range(NT)]
            M2 = [C.sb([128, 32], F32, "M2") for _ in range(NT)]
            MAf = [C.sb([128, 32], F32, "MAf") for _ in range(NT)]
            MA = [C.sb([128, 32], BF16, "MA") for _ in range(NT)]
            CWm = [C.sb([128, 32], F32, "CWm") for _ in range(NT)]
            POS = [C.sb([128, 32], F32, "POS") for _ in range(NT)]
            x1ob = [Buf(f"x1o{i}") for i in range(NT)]
            with ExitStack() as es2a:
                Ca = Ctx(nc, es2a, S)
                Wr = Ca.sb([128, KC, 36], BF16, "Wr")
                RB = Ca.sb([128, 36], F32, "RB")
                S.dma("pool", Wr[:], wr.rearrange("(k p) n -> p k n", p=128), writes=[Wr.b])
                S.dma("sp", RB[:], rb, writes=[RB.b])
                x1T = Ca.sb([128, KC, 128], BF16, "x1T")
                lg = Ca.sb([128, 36], F32, "lg")
                sm = {k: Ca.sb([128, 1], F32, k) for k in ("gmax", "ngmax", "se", "grp", "m1", "m2", "d", "ed", "den", "w1", "w2", "cw1", "cw2")}
                ex4 = Ca.sb([128, 4], F32); maskg = Ca.sb([128, 4], F32); pen = Ca.sb([128, 4], F32)
                elm = Ca.sb([128, 32], F32); elm2 = Ca.sb([128, 32], F32)
                tb = BankPool(PS[0:6])
                lb = BankPool(PS[6:8])

                def dv(fn, reads, writes):
                    S.op("dve", fn, reads=[t.b for t in reads], writes=[t.b for t in writes])

                def ac(fn, reads, writes):
                    S.op("act", fn, reads=[t.b for t in reads], writes=[t.b for t in writes])

                def ln_cb(i, t):
                    S.dma("sp", x1o[i * 128:(i + 1) * 128, :], t[:], reads=[t.b], writes=[x1ob[i]])
                    S.op("act", lambda e: e.copy(X1b[:, i, :], t[:]), reads=[t.b], writes=[X1bb[i]])
                    v = View(lambda k: X1b[:, i, k[1]], X1bb[i])
                    emit_transpose_tile(S, tb, v, identb, CSb, lambda g: (x1T[:, g * 8:(g + 1) * 8, :], x1T.b))
                    ps = lb.get()
                    for kc in range(KC):
                        S.op("pe", lambda e, kc=kc: e.matmul(ps[:, 0:36], x1T[:, kc, :], Wr[:, kc, :], start=(kc == 0), stop=(kc == KC - 1)),
                             reads=[x1T.b, Wr.b], writes=[ps.b])
                    dv(lambda e: e.tensor_tensor(out=lg[:], in0=ps[:, 0:36], in1=RB[:], op=ALU.add), [ps, RB], [lg])
                    dv(lambda e: e.reduce_max(out=sm["gmax"][:], in_=lg[:, 0:4], axis=AX.X), [lg], [sm["gmax"]])
                    dv(lambda e: e.tensor_scalar(out=sm["ngmax"][:], in0=sm["gmax"][:], scalar1=-1.0, scalar2=None, op0=ALU.mult), [sm["gmax"]], [sm["ngmax"]])
                    ac(lambda e: e.activation(out=ex4[:], in_=lg[:, 0:4], func=AF.Exp, bias=sm["ngmax"][:, 0:1], accum_out=sm["se"][:]), [lg, sm["ngmax"]], [ex4, sm["se"]])
                    dv(lambda e: e.reciprocal(sm["grp"][:], sm["se"][:]), [sm["se"]], [sm["grp"]])
                    dv(lambda e: e.tensor_scalar(out=maskg[:], in0=lg[:, 0:4], scalar1=sm["gmax"][:, 0:1], scalar2=None, op0=ALU.is_equal), [lg, sm["gmax"]], [maskg])
                    dv(lambda e: e.tensor_scalar(out=pen[:], in0=maskg[:], scalar1=-1.0, scalar2=BIG, op0=ALU.add, op1=ALU.mult), [maskg], [pen])
                    for g in range(4):
                        dv(lambda e, g=g: e.tensor_scalar(out=elm[:, 8 * g:8 * g + 8], in0=lg[:, 4 + 8 * g:12 + 8 * g], scalar1=pen[:, g:g + 1], scalar2=None, op0=ALU.add),
                           [lg, pen], [elm])
                    dv(lambda e: e.reduce_max(out=sm["m1"][:], in_=elm[:], axis=AX.X), [elm], [sm["m1"]])
                    dv(lambda e: e.tensor_scalar(out=M1[i][:], in0=elm[:], scalar1=sm["m1"][:, 0:1], scalar2=None, op0=ALU.is_equal), [elm, sm["m1"]], [M1[i]])
                    dv(lambda e: e.scalar_tensor_tensor(out=elm2[:], in0=M1[i][:], scalar=-BIG, in1=elm[:], op0=ALU.mult, op1=ALU.add), [M1[i], elm], [elm2])
                    dv(lambda e: e.reduce_max(out=sm["m2"][:], in_=elm2[:], axis=AX.X), [elm2], [sm["m2"]])
                    dv(lambda e: e.tensor_scalar(out=M2[i][:], in0=elm2[:], scalar1=sm["m2"][:, 0:1], scalar2=None, op0=ALU.is_equal), [elm2, sm["m2"]], [M2[i]])
                    dv(lambda e: e.tensor_tensor(out=sm["d"][:], in0=sm["m2"][:], in1=sm["m1"][:], op=ALU.subtract), [sm["m2"], sm["m1"]], [sm["d"]])
                    ac(lambda e: e.activation(out=sm["ed"][:], in_=sm["d"][:], func=AF.Exp), [sm["d"]], [sm["ed"]])
                    dv(lambda e: e.tensor_scalar(out=sm["den"][:], in0=sm["ed"][:], scalar1=1.0, scalar2=None, op0=ALU.add), [sm["ed"]], [sm["den"]])
                    dv(lambda e: e.reciprocal(sm["w1"][:], sm["den"][:]), [sm["den"]], [sm["w1"]])
                    dv(lambda e: e.tensor_tensor(out=sm["w2"][:], in0=sm["ed"][:], in1=sm["w1"][:], op=ALU.mult), [sm["ed"], sm["w1"]], [sm["w2"]])
                    dv(lambda e: e.tensor_tensor(out=sm["cw1"][:], in0=sm["w1"][:], in1=sm["grp"][:], op=ALU.mult), [sm["w1"], sm["grp"]], [sm["cw1"]])
                    dv(lambda e: e.tensor_tensor(out=sm["cw2"][:], in0=sm["w2"][:], in1=sm["grp"][:], op=ALU.mult), [sm["w2"], sm["grp"]], [sm["cw2"]])
                    dv(lambda e: e.tensor_tensor(out=MAf[i][:], in0=M1[i][:], in1=M2[i][:], op=ALU.add), [M1[i], M2[i]], [MAf[i]])
                    dv(lambda e: e.tensor_copy(MA[i][:], MAf[i][:]), [MAf[i]], [MA[i]])
                    dv(lambda e: e.tensor_scalar(out=CWm[i][:], in0=M1[i][:], scalar1=sm["cw1"][:, 0:1], scalar2=None, op0=ALU.mult), [M1[i], sm["cw1"]], [CWm[i]])
                    dv(lambda e: e.scalar_tensor_tensor(out=CWm[i][:], in0=M2[i][:], scalar=sm["cw2"][:, 0:1], in1=CWm[i][:], op0=ALU.mult, op1=ALU.add),
                       [M2[i], sm["cw2"], CWm[i]], [CWm[i]])

                emit_ln_tiles(S, Ca, H, Hb, lng, lnb, ln_cb)
                for i in range(NT):
                    ps = lb.get()
                    for i2 in range(i):
                        S.op("pe", lambda e, i2=i2: e.matmul(ps[:, 0:32], onesb, MA[i2][:], start=(i2 == 0), stop=False), reads=[CSb.b, MA[i2].b], writes=[ps.b])
                    S.op("pe", lambda e: e.matmul(ps[:, 0:32], lstr, MA[i][:], start=(i == 0), stop=True), reads=[CSb.b, MA[i].b], writes=[ps.b])
                    S.op("act", lambda e: e.copy(POS[i][:], ps[:, 0:32]), reads=[ps.b], writes=[POS[i].b])
                S.barrier()
            with ExitStack() as es2b:
                Cb = Ctx(nc, es2b, S)
                SELW = Cb.sb([128, N_EXPERTS, TPC], BF16, "SELW")
                Sel = [[Cb.sb([128, CAP], BF16, "Sel") for _ in range(NT)] for _ in range(2)]
                Swt = [Cb.sb([128, CAP], BF16, "Swt") for _ in range(4)]
                XG = [Cb.sb([128, KC, CAP], BF16, "XG") for _ in range(2)]
                gbk = BankPool(PS[0:5])
                sbk = BankPool(PS[5:8])
                nsw = 0
                for ex in range(N_EXPERTS):
                    sel = Sel[ex % 2]
                    ps_t = sbk.get()
                    ps_tb = ps_t[:].bitcast(BF16)
                    for i in range(NT):
                        S.op("dve", lambda e, i=i: e.tensor_scalar(out=sel[i][:], in0=iota, scalar1=POS[i][:, ex:ex + 1], scalar2=MAf[i][:, ex:ex + 1],
                                                                   op0=ALU.is_equal, op1=ALU.mult), reads=[CS.b, POS[i].b, MAf[i].b], writes=[sel[i].b])
                        sw = Swt[nsw % 4]
                        nsw += 1
                        S.op("dve", lambda e, i=i, sw=sw: e.tensor_scalar(out=sw[:], in0=iota, scalar1=POS[i][:, ex:ex + 1], scalar2=CWm[i][:, ex:ex + 1],
                                                                          op0=ALU.is_equal, op1=ALU.mult), reads=[CS.b, POS[i].b, CWm[i].b], writes=[sw.b])
                        S.op("pe", lambda e, i=i, sw=sw: e.transpose(ps_tb[:, i * 128:(i + 1) * 128], sw[:], identb), reads=[sw.b, CSb.b], writes=[ps_t.b])
                    S.op("act", lambda e: e.copy(SELW[:, ex, :], ps_tb[:, 0:1024]), reads=[ps_t.b], writes=[SELW.b])
                    xg_ = XG[ex % 2]
                    for g in range(8):
                        ps = gbk.get()
                        for j in range(4):
                            kc = g * 4 + j
                            for i in range(NT):
                                S.op("pe", lambda e, i=i, j=j, kc=kc: e.matmul(ps[:, j * 128:(j + 1) * 128], X1b[:, i, kc * 128:(kc + 1) * 128], sel[i][:],
                                                                            start=(i == 0), stop=(i == NT - 1)),
                                     reads=[X1bb[i], sel[i].b], writes=[ps.b])
                        if g % 2 == 0:
                            S.op("act", lambda e, g=g: e.copy(xg_[:, 4 * g:4 * g + 4, :], ps[:, :].rearrange("p (a b) -> p a b", a=4)), reads=[ps.b], writes=[xg_.b])
                        else:
                            S.op("dve", lambda e, g=g: e.tensor_copy(xg_[:, 4 * g:4 * g + 4, :], ps[:, :].rearrange("p (a b) -> p a b", a=4)), reads=[ps.b], writes=[xg_.b])
                    S.dma("sp", xg[ex], xg_[:], reads=[xg_.b])
                S.dma("sp", selw, SELW[:], reads=[SELW.b])
                S.barrier()
        S.barrier()
    return nc


EPC = N_EXPERTS // NCORES
SLOTS = NCORES * CAP


def build_b2():
    nc = bass.Bass("TRN2", target_bir_lowering=False)
    xg = nc.dram_tensor("xg", [EPC, 128, KC, SLOTS], BF16, kind="ExternalInput").ap()
    wg_ = nc.dram_tensor("wg", [EPC, D_MODEL, D_EXPERT], F32, kind="ExternalInput").ap()
    wu_ = nc.dram_tensor("wu", [EPC, D_MODEL, D_EXPERT], F32, kind="ExternalInput").ap()
    wd_ = nc.dram_tensor("wd", [EPC, D_EXPERT, D_MODEL], F32, kind="ExternalInput").ap()
    cst = nc.dram_tensor("cst", [128, 512], F32, kind="ExternalInput").ap()
    yo = nc.dram_tensor("yo", [EPC, SLOTS, D_MODEL], BF16, kind="ExternalOutput").ap()
    NST = SLOTS // 128
    with ExitStack() as outer:
        S = Sched(nc, outer)
        C = Ctx(nc, outer, S)
        PS = C.psum_banks(8)
        CS, CSb = emit_consts(S, C, cst)
        identb = CSb[:, 0, :]
        Wg = C.sb([128, KC, D_EXPERT], BF16, "Wg")
        Wu = C.sb([128, KC, D_EXPERT], BF16, "Wu")
        Wd = C.sb([128, 4, D_MODEL], BF16, "Wd")
        X = C.sb([128, KC, SLOTS], BF16, "X")
        hidT = C.sb([128, 4, SLOTS], BF16, "hidT")
        sg = [C.sb([128, 512], F32, "sg") for _ in range(2)]
        hid = [C.sb([128, 512], BF16, "hid") for _ in range(2)]
        Yt = [C.sb([128, D_MODEL], BF16, "Yt") for _ in range(2)]
        gub = BankPool(PS[0:4])
        tbk = BankPool(PS[4:5])
        dbk = BankPool(PS[5:8])
        ny = 0
        for ex in range(EPC):
            wgv = wg_[ex].rearrange("(k p) n -> p k n", p=128)
            wuv = wu_[ex].rearrange("(k p) n -> p k n", p=128)
            wdv = wd_[ex].rearrange("(m p) n -> p m n", p=128)
            for j in range(4):
                S.dma("pool", Wg[:, 8 * j:8 * j + 8, :], wgv[:, 8 * j:8 * j + 8, :], writes=[Wg.b])
            for j in range(4):
                S.dma("pool", Wu[:, 8 * j:8 * j + 8, :], wuv[:, 8 * j:8 * j + 8, :], writes=[Wu.b])
            for j in range(4):
                S.dma("sp", X[:, 8 * j:8 * j + 8, :], xg[ex][:, 8 * j:8 * j + 8, :], writes=[X.b])
            for j in range(4):
                S.dma("pool", Wd[:, j, :], wdv[:, j, :], writes=[Wd.b])
            for st in range(NST):
                ssl = slice(st * 128, (st + 1) * 128)
                pg = gub.get()
                pu = gub.get()
                for kc in range(KC):
                    S.op("pe", lambda e, kc=kc: e.matmul(pg[:, :], X[:, kc, ssl], Wg[:, kc, :], start=(kc == 0), stop=(kc == KC - 1)), reads=[X.b, Wg.b], writes=[pg.b])
                for kc in range(KC):
                    S.op("pe", lambda e, kc=kc: e.matmul(pu[:, :], X[:, kc, ssl], Wu[:, kc, :], start=(kc == 0), stop=(kc == KC - 1)), reads=[X.b, Wu.b], writes=[pu.b])
                s_, h_ = sg[st % 2], hid[st % 2]
                S.op("act", lambda e: e.activation(out=s_[:], in_=pg[:, :], func=AF.Silu), reads=[pg.b], writes=[s_.b])
                S.op("dve", lambda e: e.tensor_tensor(out=h_[:], in0=s_[:], in1=pu[:, :], op=ALU.mult), reads=[s_.b, pu.b], writes=[h_.b])
                pt = tbk.get()
                ptb = pt[:].bitcast(BF16)
                for mc in range(4):
                    S.op("pe", lambda e, mc=mc: e.transpose(ptb[:, mc * 128:(mc + 1) * 128], h_[:, mc * 128:(mc + 1) * 128], identb), reads=[h_.b, CSb.b], writes=[pt.b])
                S.op("act", lambda e: e.copy(hidT[:, :, ssl], ptb[:, 0:512].rearrange("p (a b) -> p a b", a=4)), reads=[pt.b], writes=[hidT.b])
            for st in range(NST):
                ssl = slice(st * 128, (st + 1) * 128)
                y_ = Yt[ny % 2]
                ny += 1
                for n in range(8):
                    pd = dbk.get()
                    for mc in range(4):
                        S.op("pe", lambda e, mc=mc: e.matmul(pd[:, :], hidT[:, mc, ssl], Wd[:, mc, n * 512:(n + 1) * 512], start=(mc == 0), stop=(mc == 3)),
                             reads=[hidT.b, Wd.b], writes=[pd.b])
                    if n % 2 == 0:
                        S.op("act", lambda e: e.copy(y_[:, n * 512:(n + 1) * 512], pd[:, :]), reads=[pd.b], writes=[y_.b])
                    else:
                        S.op("dve", lambda e: e.tensor_copy(y_[:, n * 512:(n + 1) * 512], pd[:, :]), reads=[pd.b], writes=[y_.b])
                S.dma("sp", yo[ex, ssl, :], y_[:], reads=[y_.b])
        S.barrier()
    return nc


def build_b3():
    nc = bass.Bass("TRN2", target_bir_lowering=False)
    yin = nc.dram_tensor("yin", [N_EXPERTS * CAP, D_MODEL], BF16, kind="ExternalInput").ap()
    selw = nc.dram_tensor("selw", [128, N_EXPERTS, TPC], BF16, kind="ExternalInput").ap()
    x1 = nc.dram_tensor("x1", [TPC, D_MODEL], F32, kind="ExternalInput").ap()
    lng = nc.dram_tensor("lng", [128, D_MODEL], F32, kind="ExternalInput").ap()
    lnb = nc.dram_tensor("lnb", [128, D_MODEL], F32, kind="ExternalInput").ap()
    cst = nc.dram_tensor("cst", [128, 512], F32, kind="ExternalInput").ap()
    x2 = nc.dram_tensor("x2", [TPC, D_MODEL], F32, kind="ExternalOutput").ap()
    xTo = nc.dram_tensor("xTo", [D_MODEL, TPC], BF16, kind="ExternalOutput").ap()
    H = nc.dram_tensor("Hscr", [TPC, D_MODEL], F32, kind="Internal").ap()
    with ExitStack() as outer:
        S = Sched(nc, outer)
        Hb = [Buf(f"H{i}") for i in range(NT)]
        xb_ = [Buf(f"xr{i}") for i in range(NT)]
        with ExitStack() as es:
            C = Ctx(nc, es, S)
            PS = C.psum_banks(8)
            SW = C.sb([128, N_EXPERTS, TPC], BF16, "SW")
            for j in range(4):
                S.dma("sp", SW[:, 8 * j:8 * j + 8, :], selw[:, 8 * j:8 * j + 8, :], writes=[SW.b])
            emit_proj_res(S, C, PS, lambda k, i: (SW[:, k, i * 128:(i + 1) * 128], SW.b), N_EXPERTS,
                          yin.rearrange("(e s) d -> s e d", s=128), False, x1, xb_, H, Hb)
            S.barrier()
        with ExitStack() as es2:
            C = Ctx(nc, es2, S)
            PS = C.psum_banks(8)
            CS, CSb = emit_consts(S, C, cst)
            xTs = C.sb([128, KC, TPC], BF16, "xTs")
            xb = [C.sb([128, D_MODEL], BF16, "xb") for _ in range(2)]
            banks = BankPool(PS)
            x2b = [Buf(f"x2{i}") for i in range(NT)]

            def cb(i, t):
                S.dma("sp", x2[i * 128:(i + 1) * 128, :], t[:], reads=[t.b], writes=[x2b[i]])
                b_ = xb[i % 2]
                S.op("act", lambda e: e.copy(b_[:], t[:]), reads=[t.b], writes=[b_.b])
                emit_transpose_tile(S, banks, b_, CSb[:, 0, :], CSb, lambda g: (xTs[:, g * 8:(g + 1) * 8, i * 128:(i + 1) * 128], xTs.b))

            emit_ln_tiles(S, C, H, Hb, lng, lnb, cb)
            S.dma("sp", xTo.rearrange("(kc p) t -> p kc t", p=128), xTs[:], reads=[xTs.b])
            S.barrier()
        S.barrier()
    return nc


_NC_CACHE = {}
_DBG = None


def _get_nc(name, fn):
    if name not in _NC_CACHE:
        _NC_CACHE[name] = fn()
    return _NC_CACHE[name]


def _run(nc, maps):
    res = run_bass_kernel_spmd(nc, maps, core_ids=list(range(len(maps))))
    return res.results


def mix_perm():
    p = []
    for r in range(NCORES):
        p.append(np.arange(384 * r, 384 * r + 384))
        p.append(GDN_WIDTH + np.arange(128 * r, 128 * r + 128))
    return np.concatenate(p)


def prep_b1_weights(layer, inp):
    wo = np.ascontiguousarray(inp["w_out"][layer][mix_perm(), :])
    wglu = np.ascontiguousarray(inp["s5_w_glu"][layer])
    wr = np.concatenate([inp["router_group_w"][layer]] + [inp["router_expert_w"][layer][g] for g in range(4)], axis=1)
    rbv = np.concatenate([inp["router_group_b"][layer], inp["router_expert_b"][layer].reshape(-1)])
    return {"wo": wo, "wglu": wglu, "wr": np.ascontiguousarray(wr.astype(np.float32)),
            "rb": np.ascontiguousarray(np.broadcast_to(rbv[None, :], (128, 36))).astype(np.float32),
            "lng": np.ascontiguousarray(np.broadcast_to(inp["ln1_g"][layer][None, :], (128, D_MODEL))),
            "lnb": np.ascontiguousarray(np.broadcast_to(inp["ln1_b"][layer][None, :], (128, D_MODEL))),
            "cst": b_consts()}


def run_layer(layer, inp, xT_all, xres, ncores=NCORES, L=SEQ):
    ncm = _get_nc(("mixer", L), lambda: build_mixer(L, True))
    maps = []
    for c in range(NCORES):
        m = prep_mixer(c, layer, inp)
        m["xT"] = xT_all
        maps.append(m)
    res = _run(ncm, maps)
    ymix_all = np.concatenate([np.asarray(res[c]["yT"]) for c in range(NCORES)], axis=0)
    del res, maps
    ntc = L // TPC
    w1 = prep_b1_weights(layer, inp)
    maps = []
    for c in range(ntc):
        m = dict(w1)
        m["ymix"] = np.ascontiguousarray(ymix_all[:, c * TPC:(c + 1) * TPC])
        m["x"] = xres[c]
        maps.append(m)
    res = _run(_get_nc("b1", build_b1), maps)
    x1 = [np.asarray(res[c]["x1"]) for c in range(ntc)]
    if _DBG is not None:
        _DBG["ymix"] = ymix_all
        _DBG["x1"] = x1
    xg = [np.asarray(res[c]["xg"]) for c in range(ntc)]
    selw = [np.asarray(res[c]["selw"]) for c in range(ntc)]
    del res, maps
    maps = []
    for c2 in range(NCORES):
        xin = np.zeros((EPC, 128, KC, SLOTS), ml_dtypes.bfloat16)
        for c in range(ntc):
            xin[:, :, :, c * CAP:(c + 1) * CAP] = xg[c][EPC * c2:EPC * c2 + EPC]
        maps.append({"xg": xin,
                     "wg": np.ascontiguousarray(inp["expert_w_gate"][layer][EPC * c2:EPC * c2 + EPC]),
                     "wu": np.ascontiguousarray(inp["expert_w_up"][layer][EPC * c2:EPC * c2 + EPC]),
                     "wd": np.ascontiguousarray(inp["expert_w_down"][layer][EPC * c2:EPC * c2 + EPC]),
                     "cst": b_consts()})
    del xg
    res = _run(_get_nc("b2", build_b2), maps)
    yo = [np.asarray(res[c2]["yo"]) for c2 in range(NCORES)]
    del res, maps
    lng = np.ascontiguousarray(np.broadcast_to(inp["ln2_g"][layer][None, :], (128, D_MODEL)))
    lnb = np.ascontiguousarray(np.broadcast_to(inp["ln2_b"][layer][None, :], (128, D_MODEL)))
    maps = []
    for c in range(ntc):
        yin = np.concatenate([yo[e // EPC][e % EPC, c * CAP:(c + 1) * CAP, :] for e in range(N_EXPERTS)], axis=0)
        maps.append({"yin": np.ascontiguousarray(yin), "selw": selw[c], "x1": x1[c], "lng": lng, "lnb": lnb, "cst": b_consts()})
    res = _run(_get_nc("b3", build_b3), maps)
    x2 = [np.asarray(res[c]["x2"]) for c in range(ntc)]
    xTn = [np.asarray(res[c]["xTo"]) for c in range(ntc)]
    return x2, xTn


def kernel(**inputs):
    inp = {k: np.asarray(v) for k, v in inputs.items()}
    x = inp["x"][0]
    xres = [np.ascontiguousarray(x[c * TPC:(c + 1) * TPC]) for c in range(NCORES)]
    res = _run(_get_nc("t0", build_t0), [{"x": xres[c], "cst": b_consts()} for c in range(NCORES)])
    xT_all = np.stack([np.asarray(res[c]["xTo"]) for c in range(NCORES)], axis=0)
    for layer in range(DEPTH):
        xres, xTn = run_layer(layer, inp, xT_all, xres)
        xT_all = np.stack(xTn, axis=0)
    return np.concatenate(xres, axis=0)[None].astype(np.float32)
```

```python
import numpy as np
import ml_dtypes
from contextlib import ExitStack
import concourse.bass as bass
import concourse.mybir as mybir
from concourse.bass_utils import run_bass_kernel_spmd

F32 = mybir.dt.float32
BF16 = mybir.dt.bfloat16
I32 = mybir.dt.int32
AF = mybir.ActivationFunctionType
ALU = mybir.AluOpType
AX = mybir.AxisListType

D_MODEL = 4096
SEQ = 8192
DEPTH = 2
NCORES = 8
KC = D_MODEL // 128
GDN_HEADS = 24
HPC = 3
GDN_WIDTH = 3072
S5_WIDTH = 1024
S5_STATE = 64
GPC = 8
CH = 64
N_EXPERTS = 32
D_EXPERT = 512
DN_ALPHA = (2 * DEPTH) ** 0.25
LN_EPS = 1e-5
RMS_EPS = 1e-6
L2_EPS = 1e-6
CAP = 128
TWO_PI = float(2 * np.pi)
MAGIC = 12582912.0


class Ev:
    __slots__ = ("sem", "sid", "val", "eng")

    def __init__(self, sem, sid, val, eng):
        self.sem, self.sid, self.val, self.eng = sem, sid, val, eng


class Buf:
    def __init__(self, name, excl=False):
        self.name = name
        self.w = None
        self.r = {}
        self.excl = excl


class Sched:
    ROT = 8000
    NSLOT = 12

    def __init__(self, nc, es):
        self.nc, self.es = nc, es
        self.engs = dict(pe=nc.tensor, act=nc.scalar, dve=nc.vector, pool=nc.gpsimd, sp=nc.sync)
        self.nsem = 0
        self.cur = {}
        for e in self.engs:
            self.cur[e] = [self._newsem(e), 0]
        self.known = {e: {} for e in self.engs}
        self.slots = {}
        self.slot_i = {}
        self.nops = 0
        self.prev = {}

    def _newsem(self, tag):
        self.nsem += 1
        s = self.es.enter_context(self.nc.semaphore(f"s{self.nsem}_{tag}"))
        return (s, self.nsem)

    def _wait(self, e, ev):
        if ev is None:
            return
        if e == "pe" and ev.eng == "pe":
            return
        k = self.known[e]
        if k.get(ev.sid, -1) >= ev.val:
            return
        self.engs[e].wait_ge(ev.sem, ev.val)
        k[ev.sid] = ev.val

    def _deps(self, e, reads, writes):
        for b in reads:
            self._wait(e, b.w)
            if b.excl:
                for ev in list(b.r.values()):
                    self._wait(e, ev)
        for b in writes:
            self._wait(e, b.w)
            for ev in list(b.r.values()):
                self._wait(e, ev)

    def _commit(self, ev, reads, writes):
        for b in reads:
            if b.excl:
                b.w = ev
                b.r = {}
            else:
                b.r[ev.sid] = ev
        for b in writes:
            b.w = ev
            b.r = {}

    def op(self, e, fn, reads=(), writes=()):
        self._deps(e, reads, writes)
        ins = fn(self.engs[e])
        (sem, sid), cnt = self.cur[e]
        cnt += 1
        ins.then_inc(sem, 1)
        ev = Ev(sem, sid, cnt, e)
        self.cur[e][1] = cnt
        if cnt >= self.ROT:
            self.prev[e] = ev
            self.cur[e] = [self._newsem(e), 0]
        self._commit(ev, reads, writes)
        self.nops += 1
        return ev

    def dma(self, q, out, in_, reads=(), writes=(), **kw):
        if q not in self.slots:
            self.slots[q] = [[self._newsem("d" + q), 0, None] for _ in range(self.NSLOT)]
            self.slot_i[q] = 0
        i = self.slot_i[q]
        self.slot_i[q] = (i + 1) % self.NSLOT
        slot = self.slots[q][i]
        self._wait(q, slot[2])
        self._deps(q, reads, writes)
        ins = self.engs[q].dma_start(out=out, in_=in_, **kw)
        slot[1] += 16
        (sem, sid) = slot[0]
        ins.then_inc(sem, 16)
        ev = Ev(sem, sid, slot[1], "dma")
        slot[2] = ev
        self._commit(ev, reads, writes)
        self.nops += 1
        return ev

    def dma_ins(self, q, mk, reads=(), writes=()):
        if q not in self.slots:
            self.slots[q] = [[self._newsem("d" + q), 0, None] for _ in range(self.NSLOT)]
            self.slot_i[q] = 0
        i = self.slot_i[q]
        self.slot_i[q] = (i + 1) % self.NSLOT
        slot = self.slots[q][i]
        self._wait(q, slot[2])
        self._deps(q, reads, writes)
        ins = mk(self.engs[q])
        slot[1] += 16
        (sem, sid) = slot[0]
        ins.then_inc(sem, 16)
        ev = Ev(sem, sid, slot[1], "dma")
        slot[2] = ev
        self._commit(ev, reads, writes)
        return ev

    def barrier(self):
        evs = []
        for e in self.engs:
            (sem, sid), cnt = self.cur[e]
            if cnt > 0:
                evs.append(Ev(sem, sid, cnt, e))
            elif e in self.prev:
                evs.append(self.prev[e])
        for q in self.slots:
            for slot in self.slots[q]:
                if slot[2] is not None:
                    evs.append(slot[2])
        for e in self.engs:
            for ev in evs:
                self._wait(e, ev)

    def finish(self, bufs):
        for b in bufs:
            self._wait("sp", b.w)


class T:
    def __init__(self, t, name, excl=False):
        self.t = t
        self.b = Buf(name, excl)

    def __getitem__(self, k):
        return self.t[k]


_uid = [0]


class Ctx:
    def __init__(self, nc, es, S=None):
        self.nc, self.es = nc, es
        self.S = S if S is not None else Sched(nc, es)

    @property
    def n(self):
        return _uid[0]

    @n.setter
    def n(self, v):
        _uid[0] = v

    def sb(self, shape, dt=F32, name=None):
        self.n += 1
        nm = f"{name or 't'}_{self.n}"
        return T(self.es.enter_context(self.nc.sbuf_tensor(nm, list(shape), dt)), nm)

    def psum_banks(self, n=8):
        out = []
        for i in range(n):
            self.n += 1
            nm = f"ps{i}_{self.n}"
            out.append(T(self.es.enter_context(self.nc.psum_tensor(nm, [128, 512], F32)), nm, excl=True))
        return out


class View:
    def __init__(self, fn, b):
        self.fn, self.b = fn, b

    def __getitem__(self, k):
        return self.fn(k)


class BankPool:
    def __init__(self, banks):
        self.banks = list(banks)
        self.i = 0

    def get(self):
        b = self.banks[self.i]
        self.i = (self.i + 1) % len(self.banks)
        return b


GDN_COLS = 4 * HPC * 128 + 2 * HPC
NEG = -30000.0


def gdn_consts():
    c = np.zeros((128, 8, 128), np.float32)
    c[:, 0, :] = np.eye(128)
    i = np.arange(64)
    U = (i[:, None] <= i[None, :]).astype(np.float32)
    c[:64, 1, :64] = U
    c[:64, 2, :64] = -U
    c[:64, 3, :64] = (i[:, None] > i[None, :]).astype(np.float32)
    c[:, 4, :] = 1.0
    c[:64, 5, :64] = np.where(i[:, None] > i[None, :], 0.0, NEG)
    c[:64, 6, :64] = np.where(i[None, :] >= i[:, None], 0.0, NEG)
    return c.reshape(128, 8 * 128)


def emit_gdn(nc, S, L, x_bf16, xT, wg, convw, hp, cst, yT):
    NR = max(1, L // 1024)
    TR = min(L, 1024)
    NSB = L // 512
    xq = "sp" if x_bf16 else "pool"

    with ExitStack() as es:
        C = Ctx(nc, es, S)
        W = C.sb([128, KC, GDN_COLS], BF16, "W")
        XT = [C.sb([128, 8, 512], BF16, "XT") for _ in range(4)]
        CW = C.sb([128, 36], F32, "CW")
        HP = C.sb([128, 64], F32, "HP")
        CS = C.sb([128, 8, 128], F32, "CS")
        CSb = C.sb([128, 2, 128], BF16, "CSb")
        EAL = C.sb([128, 24], F32, "EAL")
        halo = C.sb([128, 9, 3], F32, "halo")
        raw = [C.sb([128, 515], F32, "raw") for _ in range(1)]
        cv = [C.sb([128, 512], F32, "cv") for _ in range(1)]
        actb = [C.sb([128, 512], F32, "actb") for _ in range(1)]
        sqb = [C.sb([128, 512], BF16, "sqb") for _ in range(1)]
        rnb = [C.sb([128, 512], F32, "rnb") for _ in range(1)]
        ACTS = [dict(qn=[C.sb([128, 512], BF16, "qn") for _ in range(HPC)],
                     kn=[C.sb([128, 512], BF16, "kn") for _ in range(HPC)],
                     vT=[C.sb([128, 512], BF16, "vT") for _ in range(HPC)],
                     zs=[C.sb([128, 512], BF16, "zs") for _ in range(HPC)],
                     abT=C.sb([8, 512], F32, "abT")) for _ in range(2)]
        yo = [C.sb([128, 512], BF16, "yo") for _ in range(HPC)]
        Sst = [C.sb([128, 128], F32, "S") for _ in range(HPC)]
        Sb = [C.sb([128, 128], BF16, "Sb") for _ in range(HPC)]
        PS = C.psum_banks(8)
        proj_banks = BankPool(PS[0:2])
        wk_banks = BankPool(PS[2:8])

        ident = CS[:, 0, :]
        identb = CSb[:, 0, :]
        onesb = CSb[:, 1, :]

        wv = wg.rearrange("(kc p) n -> p kc n", p=128)
        for j in range(8):
            S.dma("pool", W[:, 4 * j:4 * j + 4, :], wv[:, 4 * j:4 * j + 4, :], writes=[W.b])
        S.dma("sp", CW[:], convw, writes=[CW.b])
        S.dma("sp", HP[:], hp, writes=[HP.b])
        S.dma("sp", CS[:].rearrange("p a b -> p (a b)"), cst, writes=[CS.b])
        S.op("dve", lambda e: e.tensor_copy(CSb[:, 0, :], CS[:, 0, :]), reads=[CS.b], writes=[CSb.b])
        S.op("dve", lambda e: e.tensor_copy(CSb[:, 1, :], CS[:, 4, :]), reads=[CS.b], writes=[CSb.b])
        S.op("act", lambda e: e.activation(out=EAL[:], in_=HP[:, 40:64], func=AF.Exp), reads=[HP.b], writes=[EAL.b])
        S.op("dve", lambda e: e.memset(halo[:].rearrange("p a b -> p (a b)"), 0.0), writes=[halo.b])
        for h in range(HPC):
            S.op("dve", lambda e, h=h: e.memset(Sst[h][:], 0.0), writes=[Sst[h].b])
            S.op("dve", lambda e, h=h: e.memset(Sb[h][:], 0.0), writes=[Sb[h].b])

        xv = xT.rearrange("r (kc p) t -> r p kc t", p=128)

        def small(shape, dt=F32, name="w"):
            return C.sb(shape, dt, name)

        NCG = 2
        NU = NCG * HPC
        GG = dict(
            x1=small([64, 24]), ex=small([64, 24]), sp=small([64, 24]), g=small([64, 24]), beta=small([64, 24]),
            edec=small([64, 24]), edlm=small([64, 24]), edl=small([128, 24]), bedec=small([64, 24]),
        )
        HW = []
        for u in range(NU):
            HW.append(dict(
                Gb=small([64, 64]), gs=small([64, 64]), gT=small([64, 64]),
                kbe=small([64, 128], BF16), ktail=small([64, 128], BF16),
                bv=small([64, 128], BF16), N=small([64, 64], BF16),
                P=[small([64, 64], BF16), small([64, 64], BF16)], Q=[small([64, 64], BF16), small([64, 64], BF16)],
                X=small([64, 64], BF16), Xb=small([64, 64], BF16), qkT=small([64, 64], BF16),
                nwT=small([128, 64], BF16), Lm=small([64, 128]), qd=small([128, 64], BF16),
                vn=small([64, 128], BF16), ssq=small([64, 1]), rt=small([64, 1]), rstd=small([64, 1]),
                junk=small([64, 128], BF16),
            ))

        def proj_gen(sbn):
            A = ACTS[sbn % 2]
            r = (sbn * 512) // TR
            t0 = (sbn * 512) % TR
            for j in range(4):
                S.dma(xq, XT[j][:], xv[r, :, 8 * j:8 * j + 8, t0:t0 + 512], writes=[XT[j].b])
            yield

            def ptile(c0, m):
                ps = proj_banks.get()
                for kc in range(KC):
                    S.op("pe", lambda e, kc=kc: e.matmul(ps[0:m, :], W[:, kc, c0:c0 + m], XT[kc // 8][:, kc % 8, :],
                                                         start=(kc == 0), stop=(kc == KC - 1)),
                         reads=[W.b, XT[kc // 8].b], writes=[ps.b])
                    if kc % 2 == 1:
                        yield
                return ps

            for kind, base in (("k", 384), ("q", 0), ("v", 768)):
                for h in range(HPC):
                    ct = {"q": 0, "k": 3, "v": 6}[kind] + h
                    ps = yield from ptile(base + 128 * h, 128)
                    rw, cvb, ab_, sq_, rn_ = raw[0], cv[0], actb[0], sqb[0], rnb[0]
                    S.op("act", lambda e: e.copy(rw[:, 3:515], ps[:, :]), reads=[ps.b], writes=[rw.b])
                    S.op("dve", lambda e: e.tensor_copy(rw[:, 0:3], halo[:, ct, :]), reads=[halo.b], writes=[rw.b])
                    yield
                    S.op("dve", lambda e: e.tensor_scalar(out=cvb[:], in0=rw[:, 0:512], scalar1=CW[:, 4 * ct:4 * ct + 1],
                                                          scalar2=None, op0=ALU.mult), reads=[rw.b, CW.b], writes=[cvb.b])
                    for j in range(1, 4):
                        S.op("dve", lambda e, j=j: e.scalar_tensor_tensor(out=cvb[:], in0=rw[:, j:j + 512],
                                                                          scalar=CW[:, 4 * ct + j:4 * ct + j + 1], in1=cvb[:],
                                                                          op0=ALU.mult, op1=ALU.add),
                             reads=[rw.b, CW.b, cvb.b], writes=[cvb.b])
                        yield
                    S.op("dve", lambda e: e.tensor_copy(halo[:, ct, :], rw[:, 512:515]), reads=[rw.b], writes=[halo.b])
                    if kind == "v":
                        S.op("act", lambda e: e.activation(out=A["vT"][h][:], in_=cvb[:], func=AF.Silu), reads=[cvb.b], writes=[A["vT"][h].b])
                        yield
                        continue
                    S.op("act", lambda e: e.activation(out=ab_[:], in_=cvb[:], func=AF.Silu), reads=[cvb.b], writes=[ab_.b])
                    yield
                    S.op("act", lambda e: e.activation(out=sq_[:], in_=ab_[:], func=AF.Square), reads=[ab_.b], writes=[sq_.b])
                    yield
                    ps2 = proj_banks.get()
                    S.op("pe", lambda e: e.matmul(ps2[:, :], onesb, sq_[:], start=True, stop=True), reads=[CSb.b, sq_.b], writes=[ps2.b])
                    S.op("act", lambda e: e.activation(out=rn_[:], in_=ps2[:, :], func=AF.Sqrt, bias=L2_EPS), reads=[ps2.b], writes=[rn_.b])
                    yield
                    S.op("dve", lambda e: e.reciprocal(rn_[:], rn_[:]), reads=[rn_.b], writes=[rn_.b])
                    dst = A["qn"][h] if kind == "q" else A["kn"][h]
                    sc = 128.0 ** -0.5 if kind == "q" else 1.0
                    S.op("dve", lambda e: e.scalar_tensor_tensor(out=dst[:], in0=ab_[:], scalar=sc, in1=rn_[:], op0=ALU.mult, op1=ALU.mult),
                         reads=[ab_.b, rn_.b], writes=[dst.b])
                    yield
            for h in range(HPC):
                ps = yield from ptile(1152 + 128 * h, 128)
                S.op("act", lambda e: e.activation(out=A["zs"][h][:], in_=ps[:, :], func=AF.Silu), reads=[ps.b], writes=[A["zs"][h].b])
                yield
            ps = yield from ptile(1536, 6)
            S.op("act", lambda e: e.copy(A["abT"][0:6, :], ps[0:6, :]), reads=[ps.b], writes=[A["abT"].b])
            yield

        gen_state = [None]

        def tick():
            g = gen_state[0]
            if g is not None:
                try:
                    next(g)
                except StopIteration:
                    gen_state[0] = None

        def drain():
            while gen_state[0] is not None:
                tick()

        gen_state[0] = proj_gen(0)
        drain()
        for sb in range(NSB):
            A_ = ACTS[sb % 2]
            qn, kn, vT, zs, abT = A_["qn"], A_["kn"], A_["vT"], A_["zs"], A_["abT"]
            if sb + 1 < NSB:
                gen_state[0] = proj_gen(sb + 1)

            U_ = CS[0:64, 1, 0:64]
            nU_ = CS[0:64, 2, 0:64]
            gps = PS[2]
            for c in range(8):
                S.op("pe", lambda e, c=c: e.transpose(gps[0:64, 6 * c:6 * c + 6], abT[0:6, 64 * c:64 * c + 64], ident[0:6, 0:6]), reads=[abT.b, CS.b], writes=[gps.b])
            g3 = gps[0:64, 0:48].rearrange("p (c k) -> p c k", k=6)
            S.op("dve", lambda e: e.tensor_tensor(out=GG["x1"][:].rearrange("p (c h) -> p c h", h=3), in0=g3[:, :, 0:3],
                                                  in1=HP[0:64, 16:40].rearrange("p (c h) -> p c h", h=3), op=ALU.add),
                 reads=[gps.b, HP.b], writes=[GG["x1"].b])
            S.op("act", lambda e: e.activation(out=GG["beta"][:].rearrange("p (c h) -> p c h", h=3), in_=g3[:, :, 3:6], func=AF.Sigmoid), reads=[gps.b], writes=[GG["beta"].b])
            S.op("act", lambda e: e.activation(out=GG["ex"][:], in_=GG["x1"][:], func=AF.Exp), reads=[GG["x1"].b], writes=[GG["ex"].b])
            S.op("act", lambda e: e.activation(out=GG["sp"][:], in_=GG["ex"][:], func=AF.Ln, bias=1.0), reads=[GG["ex"].b], writes=[GG["sp"].b])
            S.op("dve", lambda e: e.scalar_tensor_tensor(out=GG["g"][:], in0=GG["sp"][:], scalar=-1.0, in1=EAL[0:64, :], op0=ALU.mult, op1=ALU.mult),
                 reads=[GG["sp"].b, EAL.b], writes=[GG["g"].b])
            S.op("pe", lambda e: e.matmul(gps[0:64, 64:88], CS[0:64, 1, 0:64], GG["g"][:], start=True, stop=True), reads=[CS.b, GG["g"].b], writes=[gps.b])
            S.op("pe", lambda e: e.matmul(gps[0:64, 96:120], CS[0:64, 3, 0:64], GG["g"][:], start=True, stop=True), reads=[CS.b, GG["g"].b], writes=[gps.b])
            S.op("pe", lambda e: e.matmul(gps[:, 128:152], CS[0:64, 4, :], GG["g"][:], start=True, stop=True), reads=[CS.b, GG["g"].b], writes=[gps.b])
            S.op("act", lambda e: e.activation(out=GG["edec"][:], in_=gps[0:64, 64:88], func=AF.Exp), reads=[gps.b], writes=[GG["edec"].b])
            S.op("act", lambda e: e.activation(out=GG["edlm"][:], in_=gps[0:64, 96:120], func=AF.Exp), reads=[gps.b], writes=[GG["edlm"].b])
            S.op("act", lambda e: e.activation(out=GG["edl"][:], in_=gps[:, 128:152], func=AF.Exp), reads=[gps.b], writes=[GG["edl"].b])
            S.op("dve", lambda e: e.tensor_tensor(out=GG["bedec"][:], in0=GG["beta"][:], in1=GG["edec"][:], op=ALU.mult),
                 reads=[GG["beta"].b, GG["edec"].b], writes=[GG["bedec"].b])
            for cg in range(8 // NCG):
                units = []
                for ci in range(NCG):
                    c = cg * NCG + ci
                    for h in range(HPC):
                        u = ci * HPC + h
                        units.append(dict(h=h, col=3 * c + h, cs=slice(64 * c, 64 * c + 64), w=HW[u], G=GG, ps=PS[2 + u]))
                def psb(u):
                    return u["ps"][:].bitcast(BF16)

                s1 = [
                    lambda u: S.op("dve", lambda e: e.tensor_scalar(out=u["w"]["Gb"][:], in0=CS[0:64, 4, 0:64], scalar1=u["G"]["g"][:, u["col"]:u["col"] + 1], scalar2=None, op0=ALU.mult),
                                   reads=[CS.b, u["G"]["g"].b], writes=[u["w"]["Gb"].b]),
                    lambda u: S.op("pe", lambda e: e.matmul(u["ps"][0:64, 0:64], U_, u["w"]["Gb"][:], start=True, stop=False), reads=[CS.b, u["w"]["Gb"].b], writes=[u["ps"].b]),
                    lambda u: S.op("pe", lambda e: e.matmul(u["ps"][0:64, 0:64], u["w"]["Gb"][:], nU_, start=False, stop=True), reads=[CS.b, u["w"]["Gb"].b], writes=[u["ps"].b]),
                    lambda u: S.op("pe", lambda e: e.matmul(u["ps"][0:64, 64:128], u["w"]["Gb"][:], U_, start=True, stop=False), reads=[CS.b, u["w"]["Gb"].b], writes=[u["ps"].b]),
                    lambda u: S.op("pe", lambda e: e.matmul(u["ps"][0:64, 64:128], nU_, u["w"]["Gb"][:], start=False, stop=True), reads=[CS.b, u["w"]["Gb"].b], writes=[u["ps"].b]),
                    lambda u: S.op("pe", lambda e: e.transpose(psb(u)[0:64, 256:384], kn[u["h"]][:, u["cs"]], identb), reads=[kn[u["h"]].b, CSb.b], writes=[u["ps"].b]),
                    lambda u: S.op("pe", lambda e: e.transpose(psb(u)[0:64, 384:512], vT[u["h"]][:, u["cs"]], identb), reads=[vT[u["h"]].b, CSb.b], writes=[u["ps"].b]),
                    lambda u: S.op("pe", lambda e: e.matmul(u["ps"][0:64, 256:320], kn[u["h"]][:, u["cs"]], kn[u["h"]][:, u["cs"]], start=True, stop=True), reads=[kn[u["h"]].b], writes=[u["ps"].b]),
                    lambda u: S.op("pe", lambda e: e.matmul(u["ps"][0:64, 320:384], kn[u["h"]][:, u["cs"]], qn[u["h"]][:, u["cs"]], start=True, stop=True), reads=[kn[u["h"]].b, qn[u["h"]].b], writes=[u["ps"].b]),
                    lambda u: S.op("dve", lambda e: e.tensor_tensor(out=u["w"]["gs"][:], in0=u["ps"][0:64, 0:64], in1=CS[0:64, 5, 0:64], op=ALU.add),
                                   reads=[u["ps"].b, CS.b], writes=[u["w"]["gs"].b]),
                    lambda u: S.op("dve", lambda e: e.tensor_tensor(out=u["w"]["gT"][:], in0=u["ps"][0:64, 64:128], in1=CS[0:64, 6, 0:64], op=ALU.add),
                                   reads=[u["ps"].b, CS.b], writes=[u["w"]["gT"].b]),
                    lambda u: S.op("act", lambda e: e.activation(out=u["w"]["gs"][:], in_=u["w"]["gs"][:], func=AF.Exp), reads=[u["w"]["gs"].b], writes=[u["w"]["gs"].b]),
                    lambda u: S.op("act", lambda e: e.activation(out=u["w"]["gT"][:], in_=u["w"]["gT"][:], func=AF.Exp), reads=[u["w"]["gT"].b], writes=[u["w"]["gT"].b]),
                    lambda u: S.op("dve", lambda e: e.tensor_scalar(out=u["w"]["kbe"][:], in0=psb(u)[0:64, 256:384], scalar1=u["G"]["bedec"][:, u["col"]:u["col"] + 1], scalar2=None, op0=ALU.mult),
                                   reads=[u["ps"].b, u["G"]["bedec"].b], writes=[u["w"]["kbe"].b]),
                    lambda u: S.op("dve", lambda e: e.tensor_scalar(out=u["w"]["ktail"][:], in0=psb(u)[0:64, 256:384], scalar1=u["G"]["edlm"][:, u["col"]:u["col"] + 1], scalar2=None, op0=ALU.mult),
                                   reads=[u["ps"].b, u["G"]["edlm"].b], writes=[u["w"]["ktail"].b]),
                    lambda u: S.op("dve", lambda e: e.tensor_scalar(out=u["w"]["bv"][:], in0=psb(u)[0:64, 384:512], scalar1=u["G"]["beta"][:, u["col"]:u["col"] + 1], scalar2=None, op0=ALU.mult),
                                   reads=[u["ps"].b, u["G"]["beta"].b], writes=[u["w"]["bv"].b]),
                    lambda u: S.op("dve", lambda e: e.scalar_tensor_tensor(out=u["w"]["N"][:], in0=u["ps"][0:64, 256:320], scalar=u["G"]["beta"][:, u["col"]:u["col"] + 1], in1=u["w"]["gs"][:],
                                                                         op0=ALU.mult, op1=ALU.mult),
                                   reads=[u["ps"].b, u["G"]["beta"].b, u["w"]["gs"].b], writes=[u["w"]["N"].b]),
                    lambda u: S.op("dve", lambda e: e.tensor_tensor(out=u["w"]["qkT"][:], in0=u["ps"][0:64, 320:384], in1=u["w"]["gT"][:], op=ALU.mult),
                                   reads=[u["ps"].b, u["w"]["gT"].b], writes=[u["w"]["qkT"].b]),
                    lambda u: S.op("dve", lambda e: e.tensor_scalar(out=u["w"]["Lm"][:], in0=CS[0:64, 4, :], scalar1=u["G"]["edec"][:, u["col"]:u["col"] + 1], scalar2=None, op0=ALU.mult),
                                   reads=[CS.b, u["G"]["edec"].b], writes=[u["w"]["Lm"].b]),
                    lambda u: S.op("pe", lambda e: e.matmul(u["ps"][:, 384:448], u["w"]["Lm"][:], ident[0:64, 0:64], start=True, stop=True), reads=[u["w"]["Lm"].b, CS.b], writes=[u["ps"].b]),
                    lambda u: S.op("pe", lambda e: e.transpose(psb(u)[0:64, 896:960], u["w"]["N"][:], identb[0:64, 0:64]), reads=[u["w"]["N"].b, CSb.b], writes=[u["ps"].b]),
                    lambda u: S.op("dve", lambda e: e.tensor_tensor(out=u["w"]["qd"][:], in0=qn[u["h"]][:, u["cs"]], in1=u["ps"][:, 384:448], op=ALU.mult),
                                   reads=[qn[u["h"]].b, u["ps"].b], writes=[u["w"]["qd"].b]),
                    lambda u: S.op("act", lambda e: e.copy(u["w"]["P"][0][:], psb(u)[0:64, 896:960]), reads=[u["ps"].b], writes=[u["w"]["P"][0].b]),
                    lambda u: S.op("dve", lambda e: e.tensor_tensor(out=u["w"]["X"][:], in0=ident[0:64, 0:64], in1=psb(u)[0:64, 896:960], op=ALU.subtract),
                                   reads=[u["ps"].b, CS.b], writes=[u["w"]["X"].b]),
                ]
                for st in s1:
                    for u in units:
                        st(u)
                    tick()
                for lvl in range(1, 6):
                    ci_ = (lvl - 1) % 2
                    for u in units:
                        w = u["w"]
                        u["Pc"] = w["P"][ci_]
                        u["Qc"] = w["N"] if lvl == 1 else w["Q"][ci_]
                        u["Pn"] = w["P"][1 - ci_]
                        u["Qn"] = w["Q"][1 - ci_]
                    s2 = []
                    if lvl < 5:
                        s2.append(lambda u: S.op("pe", lambda e: e.matmul(u["ps"][0:64, 0:64], u["Qc"][:], u["Pc"][:], start=True, stop=True), reads=[u["Pc"].b, u["Qc"].b], writes=[u["ps"].b]))
                    s2.append(lambda u: S.op("pe", lambda e: e.matmul(u["ps"][0:64, 64:128], u["Pc"][:], u["Qc"][:], start=True, stop=True), reads=[u["Pc"].b, u["Qc"].b], writes=[u["ps"].b]))
                    if lvl < 5:
                        s2.append(lambda u: S.op("act", lambda e: e.copy(u["Pn"][:], u["ps"][0:64, 0:64]), reads=[u["ps"].b], writes=[u["Pn"].b]))
                    s2.append(lambda u: S.op("dve", lambda e: e.tensor_copy(u["Qn"][:], u["ps"][0:64, 64:128]), reads=[u["ps"].b], writes=[u["Qn"].b]))
                    s2.append(lambda u: S.op("pe", lambda e: e.matmul(u["ps"][0:64, 128:192], u["Qn"][:], u["w"]["X"][:], start=True, stop=True), reads=[u["Qn"].b, u["w"]["X"].b], writes=[u["ps"].b]))
                    s2.append(lambda u: S.op("dve", lambda e: e.tensor_tensor(out=u["w"]["X"][:], in0=u["w"]["X"][:], in1=u["ps"][0:64, 128:192], op=ALU.add),
                                             reads=[u["ps"].b, u["w"]["X"].b], writes=[u["w"]["X"].b]))
                    for st in s2:
                        for u in units:
                            st(u)
                        tick()
                s3a = [
                    lambda u: S.op("pe", lambda e: e.matmul(u["ps"][:, 0:64], u["w"]["kbe"][:], u["w"]["X"][:], start=True, stop=True), reads=[u["w"]["kbe"].b, u["w"]["X"].b], writes=[u["ps"].b]),
                    lambda u: S.op("act", lambda e: e.mul(u["w"]["nwT"][:], u["ps"][:, 0:64], -1.0), reads=[u["ps"].b], writes=[u["w"]["nwT"].b]),
                ]
                for st in s3a:
                    for u in units:
                        st(u)
                    tick()
                s3b = [
                    lambda u: S.op("pe", lambda e: e.matmul(u["ps"][0:64, 128:256], u["w"]["X"][:], u["w"]["bv"][:], start=True, stop=False), reads=[u["w"]["X"].b, u["w"]["bv"].b], writes=[u["ps"].b]),
                    lambda u: S.op("pe", lambda e: e.matmul(u["ps"][0:64, 128:256], u["w"]["nwT"][:], Sb[u["h"]][:], start=False, stop=True), reads=[u["w"]["nwT"].b, Sb[u["h"]].b], writes=[u["ps"].b]),
                    lambda u: S.op("act", lambda e: e.copy(u["w"]["vn"][:], u["ps"][0:64, 128:256]), reads=[u["ps"].b], writes=[u["w"]["vn"].b]),
                    lambda u: S.op("pe", lambda e: e.matmul(u["ps"][0:64, 256:384], u["w"]["qd"][:], Sb[u["h"]][:], start=True, stop=False), reads=[u["w"]["qd"].b, Sb[u["h"]].b], writes=[u["ps"].b]),
                    lambda u: S.op("pe", lambda e: e.matmul(u["ps"][0:64, 256:384], u["w"]["qkT"][:], u["w"]["vn"][:], start=False, stop=True), reads=[u["w"]["qkT"].b, u["w"]["vn"].b], writes=[u["ps"].b]),
                    lambda u: S.op("pe", lambda e: e.matmul(u["ps"][:, 384:512], u["w"]["ktail"][:], u["w"]["vn"][:], start=True, stop=True), reads=[u["w"]["ktail"].b, u["w"]["vn"].b], writes=[u["ps"].b]),
                    lambda u: S.op("dve", lambda e: e.scalar_tensor_tensor(out=Sst[u["h"]][:], in0=Sst[u["h"]][:], scalar=u["G"]["edl"][:, u["col"]:u["col"] + 1], in1=u["ps"][:, 384:512],
                                                                         op0=ALU.mult, op1=ALU.add),
                                   reads=[Sst[u["h"]].b, u["G"]["edl"].b, u["ps"].b], writes=[Sst[u["h"]].b]),
                    lambda u: S.op("act", lambda e: e.copy(Sb[u["h"]][:], Sst[u["h"]][:]), reads=[Sst[u["h"]].b], writes=[Sb[u["h"]].b]),
                ]
                s3c = [
                    lambda u: S.op("act", lambda e: e.activation(out=u["w"]["junk"][:], in_=u["ps"][0:64, 256:384], func=AF.Square, accum_out=u["w"]["ssq"][:]),
                                   reads=[u["ps"].b], writes=[u["w"]["junk"].b, u["w"]["ssq"].b]),
                    lambda u: S.op("act", lambda e: e.activation(out=u["w"]["rt"][:], in_=u["w"]["ssq"][:], func=AF.Sqrt, bias=RMS_EPS, scale=1.0 / 128.0),
                                   reads=[u["w"]["ssq"].b], writes=[u["w"]["rt"].b]),
                    lambda u: S.op("dve", lambda e: e.reciprocal(u["w"]["rstd"][:], u["w"]["rt"][:]), reads=[u["w"]["rt"].b], writes=[u["w"]["rstd"].b]),
                    lambda u: S.op("dve", lambda e: e.tensor_scalar(out=u["w"]["Lm"][:], in0=u["ps"][0:64, 256:384], scalar1=u["w"]["rstd"][:, 0:1], scalar2=None, op0=ALU.mult),
                                   reads=[u["ps"].b, u["w"]["rstd"].b], writes=[u["w"]["Lm"].b]),
                    lambda u: S.op("pe", lambda e: e.transpose(u["ps"][:, 0:64], u["w"]["Lm"][:], ident[0:64, 0:64]), reads=[u["w"]["Lm"].b, CS.b], writes=[u["ps"].b]),
                    lambda u: S.op("dve", lambda e: e.scalar_tensor_tensor(out=yo[u["h"]][:, u["cs"]], in0=u["ps"][:, 0:64], scalar=HP[:, 8:9], in1=zs[u["h"]][:, u["cs"]],
                                                                         op0=ALU.mult, op1=ALU.mult),
                                   reads=[u["ps"].b, HP.b, zs[u["h"]].b], writes=[yo[u["h"]].b]),
                ]
                for ci in range(NCG):
                    us = units[ci * HPC:(ci + 1) * HPC]
                    for st in s3b:
                        for u in us:
                            st(u)
                        tick()
                for st in s3c:
                    for u in units:
                        st(u)
                    tick()

            drain()
            for h in range(HPC):
                S.dma("sp", yT[128 * h:128 * h + 128, sb * 512:sb * 512 + 512], yo[h][:], reads=[yo[h].b])
        S.barrier()


def x_to_xT(x2d):
    L = x2d.shape[0]
    TR = min(L, 1024)
    return np.ascontiguousarray(x2d.reshape(L // TR, TR, D_MODEL).transpose(0, 2, 1))


def prep_gdn(c, layer, inp):
    hs = [HPC * c + i for i in range(HPC)]
    w_in = inp["w_in"][layer]
    cols = []
    for blk in range(4):
        for h in hs:
            cols.append(np.arange(blk * GDN_WIDTH + h * 128, blk * GDN_WIDTH + (h + 1) * 128))
    cols.append(np.array([4 * GDN_WIDTH + h for h in hs]))
    cols.append(np.array([4 * GDN_WIDTH + GDN_HEADS + h for h in hs]))
    cols = np.concatenate(cols)
    wg = np.ascontiguousarray(w_in[:, cols])
    cw = inp["gdn_conv_w"][layer]
    convw = np.zeros((128, 36), np.float32)
    for blk in range(3):
        for i, h in enumerate(hs):
            ct = blk * 3 + i
            ch = blk * GDN_WIDTH + h * 128 + np.arange(128)
            convw[:, 4 * ct:4 * ct + 4] = cw[:, ch].T
    hp = np.zeros((128, 64), np.float32)
    hp[:, 0:3] = inp["gdn_a_log"][layer][hs][None, :]
    hp[:, 3:6] = inp["gdn_dt_bias"][layer][hs][None, :]
    hp[:, 8] = inp["gdn_norm_w"][layer]
    hp[:, 16:40] = np.tile(inp["gdn_dt_bias"][layer][hs], 8)[None, :]
    hp[:, 40:64] = np.tile(inp["gdn_a_log"][layer][hs], 8)[None, :]
    return {"wg": wg, "convw": convw, "hp": hp, "cst": gdn_consts()}


S5C = 256


def s5_consts():
    c = np.zeros((128, 5, 256), np.float32)
    c[:, 0, :128] = np.eye(128)
    k = np.arange(128)
    sw = np.zeros((128, 128), np.float32)
    sw[k, (k + 64) % 128] = 1.0
    c[:, 1, :128] = sw
    c[:, 2, :] = np.arange(256)[None, :]
    g = np.arange(128) // 16
    c[:, 3, :8] = (g[:, None] == np.arange(8)[None, :])
    c[:64, 3, 8] = 1.0
    c[64:, 3, 8] = -1.0
    c[:, 3, 9] = -1.0
    return c.reshape(128, 5 * 256)


def prep_s5(c, layer, inp):
    gs = np.arange(GPC * c, GPC * c + GPC)
    w_in = inp["w_in"][layer]
    c_u = 4 * GDN_WIDTH + 2 * GDN_HEADS
    wu = np.ascontiguousarray(w_in[:, c_u + 128 * c:c_u + 128 * c + 128])
    lre = inp["s5_lambda_re"][layer][gs]
    lim = inp["s5_lambda_im"][layer][gs]
    ldt = inp["s5_log_dt"][layer][gs]
    bre = inp["s5_b_re"][layer][gs]
    bim = inp["s5_b_im"][layer][gs]
    cre = inp["s5_c_re"][layer][gs]
    cim = inp["s5_c_im"][layer][gs]
    pr = np.zeros((128, 8, 64), np.float32)
    pr[:, 0, :] = np.repeat(lre, 16, axis=0)
    pr[:, 1, :] = np.repeat(lim, 16, axis=0)
    pr[:, 2, :] = bre.transpose(0, 2, 1).reshape(128, 64)
    pr[:, 3, :] = bim.transpose(0, 2, 1).reshape(128, 64)
    pr[:, 4, 0] = np.repeat(ldt, 16)
    pr[:, 4, 1] = inp["s5_d"][layer][128 * c:128 * c + 128]
    pc = np.zeros((128, 4, 128), np.float32)
    cTre = cre.transpose(2, 0, 1).reshape(64, 128)
    cTim = cim.transpose(2, 0, 1).reshape(64, 128)
    pc[:64, 0, :] = cTre
    pc[64:, 0, :] = cTim
    pc[:64, 1, :] = cTim
    pc[64:, 1, :] = cTre
    pc[:, 2, 0:8] = np.tile(lre.T, (2, 1))
    pc[:, 2, 8:16] = np.tile(lim.T, (2, 1))
    pc[:, 2, 16:24] = ldt[None, :]
    return {"wu": wu, "pr": pr.reshape(128, 512), "pc": pc.reshape(128, 512), "cst": s5_consts()}


def emit_s5(nc, S, L, x_bf16, xT, wu, pr_d, pc_d, cst, yT):
    NR = max(1, L // 1024)
    TR = min(L, 1024)
    NSB = L // 512
    xq = "sp" if x_bf16 else "pool"

    with ExitStack() as es:
        C = Ctx(nc, es, S)
        W = C.sb([128, KC, 128], BF16, "W")
        XT = [C.sb([128, 8, 512], BF16, "XT") for _ in range(8)]
        PR = C.sb([128, 8, 64], F32, "PR")
        PC = C.sb([128, 4, 128], F32, "PC")
        CS = C.sb([128, 5, 256], F32, "CS")
        PS = C.psum_banks(8)
        ident = CS[:, 0, 0:128]
        swap = CS[:, 1, 0:128]
        iota = CS[:, 2, :]

        wv = wu.rearrange("(kc p) n -> p kc n", p=128)
        S.dma("pool", W[:], wv, writes=[W.b])
        S.dma("sp", PR[:].rearrange("p a b -> p (a b)"), pr_d, writes=[PR.b])
        S.dma("sp", PC[:].rearrange("p a b -> p (a b)"), pc_d, writes=[PC.b])
        S.dma("sp", CS[:].rearrange("p a b -> p (a b)"), cst, writes=[CS.b])

        n_tmp = [0]

        def tmp(shape, dt=F32):
            n_tmp[0] += 1
            return C.sb(shape, dt, "tmp")

        def dve(fn, reads, writes):
            return S.op("dve", fn, reads=[t.b for t in reads], writes=[t.b for t in writes])

        def act(fn, reads, writes):
            return S.op("act", fn, reads=[t.b for t in reads], writes=[t.b for t in writes])

        sin_tmp = {}

        def sin_of(dst, ang, shift):
            key = tuple(dst.shape_)
            if key not in sin_tmp:
                sin_tmp[key] = (tmp(list(key)), tmp(list(key)))
            k, r = sin_tmp[key]
            dve(lambda e: e.tensor_scalar(out=k[:], in0=ang[:], scalar1=shift, scalar2=1.0 / TWO_PI, op0=ALU.add, op1=ALU.mult), [ang], [k])
            dve(lambda e: e.tensor_scalar(out=k[:], in0=k[:], scalar1=MAGIC, scalar2=MAGIC, op0=ALU.add, op1=ALU.subtract), [k], [k])
            dve(lambda e: e.scalar_tensor_tensor(out=r[:], in0=k[:], scalar=-TWO_PI, in1=ang[:], op0=ALU.mult, op1=ALU.add), [k, ang], [r])
            dve(lambda e: e.tensor_scalar(out=r[:], in0=r[:], scalar1=shift, scalar2=3.1415925, op0=ALU.add, op1=ALU.min), [r], [r])
            dve(lambda e: e.tensor_scalar(out=r[:], in0=r[:], scalar1=-3.1415925, scalar2=None, op0=ALU.max), [r], [r])
            act(lambda e: e.activation(out=dst[:], in_=r[:], func=AF.Sin), [r], [dst])

        def dst_shape(t):
            return t.shape_

        def mk(shape, dt=F32):
            t = tmp(shape, dt)
            t.shape_ = shape
            return t

        dtc = mk([128, 1])
        act(lambda e: e.activation(out=dtc[:], in_=PR[:, 4, 0:1], func=AF.Exp), [PR], [dtc])
        lrd = mk([128, 64]); lid = mk([128, 64]); mag = mk([128, 64]); sn = mk([128, 64]); cs_ = mk([128, 64])
        dve(lambda e: e.tensor_scalar(out=lrd[:], in0=PR[:, 0, :], scalar1=dtc[:, 0:1], scalar2=None, op0=ALU.mult), [PR, dtc], [lrd])
        dve(lambda e: e.tensor_scalar(out=lid[:], in0=PR[:, 1, :], scalar1=dtc[:, 0:1], scalar2=None, op0=ALU.mult), [PR, dtc], [lid])
        act(lambda e: e.activation(out=mag[:], in_=lrd[:], func=AF.Exp), [lrd], [mag])
        sin_of(sn, lid, 0.0)
        sin_of(cs_, lid, float(np.pi / 2))
        nr = mk([128, 64]); ni = mk([128, 64]); den = mk([128, 64]); t1 = mk([128, 64]); t2 = mk([128, 64])
        cre = mk([128, 64]); cim = mk([128, 64])
        dve(lambda e: e.tensor_tensor(out=nr[:], in0=mag[:], in1=cs_[:], op=ALU.mult), [mag, cs_], [nr])
        dve(lambda e: e.tensor_scalar(out=nr[:], in0=nr[:], scalar1=-1.0, scalar2=None, op0=ALU.add), [nr], [nr])
        dve(lambda e: e.tensor_tensor(out=ni[:], in0=mag[:], in1=sn[:], op=ALU.mult), [mag, sn], [ni])
        dve(lambda e: e.tensor_tensor(out=den[:], in0=PR[:, 0, :], in1=PR[:, 0, :], op=ALU.mult), [PR], [den])
        dve(lambda e: e.tensor_tensor(out=t1[:], in0=PR[:, 1, :], in1=PR[:, 1, :], op=ALU.mult), [PR], [t1])
        dve(lambda e: e.tensor_tensor(out=den[:], in0=den[:], in1=t1[:], op=ALU.add), [den, t1], [den])
        dve(lambda e: e.reciprocal(den[:], den[:]), [den], [den])
        dve(lambda e: e.tensor_tensor(out=t1[:], in0=nr[:], in1=PR[:, 0, :], op=ALU.mult), [nr, PR], [t1])
        dve(lambda e: e.tensor_tensor(out=t2[:], in0=ni[:], in1=PR[:, 1, :], op=ALU.mult), [ni, PR], [t2])
        dve(lambda e: e.tensor_tensor(out=t1[:], in0=t1[:], in1=t2[:], op=ALU.add), [t1, t2], [t1])
        dve(lambda e: e.tensor_tensor(out=cre[:], in0=t1[:], in1=den[:], op=ALU.mult), [t1, den], [cre])
        dve(lambda e: e.tensor_tensor(out=t1[:], in0=ni[:], in1=PR[:, 0, :], op=ALU.mult), [ni, PR], [t1])
        dve(lambda e: e.tensor_tensor(out=t2[:], in0=nr[:], in1=PR[:, 1, :], op=ALU.mult), [nr, PR], [t2])
        dve(lambda e: e.tensor_tensor(out=t1[:], in0=t1[:], in1=t2[:], op=ALU.subtract), [t1, t2], [t1])
        dve(lambda e: e.tensor_tensor(out=cim[:], in0=t1[:], in1=den[:], op=ALU.mult), [t1, den], [cim])
        BB1 = mk([128, 128]); BB2 = mk([128, 128])
        dve(lambda e: e.tensor_tensor(out=t1[:], in0=cre[:], in1=PR[:, 2, :], op=ALU.mult), [cre, PR], [t1])
        dve(lambda e: e.tensor_tensor(out=t2[:], in0=cim[:], in1=PR[:, 3, :], op=ALU.mult), [cim, PR], [t2])
        dve(lambda e: e.tensor_tensor(out=BB1[:, 0:64], in0=t1[:], in1=t2[:], op=ALU.subtract), [t1, t2], [BB1])
        dve(lambda e: e.tensor_tensor(out=t1[:], in0=cre[:], in1=PR[:, 3, :], op=ALU.mult), [cre, PR], [t1])
        dve(lambda e: e.tensor_tensor(out=t2[:], in0=cim[:], in1=PR[:, 2, :], op=ALU.mult), [cim, PR], [t2])
        dve(lambda e: e.tensor_tensor(out=BB1[:, 64:128], in0=t1[:], in1=t2[:], op=ALU.add), [t1, t2], [BB1])
        dve(lambda e: e.tensor_copy(BB2[:, 0:64], BB1[:, 64:128]), [BB1], [BB2])
        dve(lambda e: e.tensor_scalar(out=BB2[:, 64:128], in0=BB1[:, 0:64], scalar1=-1.0, scalar2=None, op0=ALU.mult), [BB1], [BB2])
        Bm1 = mk([128, GPC, 128], BF16); Bm2 = mk([128, GPC, 128], BF16)
        for g in range(GPC):
            dve(lambda e, g=g: e.tensor_scalar(out=Bm1[:, g, :], in0=BB1[:], scalar1=CS[:, 3, g:g + 1], scalar2=None, op0=ALU.mult), [BB1, CS], [Bm1])
            dve(lambda e, g=g: e.tensor_scalar(out=Bm2[:, g, :], in0=BB2[:], scalar1=CS[:, 3, g:g + 1], scalar2=None, op0=ALU.mult), [BB2, CS], [Bm2])
        W1 = mk([128, GPC, 128], BF16); W2 = mk([128, GPC, 128], BF16)
        dve(lambda e: e.memset(W1[:].rearrange("p a b -> p (a b)"), 0.0), [], [W1])
        dve(lambda e: e.memset(W2[:].rearrange("p a b -> p (a b)"), 0.0), [], [W2])
        for g in range(GPC):
            sl = slice(16 * g, 16 * g + 16)
            dve(lambda e, g=g, sl=sl: e.tensor_scalar(out=W1[:, g, sl], in0=PC[:, 0, sl], scalar1=CS[:, 3, 8:9], scalar2=None, op0=ALU.mult), [PC, CS], [W1])
            dve(lambda e, g=g, sl=sl: e.tensor_scalar(out=W2[:, g, sl], in0=PC[:, 1, sl], scalar1=CS[:, 3, 9:10], scalar2=None, op0=ALU.mult), [PC, CS], [W2])
        dt2 = mk([128, 8]); th = mk([128, 8]); rho = mk([128, 8]); lr2 = mk([128, 8])
        act(lambda e: e.activation(out=dt2[:], in_=PC[:, 2, 16:24], func=AF.Exp), [PC], [dt2])
        dve(lambda e: e.tensor_tensor(out=th[:], in0=PC[:, 2, 8:16], in1=dt2[:], op=ALU.mult), [PC, dt2], [th])
        dve(lambda e: e.tensor_tensor(out=lr2[:], in0=PC[:, 2, 0:8], in1=dt2[:], op=ALU.mult), [PC, dt2], [lr2])
        act(lambda e: e.activation(out=rho[:], in_=lr2[:], func=AF.Exp), [lr2], [rho])
        C2 = mk([128, GPC, S5C]); S2 = mk([128, GPC, S5C])
        ang = mk([128, S5C]); sg = mk([128, S5C]); cg = mk([128, S5C])
        for g in range(GPC):
            dve(lambda e, g=g: e.tensor_scalar(out=ang[:], in0=iota, scalar1=th[:, g:g + 1], scalar2=None, op0=ALU.mult), [CS, th], [ang])
            sin_of(sg, ang, 0.0)
            sin_of(cg, ang, float(np.pi / 2))
            dve(lambda e, g=g, sg=sg: e.tensor_copy(S2[:, g, :], sg[:]), [sg], [S2])
            dve(lambda e, g=g, cg=cg: e.tensor_copy(C2[:, g, :], cg[:]), [cg], [C2])
        angc = mk([128, 8]); crr = mk([128, 8]); srr = mk([128, 8])
        dve(lambda e: e.tensor_scalar(out=angc[:], in0=th[:], scalar1=float(S5C), scalar2=None, op0=ALU.mult), [th], [angc])
        sin_of(srr, angc, 0.0)
        sin_of(crr, angc, float(np.pi / 2))
        dve(lambda e: e.tensor_scalar(out=srr[:], in0=srr[:], scalar1=CS[:, 3, 8:9], scalar2=None, op0=ALU.mult), [srr, CS], [srr])
        ROT = mk([128, GPC, 128])
        for g in range(GPC):
            dve(lambda e, g=g: e.tensor_scalar(out=ROT[:, g, :], in0=ident, scalar1=crr[:, g:g + 1], scalar2=None, op0=ALU.mult), [CS, crr], [ROT])
            dve(lambda e, g=g: e.scalar_tensor_tensor(out=ROT[:, g, :], in0=swap, scalar=srr[:, g:g + 1], in1=ROT[:, g, :], op0=ALU.mult, op1=ALU.add),
                [CS, srr, ROT], [ROT])

        uT = [mk([128, 512]) for _ in range(2)]
        uTb = [mk([128, 512], BF16) for _ in range(2)]
        mbuf = [mk([128, S5C]) for _ in range(2)]
        tbuf = [mk([128, S5C]) for _ in range(2)]
        zeta = [mk([128, S5C]) for _ in range(2)]
        Zc = [mk([128, S5C], BF16) for _ in range(2)]
        Zs = [mk([128, S5C], BF16) for _ in range(2)]
        zl = [mk([128, 1]) for _ in range(GPC)]
        zi = [mk([128, 1]) for _ in range(GPC)]
        yf = [mk([128, S5C]) for _ in range(2)]
        yo = [mk([128, S5C], BF16) for _ in range(2)]
        gl = [(mk([128, S5C]), mk([128, S5C])) for _ in range(2)]
        proj_banks = BankPool(PS[0:2])
        p_banks = BankPool(PS[2:6])
        y_banks = BankPool(PS[6:8])
        xv = xT.rearrange("r (kc p) t -> r p kc t", p=128)
        rot = 0
        nchunk = 0
        for sb in range(NSB):
            r = (sb * 512) // TR
            t0 = (sb * 512) % TR
            xs = XT[4 * (sb % 2):4 * (sb % 2) + 4]
            for j in range(4):
                S.dma(xq, xs[j][:], xv[r, :, 8 * j:8 * j + 8, t0:t0 + 512], writes=[xs[j].b])
            ps = proj_banks.get()
            for kc in range(KC):
                S.op("pe", lambda e, kc=kc: e.matmul(ps[:, :], W[:, kc, :], xs[kc // 8][:, kc % 8, :], start=(kc == 0), stop=(kc == KC - 1)),
                     reads=[W.b, xs[kc // 8].b], writes=[ps.b])
            u_, ub_ = uT[sb % 2], uTb[sb % 2]
            act(lambda e: e.copy(u_[:], ps[:, :]), [ps], [u_])
            dve(lambda e: e.tensor_copy(ub_[:], ps[:, :]), [ps], [ub_])
            for cc in range(512 // S5C):
                csl = slice(cc * S5C, (cc + 1) * S5C)
                yps = y_banks.get()
                for g in range(GPC):
                    pb = p_banks.get()
                    m_, t_, z_, zc_, zs_ = mbuf[rot], tbuf[rot], zeta[rot], Zc[rot], Zs[rot]
                    rot ^= 1
                    S.op("pe", lambda e, g=g: e.matmul(pb[:, 0:S5C], Bm1[:, g, :], ub_[:, csl], start=True, stop=True), reads=[Bm1.b, ub_.b], writes=[pb.b])
                    S.op("pe", lambda e, g=g: e.matmul(pb[:, S5C:2 * S5C], Bm2[:, g, :], ub_[:, csl], start=True, stop=True), reads=[Bm2.b, ub_.b], writes=[pb.b])
                    dve(lambda e, g=g: e.tensor_tensor(out=m_[:], in0=pb[:, 0:S5C], in1=C2[:, g, :], op=ALU.mult), [pb, C2], [m_])
                    dve(lambda e, g=g: e.tensor_tensor(out=t_[:], in0=pb[:, S5C:2 * S5C], in1=S2[:, g, :], op=ALU.mult), [pb, S2], [t_])
                    dve(lambda e: e.tensor_tensor(out=m_[:], in0=m_[:], in1=t_[:], op=ALU.add), [m_, t_], [m_])
                    if nchunk == 0:
                        dve(lambda e, g=g: e.tensor_tensor_scan(out=z_[:], data0=rho[:, g:g + 1].to_broadcast([128, S5C]), data1=m_[:], initial=0.0,
                                                              op0=ALU.mult, op1=ALU.add), [rho, m_], [z_])
                    else:
                        pr_ = p_banks.get()
                        S.op("pe", lambda e, g=g: e.matmul(pr_[:, 0:1], ROT[:, g, :], zl[g][:], start=True, stop=True), reads=[ROT.b, zl[g].b], writes=[pr_.b])
                        act(lambda e, g=g: e.copy(zi[g][:], pr_[:, 0:1]), [pr_], [zi[g]])
                        dve(lambda e, g=g: e.tensor_tensor_scan(out=z_[:], data0=rho[:, g:g + 1].to_broadcast([128, S5C]), data1=m_[:], initial=zi[g][:, 0:1],
                                                              op0=ALU.mult, op1=ALU.add), [rho, m_, zi[g]], [z_])
                    dve(lambda e, g=g: e.tensor_copy(zl[g][:], z_[:, S5C - 1:S5C]), [z_], [zl[g]])
                    dve(lambda e, g=g: e.tensor_tensor(out=zc_[:], in0=z_[:], in1=C2[:, g, :], op=ALU.mult), [z_, C2], [zc_])
                    dve(lambda e, g=g: e.tensor_tensor(out=zs_[:], in0=z_[:], in1=S2[:, g, :], op=ALU.mult), [z_, S2], [zs_])
                    S.op("pe", lambda e, g=g: e.matmul(yps[:, 0:S5C], W1[:, g, :], zc_[:], start=(g == 0), stop=False), reads=[W1.b, zc_.b], writes=[yps.b])
                    S.op("pe", lambda e, g=g: e.matmul(yps[:, 0:S5C], W2[:, g, :], zs_[:], start=False, stop=(g == GPC - 1)), reads=[W2.b, zs_.b], writes=[yps.b])
                yf_, yo_ = yf[nchunk % 2], yo[nchunk % 2]
                dve(lambda e: e.scalar_tensor_tensor(out=yf_[:], in0=u_[:, csl], scalar=PR[:, 4, 1:2], in1=yps[:, 0:S5C], op0=ALU.mult, op1=ALU.add),
                    [u_, PR, yps], [yf_])
                g1, g2 = gl[nchunk % 2]
                dve(lambda e: e.tensor_tensor(out=g1[:], in0=yf_[:], in1=yf_[:], op=ALU.mult), [yf_], [g1])
                dve(lambda e: e.tensor_scalar(out=g1[:], in0=g1[:], scalar1=0.044715, scalar2=1.0, op0=ALU.mult, op1=ALU.add), [g1], [g1])
                dve(lambda e: e.tensor_tensor(out=g1[:], in0=g1[:], in1=yf_[:], op=ALU.mult), [g1, yf_], [g1])
                act(lambda e: e.activation(out=g2[:], in_=g1[:], func=AF.Sigmoid, scale=float(2.0 * np.sqrt(2.0 / np.pi))), [g1], [g2])
                dve(lambda e: e.tensor_tensor(out=yo_[:], in0=yf_[:], in1=g2[:], op=ALU.mult), [yf_, g2], [yo_])
                S.dma("sp", yT[:, sb * 512 + cc * S5C: sb * 512 + (cc + 1) * S5C], yo_[:], reads=[yo_.b])
                nchunk += 1
        S.barrier()


def build_mixer(L, x_bf16, do_gdn=True, do_s5=True):
    NR = max(1, L // 1024)
    TR = min(L, 1024)
    nc = bass.Bass("TRN2", target_bir_lowering=False)
    xT = nc.dram_tensor("xT", [NR, D_MODEL, TR], BF16 if x_bf16 else F32, kind="ExternalInput").ap()
    wg = nc.dram_tensor("wg", [D_MODEL, GDN_COLS], F32, kind="ExternalInput").ap()
    convw = nc.dram_tensor("convw", [128, 36], F32, kind="ExternalInput").ap()
    hp = nc.dram_tensor("hp", [128, 64], F32, kind="ExternalInput").ap()
    cstg = nc.dram_tensor("cstg", [128, 8 * 128], F32, kind="ExternalInput").ap()
    wu = nc.dram_tensor("wu", [D_MODEL, 128], F32, kind="ExternalInput").ap()
    pr_d = nc.dram_tensor("pr", [128, 512], F32, kind="ExternalInput").ap()
    pc_d = nc.dram_tensor("pc", [128, 512], F32, kind="ExternalInput").ap()
    csts = nc.dram_tensor("csts", [128, 5 * 256], F32, kind="ExternalInput").ap()
    yT = nc.dram_tensor("yT", [512, L], BF16, kind="ExternalOutput").ap()
    with ExitStack() as outer:
        S = Sched(nc, outer)
        if do_gdn:
            emit_gdn(nc, S, L, x_bf16, xT, wg, convw, hp, cstg, yT[0:384, :])
        if do_s5:
            emit_s5(nc, S, L, x_bf16, xT, wu, pr_d, pc_d, csts, yT[384:512, :])
        S.barrier()
    return nc


def prep_mixer(c, layer, inp):
    g = prep_gdn(c, layer, inp)
    s_ = prep_s5(c, layer, inp)
    return {"wg": g["wg"], "convw": g["convw"], "hp": g["hp"], "cstg": g["cst"],
            "wu": s_["wu"], "pr": s_["pr"], "pc": s_["pc"], "csts": s_["cst"]}


TPC = 1024
NT = TPC // 128
BIG = 1.0e30


def b_consts():
    c = np.zeros((128, 4, 128), np.float32)
    c[:, 0, :] = np.eye(128)
    c[:, 1, :] = 1.0
    k = np.arange(128)
    c[:, 2, :] = (k[:, None] < k[None, :])
    c[:, 3, :] = k[None, :]
    return c.reshape(128, 512)


def emit_consts(S, C, cst):
    CS = C.sb([128, 4, 128], F32, "CS")
    CSb = C.sb([128, 3, 128], BF16, "CSb")
    S.dma("sp", CS[:].rearrange("p a b -> p (a b)"), cst, writes=[CS.b])
    for j in range(3):
        S.op("dve", lambda e, j=j: e.tensor_copy(CSb[:, j, :], CS[:, j, :]), reads=[CS.b], writes=[CSb.b])
    return CS, CSb


def emit_proj_res(S, C, PS, lhs_fn, nk, rhs_view, rhs_f32, resid, resid_bufs, H, Hbufs):
    Wn = [C.sb([128, nk, 512], BF16, "Wn") for _ in range(2)]
    xt = [C.sb([128, 512], F32, "xt") for _ in range(3)]
    ht = [C.sb([128, 512], F32, "ht") for _ in range(3)]
    banks = BankPool(PS[0:4])
    q = "pool" if rhs_f32 else "sp"
    cnt = 0
    for n in range(8):
        w = Wn[n % 2]
        for j in range(4):
            ks = slice(j * nk // 4, (j + 1) * nk // 4)
            S.dma(q, w[:, ks, :], rhs_view[:, ks, n * 512:(n + 1) * 512], writes=[w.b])
        for i in range(NT):
            ps = banks.get()
            for k in range(nk):
                ap, b = lhs_fn(k, i)
                S.op("pe", lambda e, ap=ap, k=k: e.matmul(ps[:, :], ap, w[:, k, :], start=(k == 0), stop=(k == nk - 1)),
                     reads=[b, w.b], writes=[ps.b])
            x_, h_ = xt[cnt % 3], ht[cnt % 3]
            cnt += 1
            S.dma("sp", x_[:], resid[i * 128:(i + 1) * 128, n * 512:(n + 1) * 512], reads=[resid_bufs[i]], writes=[x_.b])
            S.op("dve", lambda e: e.scalar_tensor_tensor(out=h_[:], in0=x_[:], scalar=float(DN_ALPHA), in1=ps[:, :], op0=ALU.mult, op1=ALU.add),
                 reads=[x_.b, ps.b], writes=[h_.b])
            S.dma("sp", H[i * 128:(i + 1) * 128, n * 512:(n + 1) * 512], h_[:], reads=[h_.b], writes=[Hbufs[i]])


def emit_ln_tiles(S, C, H, Hbufs, lng, lnb, out_cb, eps=LN_EPS):
    G = C.sb([128, D_MODEL], F32, "lnG")
    Bt = C.sb([128, D_MODEL], F32, "lnB")
    S.dma("sp", G[:], lng, writes=[G.b])
    S.dma("sp", Bt[:], lnb, writes=[Bt.b])
    tiles = [C.sb([128, D_MODEL], F32, "lt") for _ in range(2)]
    junk = C.sb([128, D_MODEL], BF16, "junk")
    s1 = C.sb([128, 1], F32); nm = C.sb([128, 1], F32); s2 = C.sb([128, 1], F32); rt = C.sb([128, 1], F32); rstd = C.sb([128, 1], F32)
    for i in range(NT):
        t = tiles[i % 2]
        S.dma("sp", t[:], H[i * 128:(i + 1) * 128, :], reads=[Hbufs[i]], writes=[t.b])
        S.op("dve", lambda e: e.reduce_sum(out=s1[:], in_=t[:], axis=AX.X), reads=[t.b], writes=[s1.b])
        S.op("dve", lambda e: e.tensor_scalar(out=nm[:], in0=s1[:], scalar1=-1.0 / D_MODEL, scalar2=None, op0=ALU.mult), reads=[s1.b], writes=[nm.b])
        S.op("act", lambda e: e.activation(out=junk[:], in_=t[:], func=AF.Square, bias=nm[:, 0:1], accum_out=s2[:]), reads=[t.b, nm.b], writes=[junk.b, s2.b])
        S.op("act", lambda e: e.activation(out=rt[:], in_=s2[:], func=AF.Sqrt, bias=eps, scale=1.0 / D_MODEL), reads=[s2.b], writes=[rt.b])
        S.op("dve", lambda e: e.reciprocal(rstd[:], rt[:]), reads=[rt.b], writes=[rstd.b])
        S.op("dve", lambda e: e.tensor_scalar(out=t[:], in0=t[:], scalar1=nm[:, 0:1], scalar2=rstd[:, 0:1], op0=ALU.add, op1=ALU.mult),
             reads=[t.b, nm.b, rstd.b], writes=[t.b])
        S.op("dve", lambda e: e.tensor_tensor(out=t[:], in0=t[:], in1=G[:], op=ALU.mult), reads=[t.b, G.b], writes=[t.b])
        S.op("dve", lambda e: e.tensor_tensor(out=t[:], in0=t[:], in1=Bt[:], op=ALU.add), reads=[t.b, Bt.b], writes=[t.b])
        out_cb(i, t)


def emit_transpose_tile(S, banks, src, identb, CSb, dst_fn, eng_alt=[0]):
    for g in range(4):
        ps = banks.get()
        psb = ps[:].bitcast(BF16)
        for j in range(8):
            kc = g * 8 + j
            S.op("pe", lambda e, j=j, kc=kc: e.transpose(psb[:, j * 128:(j + 1) * 128], src[:, kc * 128:(kc + 1) * 128], identb),
                 reads=[src.b, CSb.b], writes=[ps.b])
        ap, b = dst_fn(g)
        en = "act" if (eng_alt[0] % 2 == 0) else "dve"
        eng_alt[0] += 1
        if en == "act":
            S.op("act", lambda e: e.copy(ap, psb[:, 0:1024].rearrange("p (a b) -> p a b", a=8)), reads=[ps.b], writes=[b])
        else:
            S.op("dve", lambda e: e.tensor_copy(ap, psb[:, 0:1024].rearrange("p (a b) -> p a b", a=8)), reads=[ps.b], writes=[b])


def build_t0():
    nc = bass.Bass("TRN2", target_bir_lowering=False)
    x = nc.dram_tensor("x", [TPC, D_MODEL], F32, kind="ExternalInput").ap()
    cst = nc.dram_tensor("cst", [128, 512], F32, kind="ExternalInput").ap()
    xT = nc.dram_tensor("xTo", [D_MODEL, TPC], BF16, kind="ExternalOutput").ap()
    with ExitStack() as outer:
        S = Sched(nc, outer)
        C = Ctx(nc, outer, S)
        PS = C.psum_banks(8)
        CS, CSb = emit_consts(S, C, cst)
        xTs = C.sb([128, KC, TPC], BF16, "xTs")
        xb = [C.sb([128, D_MODEL], BF16, "xb") for _ in range(2)]
        banks = BankPool(PS)
        for i in range(NT):
            S.dma("pool", xb[i % 2][:], x[i * 128:(i + 1) * 128, :], writes=[xb[i % 2].b])
            emit_transpose_tile(S, banks, xb[i % 2], CSb[:, 0, :], CSb,
                                lambda g, i=i: (xTs[:, g * 8:(g + 1) * 8, i * 128:(i + 1) * 128], xTs.b))
        S.dma("sp", xT.rearrange("(kc p) t -> p kc t", p=128), xTs[:], reads=[xTs.b])
        S.barrier()
    return nc


def build_b1():
    nc = bass.Bass("TRN2", target_bir_lowering=False)
    ymix = nc.dram_tensor("ymix", [D_MODEL, TPC], BF16, kind="ExternalInput").ap()
    wo = nc.dram_tensor("wo", [D_MODEL, D_MODEL], F32, kind="ExternalInput").ap()
    wglu = nc.dram_tensor("wglu", [S5_WIDTH, S5_WIDTH], F32, kind="ExternalInput").ap()
    x = nc.dram_tensor("x", [TPC, D_MODEL], F32, kind="ExternalInput").ap()
    lng = nc.dram_tensor("lng", [128, D_MODEL], F32, kind="ExternalInput").ap()
    lnb = nc.dram_tensor("lnb", [128, D_MODEL], F32, kind="ExternalInput").ap()
    wr = nc.dram_tensor("wr", [D_MODEL, 36], F32, kind="ExternalInput").ap()
    rb = nc.dram_tensor("rb", [128, 36], F32, kind="ExternalInput").ap()
    cst = nc.dram_tensor("cst", [128, 512], F32, kind="ExternalInput").ap()
    x1o = nc.dram_tensor("x1", [TPC, D_MODEL], F32, kind="ExternalOutput").ap()
    xg = nc.dram_tensor("xg", [N_EXPERTS, 128, KC, CAP], BF16, kind="ExternalOutput").ap()
    selw = nc.dram_tensor("selw", [128, N_EXPERTS, TPC], BF16, kind="ExternalOutput").ap()
    H = nc.dram_tensor("Hscr", [TPC, D_MODEL], F32, kind="Internal").ap()
    with ExitStack() as outer:
        S = Sched(nc, outer)
        Hb = [Buf(f"H{i}") for i in range(NT)]
        xb_ = [Buf(f"xr{i}") for i in range(NT)]
        with ExitStack() as es:
            C = Ctx(nc, es, S)
            PS = C.psum_banks(8)
            Y = C.sb([128, KC, TPC], BF16, "Y")
            y2 = C.sb([128, 8, TPC], BF16, "y2")
            Wg = C.sb([128, 8, S5_WIDTH], BF16, "Wglu")
            sig = [C.sb([128, 512], F32, "sig") for _ in range(2)]
            yv = ymix.rearrange("(k p) t -> p k t", p=128)
            for j in range(4):
                S.dma("sp", Y[:, 8 * j:8 * j + 8, :], yv[:, 8 * j:8 * j + 8, :], writes=[Y.b])
            S.dma("pool", Wg[:], wglu.rearrange("(r p) n -> p r n", p=128), writes=[Wg.b])
            gb = BankPool(PS[4:8])
            n = 0
            for jc in range(8):
                for th in range(2):
                    ps = gb.get()
                    tsl = slice(th * 512, (th + 1) * 512)
                    for r in range(8):
                        S.op("pe", lambda e, r=r: e.matmul(ps[:, :], Wg[:, r, jc * 128:(jc + 1) * 128], Y[:, 4 * r + 3, tsl], start=(r == 0), stop=(r == 7)),
                             reads=[Wg.b, Y.b], writes=[ps.b])
                    sg = sig[n % 2]
                    n += 1
                    S.op("act", lambda e: e.activation(out=sg[:], in_=ps[:, :], func=AF.Sigmoid), reads=[ps.b], writes=[sg.b])
                    S.op("dve", lambda e: e.tensor_tensor(out=y2[:, jc, tsl], in0=Y[:, 4 * jc + 3, tsl], in1=sg[:], op=ALU.mult),
                         reads=[Y.b, sg.b], writes=[y2.b])

            def lhs_fn(k, i):
                tsl = slice(i * 128, (i + 1) * 128)
                if k % 4 == 3:
                    return y2[:, k // 4, tsl], y2.b
                return Y[:, k, tsl], Y.b

            emit_proj_res(S, C, PS, lhs_fn, KC, wo.rearrange("(k p) n -> p k n", p=128), True, x, xb_, H, Hb)
            S.barrier()
        with ExitStack() as es2:
            C = Ctx(nc, es2, S)
            PS = C.psum_banks(8)
            CS, CSb = emit_consts(S, C, cst)
            identb, onesb, lstr = CSb[:, 0, :], CSb[:, 1, :], CSb[:, 2, :]
            iota = CS[:, 3, :]
            X1b = C.sb([128, NT, D_MODEL], BF16, "X1b")
            X1bb = [Buf(f"x1b{i}") for i in range(NT)]
            M1 = [C.sb([128, 32], F32, "M1") for _ in range(NT)]
            M2 = [C.sb([128, 32], F32, "M2") for _ in range(NT)]
            MAf = [C.sb([128, 32], F32, "MAf") for _ in range(NT)]
            MA = [C.sb([128, 32], BF16, "MA") for _ in range(NT)]
            CWm = [C.sb([128, 32], F32, "CWm") for _ in range(NT)]
            POS = [C.sb([128, 32], F32, "POS") for _ in range(NT)]
            x1ob = [Buf(f"x1o{i}") for i in range(NT)]
            with ExitStack() as es2a:
                Ca = Ctx(nc, es2a, S)
                Wr = Ca.sb([128, KC, 36], BF16, "Wr")
                RB = Ca.sb([128, 36], F32, "RB")
                S.dma("pool", Wr[:], wr.rearrange("(k p) n -> p k n", p=128), writes=[Wr.b])
                S.dma("sp", RB[:], rb, writes=[RB.b])
                x1T = Ca.sb([128, KC, 128], BF16, "x1T")
                lg = Ca.sb([128, 36], F32, "lg")
                sm = {k: Ca.sb([128, 1], F32, k) for k in ("gmax", "ngmax", "se", "grp", "m1", "m2", "d", "ed", "den", "w1", "w2", "cw1", "cw2")}
                ex4 = Ca.sb([128, 4], F32); maskg = Ca.sb([128, 4], F32); pen = Ca.sb([128, 4], F32)
                elm = Ca.sb([128, 32], F32); elm2 = Ca.sb([128, 32], F32)
                tb = BankPool(PS[0:6])
                lb = BankPool(PS[6:8])

                def dv(fn, reads, writes):
                    S.op("dve", fn, reads=[t.b for t in reads], writes=[t.b for t in writes])

                def ac(fn, reads, writes):
                    S.op("act", fn, reads=[t.b for t in reads], writes=[t.b for t in writes])

                def ln_cb(i, t):
                    S.dma("sp", x1o[i * 128:(i + 1) * 128, :], t[:], reads=[t.b], writes=[x1ob[i]])
                    S.op("act", lambda e: e.copy(X1b[:, i, :], t[:]), reads=[t.b], writes=[X1bb[i]])
                    v = View(lambda k: X1b[:, i, k[1]], X1bb[i])
                    emit_transpose_tile(S, tb, v, identb, CSb, lambda g: (x1T[:, g * 8:(g + 1) * 8, :], x1T.b))
                    ps = lb.get()
                    for kc in range(KC):
                        S.op("pe", lambda e, kc=kc: e.matmul(ps[:, 0:36], x1T[:, kc, :], Wr[:, kc, :], start=(kc == 0), stop=(kc == KC - 1)),
                             reads=[x1T.b, Wr.b], writes=[ps.b])
                    dv(lambda e: e.tensor_tensor(out=lg[:], in0=ps[:, 0:36], in1=RB[:], op=ALU.add), [ps, RB], [lg])
                    dv(lambda e: e.reduce_max(out=sm["gmax"][:], in_=lg[:, 0:4], axis=AX.X), [lg], [sm["gmax"]])
                    dv(lambda e: e.tensor_scalar(out=sm["ngmax"][:], in0=sm["gmax"][:], scalar1=-1.0, scalar2=None, op0=ALU.mult), [sm["gmax"]], [sm["ngmax"]])
                    ac(lambda e: e.activation(out=ex4[:], in_=lg[:, 0:4], func=AF.Exp, bias=sm["ngmax"][:, 0:1], accum_out=sm["se"][:]), [lg, sm["ngmax"]], [ex4, sm["se"]])
                    dv(lambda e: e.reciprocal(sm["grp"][:], sm["se"][:]), [sm["se"]], [sm["grp"]])
                    dv(lambda e: e.tensor_scalar(out=maskg[:], in0=lg[:, 0:4], scalar1=sm["gmax"][:, 0:1], scalar2=None, op0=ALU.is_equal), [lg, sm["gmax"]], [maskg])
                    dv(lambda e: e.tensor_scalar(out=pen[:], in0=maskg[:], scalar1=-1.0, scalar2=BIG, op0=ALU.add, op1=ALU.mult), [maskg], [pen])
                    for g in range(4):
                        dv(lambda e, g=g: e.tensor_scalar(out=elm[:, 8 * g:8 * g + 8], in0=lg[:, 4 + 8 * g:12 + 8 * g], scalar1=pen[:, g:g + 1], scalar2=None, op0=ALU.add),
                           [lg, pen], [elm])
                    dv(lambda e: e.reduce_max(out=sm["m1"][:], in_=elm[:], axis=AX.X), [elm], [sm["m1"]])
                    dv(lambda e: e.tensor_scalar(out=M1[i][:], in0=elm[:], scalar1=sm["m1"][:, 0:1], scalar2=None, op0=ALU.is_equal), [elm, sm["m1"]], [M1[i]])
                    dv(lambda e: e.scalar_tensor_tensor(out=elm2[:], in0=M1[i][:], scalar=-BIG, in1=elm[:], op0=ALU.mult, op1=ALU.add), [M1[i], elm], [elm2])
                    dv(lambda e: e.reduce_max(out=sm["m2"][:], in_=elm2[:], axis=AX.X), [elm2], [sm["m2"]])
                    dv(lambda e: e.tensor_scalar(out=M2[i][:], in0=elm2[:], scalar1=sm["m2"][:, 0:1], scalar2=None, op0=ALU.is_equal), [elm2, sm["m2"]], [M2[i]])
                    dv(lambda e: e.tensor_tensor(out=sm["d"][:], in0=sm["m2"][:], in1=sm["m1"][:], op=ALU.subtract), [sm["m2"], sm["m1"]], [sm["d"]])
                    ac(lambda e: e.activation(out=sm["ed"][:], in_=sm["d"][:], func=AF.Exp), [sm["d"]], [sm["ed"]])
                    dv(lambda e: e.tensor_scalar(out=sm["den"][:], in0=sm["ed"][:], scalar1=1.0, scalar2=None, op0=ALU.add), [sm["ed"]], [sm["den"]])
                    dv(lambda e: e.reciprocal(sm["w1"][:], sm["den"][:]), [sm["den"]], [sm["w1"]])
                    dv(lambda e: e.tensor_tensor(out=sm["w2"][:], in0=sm["ed"][:], in1=sm["w1"][:], op=ALU.mult), [sm["ed"], sm["w1"]], [sm["w2"]])
                    dv(lambda e: e.tensor_tensor(out=sm["cw1"][:], in0=sm["w1"][:], in1=sm["grp"][:], op=ALU.mult), [sm["w1"], sm["grp"]], [sm["cw1"]])
                    dv(lambda e: e.tensor_tensor(out=sm["cw2"][:], in0=sm["w2"][:], in1=sm["grp"][:], op=ALU.mult), [sm["w2"], sm["grp"]], [sm["cw2"]])
                    dv(lambda e: e.tensor_tensor(out=MAf[i][:], in0=M1[i][:], in1=M2[i][:], op=ALU.add), [M1[i], M2[i]], [MAf[i]])
                    dv(lambda e: e.tensor_copy(MA[i][:], MAf[i][:]), [MAf[i]], [MA[i]])
                    dv(lambda e: e.tensor_scalar(out=CWm[i][:], in0=M1[i][:], scalar1=sm["cw1"][:, 0:1], scalar2=None, op0=ALU.mult), [M1[i], sm["cw1"]], [CWm[i]])
                    dv(lambda e: e.scalar_tensor_tensor(out=CWm[i][:], in0=M2[i][:], scalar=sm["cw2"][:, 0:1], in1=CWm[i][:], op0=ALU.mult, op1=ALU.add),
                       [M2[i], sm["cw2"], CWm[i]], [CWm[i]])

                emit_ln_tiles(S, Ca, H, Hb, lng, lnb, ln_cb)
                for i in range(NT):
                    ps = lb.get()
                    for i2 in range(i):
                        S.op("pe", lambda e, i2=i2: e.matmul(ps[:, 0:32], onesb, MA[i2][:], start=(i2 == 0), stop=False), reads=[CSb.b, MA[i2].b], writes=[ps.b])
                    S.op("pe", lambda e: e.matmul(ps[:, 0:32], lstr, MA[i][:], start=(i == 0), stop=True), reads=[CSb.b, MA[i].b], writes=[ps.b])
                    S.op("act", lambda e: e.copy(POS[i][:], ps[:, 0:32]), reads=[ps.b], writes=[POS[i].b])
                S.barrier()
            with ExitStack() as es2b:
                Cb = Ctx(nc, es2b, S)
                SELW = Cb.sb([128, N_EXPERTS, TPC], BF16, "SELW")
                Sel = [[Cb.sb([128, CAP], BF16, "Sel") for _ in range(NT)] for _ in range(2)]
                Swt = [Cb.sb([128, CAP], BF16, "Swt") for _ in range(4)]
                XG = [Cb.sb([128, KC, CAP], BF16, "XG") for _ in range(2)]
                gbk = BankPool(PS[0:5])
                sbk = BankPool(PS[5:8])
                nsw = 0
                for ex in range(N_EXPERTS):
                    sel = Sel[ex % 2]
                    ps_t = sbk.get()
                    ps_tb = ps_t[:].bitcast(BF16)
                    for i in range(NT):
                        S.op("dve", lambda e, i=i: e.tensor_scalar(out=sel[i][:], in0=iota, scalar1=POS[i][:, ex:ex + 1], scalar2=MAf[i][:, ex:ex + 1],
                                                                   op0=ALU.is_equal, op1=ALU.mult), reads=[CS.b, POS[i].b, MAf[i].b], writes=[sel[i].b])
                        sw = Swt[nsw % 4]
                        nsw += 1
                        S.op("dve", lambda e, i=i, sw=sw: e.tensor_scalar(out=sw[:], in0=iota, scalar1=POS[i][:, ex:ex + 1], scalar2=CWm[i][:, ex:ex + 1],
                                                                          op0=ALU.is_equal, op1=ALU.mult), reads=[CS.b, POS[i].b, CWm[i].b], writes=[sw.b])
                        S.op("pe", lambda e, i=i, sw=sw: e.transpose(ps_tb[:, i * 128:(i + 1) * 128], sw[:], identb), reads=[sw.b, CSb.b], writes=[ps_t.b])
                    S.op("act", lambda e: e.copy(SELW[:, ex, :], ps_tb[:, 0:1024]), reads=[ps_t.b], writes=[SELW.b])
                    xg_ = XG[ex % 2]
                    for g in range(8):
                        ps = gbk.get()
                        for j in range(4):
                            kc = g * 4 + j
                            for i in range(NT):
                                S.op("pe", lambda e, i=i, j=j, kc=kc: e.matmul(ps[:, j * 128:(j + 1) * 128], X1b[:, i, kc * 128:(kc + 1) * 128], sel[i][:],
                                                                            start=(i == 0), stop=(i == NT - 1)),
                                     reads=[X1bb[i], sel[i].b], writes=[ps.b])
                        if g % 2 == 0:
                            S.op("act", lambda e, g=g: e.copy(xg_[:, 4 * g:4 * g + 4, :], ps[:, :].rearrange("p (a b) -> p a b", a=4)), reads=[ps.b], writes=[xg_.b])
                        else:
                            S.op("dve", lambda e, g=g: e.tensor_copy(xg_[:, 4 * g:4 * g + 4, :], ps[:, :].rearrange("p (a b) -> p a b", a=4)), reads=[ps.b], writes=[xg_.b])
                    S.dma("sp", xg[ex], xg_[:], reads=[xg_.b])
                S.dma("sp", selw, SELW[:], reads=[SELW.b])
                S.barrier()
        S.barrier()
    return nc


EPC = N_EXPERTS // NCORES
SLOTS = NCORES * CAP


def build_b2():
    nc = bass.Bass("TRN2", target_bir_lowering=False)
    xg = nc.dram_tensor("xg", [EPC, 128, KC, SLOTS], BF16, kind="ExternalInput").ap()
    wg_ = nc.dram_tensor("wg", [EPC, D_MODEL, D_EXPERT], F32, kind="ExternalInput").ap()
    wu_ = nc.dram_tensor("wu", [EPC, D_MODEL, D_EXPERT], F32, kind="ExternalInput").ap()
    wd_ = nc.dram_tensor("wd", [EPC, D_EXPERT, D_MODEL], F32, kind="ExternalInput").ap()
    cst = nc.dram_tensor("cst", [128, 512], F32, kind="ExternalInput").ap()
    yo = nc.dram_tensor("yo", [EPC, SLOTS, D_MODEL], BF16, kind="ExternalOutput").ap()
    NST = SLOTS // 128
    with ExitStack() as outer:
        S = Sched(nc, outer)
        C = Ctx(nc, outer, S)
        PS = C.psum_banks(8)
        CS, CSb = emit_consts(S, C, cst)
        identb = CSb[:, 0, :]
        Wg = C.sb([128, KC, D_EXPERT], BF16, "Wg")
        Wu = C.sb([128, KC, D_EXPERT], BF16, "Wu")
        Wd = C.sb([128, 4, D_MODEL], BF16, "Wd")
        X = C.sb([128, KC, SLOTS], BF16, "X")
        hidT = C.sb([128, 4, SLOTS], BF16, "hidT")
        sg = [C.sb([128, 512], F32, "sg") for _ in range(2)]
        hid = [C.sb([128, 512], BF16, "hid") for _ in range(2)]
        Yt = [C.sb([128, D_MODEL], BF16, "Yt") for _ in range(2)]
        gub = BankPool(PS[0:4])
        tbk = BankPool(PS[4:5])
        dbk = BankPool(PS[5:8])
        ny = 0
        for ex in range(EPC):
            wgv = wg_[ex].rearrange("(k p) n -> p k n", p=128)
            wuv = wu_[ex].rearrange("(k p) n -> p k n", p=128)
            wdv = wd_[ex].rearrange("(m p) n -> p m n", p=128)
            for j in range(4):
                S.dma("pool", Wg[:, 8 * j:8 * j + 8, :], wgv[:, 8 * j:8 * j + 8, :], writes=[Wg.b])
            for j in range(4):
                S.dma("pool", Wu[:, 8 * j:8 * j + 8, :], wuv[:, 8 * j:8 * j + 8, :], writes=[Wu.b])
            for j in range(4):
                S.dma("sp", X[:, 8 * j:8 * j + 8, :], xg[ex][:, 8 * j:8 * j + 8, :], writes=[X.b])
            for j in range(4):
                S.dma("pool", Wd[:, j, :], wdv[:, j, :], writes=[Wd.b])
            for st in range(NST):
                ssl = slice(st * 128, (st + 1) * 128)
                pg = gub.get()
                pu = gub.get()
                for kc in range(KC):
                    S.op("pe", lambda e, kc=kc: e.matmul(pg[:, :], X[:, kc, ssl], Wg[:, kc, :], start=(kc == 0), stop=(kc == KC - 1)), reads=[X.b, Wg.b], writes=[pg.b])
                for kc in range(KC):
                    S.op("pe", lambda e, kc=kc: e.matmul(pu[:, :], X[:, kc, ssl], Wu[:, kc, :], start=(kc == 0), stop=(kc == KC - 1)), reads=[X.b, Wu.b], writes=[pu.b])
                s_, h_ = sg[st % 2], hid[st % 2]
                S.op("act", lambda e: e.activation(out=s_[:], in_=pg[:, :], func=AF.Silu), reads=[pg.b], writes=[s_.b])
                S.op("dve", lambda e: e.tensor_tensor(out=h_[:], in0=s_[:], in1=pu[:, :], op=ALU.mult), reads=[s_.b, pu.b], writes=[h_.b])
                pt = tbk.get()
                ptb = pt[:].bitcast(BF16)
                for mc in range(4):
                    S.op("pe", lambda e, mc=mc: e.transpose(ptb[:, mc * 128:(mc + 1) * 128], h_[:, mc * 128:(mc + 1) * 128], identb), reads=[h_.b, CSb.b], writes=[pt.b])
                S.op("act", lambda e: e.copy(hidT[:, :, ssl], ptb[:, 0:512].rearrange("p (a b) -> p a b", a=4)), reads=[pt.b], writes=[hidT.b])
            for st in range(NST):
                ssl = slice(st * 128, (st + 1) * 128)
                y_ = Yt[ny % 2]
                ny += 1
                for n in range(8):
                    pd = dbk.get()
                    for mc in range(4):
                        S.op("pe", lambda e, mc=mc: e.matmul(pd[:, :], hidT[:, mc, ssl], Wd[:, mc, n * 512:(n + 1) * 512], start=(mc == 0), stop=(mc == 3)),
                             reads=[hidT.b, Wd.b], writes=[pd.b])
                    if n % 2 == 0:
                        S.op("act", lambda e: e.copy(y_[:, n * 512:(n + 1) * 512], pd[:, :]), reads=[pd.b], writes=[y_.b])
                    else:
                        S.op("dve", lambda e: e.tensor_copy(y_[:, n * 512:(n + 1) * 512], pd[:, :]), reads=[pd.b], writes=[y_.b])
                S.dma("sp", yo[ex, ssl, :], y_[:], reads=[y_.b])
        S.barrier()
    return nc


def build_b3():
    nc = bass.Bass("TRN2", target_bir_lowering=False)
    yin = nc.dram_tensor("yin", [N_EXPERTS * CAP, D_MODEL], BF16, kind="ExternalInput").ap()
    selw = nc.dram_tensor("selw", [128, N_EXPERTS, TPC], BF16, kind="ExternalInput").ap()
    x1 = nc.dram_tensor("x1", [TPC, D_MODEL], F32, kind="ExternalInput").ap()
    lng = nc.dram_tensor("lng", [128, D_MODEL], F32, kind="ExternalInput").ap()
    lnb = nc.dram_tensor("lnb", [128, D_MODEL], F32, kind="ExternalInput").ap()
    cst = nc.dram_tensor("cst", [128, 512], F32, kind="ExternalInput").ap()
    x2 = nc.dram_tensor("x2", [TPC, D_MODEL], F32, kind="ExternalOutput").ap()
    xTo = nc.dram_tensor("xTo", [D_MODEL, TPC], BF16, kind="ExternalOutput").ap()
    H = nc.dram_tensor("Hscr", [TPC, D_MODEL], F32, kind="Internal").ap()
    with ExitStack() as outer:
        S = Sched(nc, outer)
        Hb = [Buf(f"H{i}") for i in range(NT)]
        xb_ = [Buf(f"xr{i}") for i in range(NT)]
        with ExitStack() as es:
            C = Ctx(nc, es, S)
            PS = C.psum_banks(8)
            SW = C.sb([128, N_EXPERTS, TPC], BF16, "SW")
            for j in range(4):
                S.dma("sp", SW[:, 8 * j:8 * j + 8, :], selw[:, 8 * j:8 * j + 8, :], writes=[SW.b])
            emit_proj_res(S, C, PS, lambda k, i: (SW[:, k, i * 128:(i + 1) * 128], SW.b), N_EXPERTS,
                          yin.rearrange("(e s) d -> s e d", s=128), False, x1, xb_, H, Hb)
            S.barrier()
        with ExitStack() as es2:
            C = Ctx(nc, es2, S)
            PS = C.psum_banks(8)
            CS, CSb = emit_consts(S, C, cst)
            xTs = C.sb([128, KC, TPC], BF16, "xTs")
            xb = [C.sb([128, D_MODEL], BF16, "xb") for _ in range(2)]
            banks = BankPool(PS)
            x2b = [Buf(f"x2{i}") for i in range(NT)]

            def cb(i, t):
                S.dma("sp", x2[i * 128:(i + 1) * 128, :], t[:], reads=[t.b], writes=[x2b[i]])
                b_ = xb[i % 2]
                S.op("act", lambda e: e.copy(b_[:], t[:]), reads=[t.b], writes=[b_.b])
                emit_transpose_tile(S, banks, b_, CSb[:, 0, :], CSb, lambda g: (xTs[:, g * 8:(g + 1) * 8, i * 128:(i + 1) * 128], xTs.b))

            emit_ln_tiles(S, C, H, Hb, lng, lnb, cb)
            S.dma("sp", xTo.rearrange("(kc p) t -> p kc t", p=128), xTs[:], reads=[xTs.b])
            S.barrier()
        S.barrier()
    return nc


_NC_CACHE = {}
_DBG = None


def _get_nc(name, fn):
    if name not in _NC_CACHE:
        _NC_CACHE[name] = fn()
    return _NC_CACHE[name]


def _run(nc, maps):
    res = run_bass_kernel_spmd(nc, maps, core_ids=list(range(len(maps))))
    return res.results


def mix_perm():
    p = []
    for r in range(NCORES):
        p.append(np.arange(384 * r, 384 * r + 384))
        p.append(GDN_WIDTH + np.arange(128 * r, 128 * r + 128))
    return np.concatenate(p)


def prep_b1_weights(layer, inp):
    wo = np.ascontiguousarray(inp["w_out"][layer][mix_perm(), :])
    wglu = np.ascontiguousarray(inp["s5_w_glu"][layer])
    wr = np.concatenate([inp["router_group_w"][layer]] + [inp["router_expert_w"][layer][g] for g in range(4)], axis=1)
    rbv = np.concatenate([inp["router_group_b"][layer], inp["router_expert_b"][layer].reshape(-1)])
    return {"wo": wo, "wglu": wglu, "wr": np.ascontiguousarray(wr.astype(np.float32)),
            "rb": np.ascontiguousarray(np.broadcast_to(rbv[None, :], (128, 36))).astype(np.float32),
            "lng": np.ascontiguousarray(np.broadcast_to(inp["ln1_g"][layer][None, :], (128, D_MODEL))),
            "lnb": np.ascontiguousarray(np.broadcast_to(inp["ln1_b"][layer][None, :], (128, D_MODEL))),
            "cst": b_consts()}


def run_layer(layer, inp, xT_all, xres, ncores=NCORES, L=SEQ):
    ncm = _get_nc(("mixer", L), lambda: build_mixer(L, True))
    maps = []
    for c in range(NCORES):
        m = prep_mixer(c, layer, inp)
        m["xT"] = xT_all
        maps.append(m)
    res = _run(ncm, maps)
    ymix_all = np.concatenate([np.asarray(res[c]["yT"]) for c in range(NCORES)], axis=0)
    del res, maps
    ntc = L // TPC
    w1 = prep_b1_weights(layer, inp)
    maps = []
    for c in range(ntc):
        m = dict(w1)
        m["ymix"] = np.ascontiguousarray(ymix_all[:, c * TPC:(c + 1) * TPC])
        m["x"] = xres[c]
        maps.append(m)
    res = _run(_get_nc("b1", build_b1), maps)
    x1 = [np.asarray(res[c]["x1"]) for c in range(ntc)]
    if _DBG is not None:
        _DBG["ymix"] = ymix_all
        _DBG["x1"] = x1
    xg = [np.asarray(res[c]["xg"]) for c in range(ntc)]
    selw = [np.asarray(res[c]["selw"]) for c in range(ntc)]
    del res, maps
    maps = []
    for c2 in range(NCORES):
        xin = np.zeros((EPC, 128, KC, SLOTS), ml_dtypes.bfloat16)
        for c in range(ntc):
            xin[:, :, :, c * CAP:(c + 1) * CAP] = xg[c][EPC * c2:EPC * c2 + EPC]
        maps.append({"xg": xin,
                     "wg": np.ascontiguousarray(inp["expert_w_gate"][layer][EPC * c2:EPC * c2 + EPC]),
                     "wu": np.ascontiguousarray(inp["expert_w_up"][layer][EPC * c2:EPC * c2 + EPC]),
                     "wd": np.ascontiguousarray(inp["expert_w_down"][layer][EPC * c2:EPC * c2 + EPC]),
                     "cst": b_consts()})
    del xg
    res = _run(_get_nc("b2", build_b2), maps)
    yo = [np.asarray(res[c2]["yo"]) for c2 in range(NCORES)]
    del res, maps
    lng = np.ascontiguousarray(np.broadcast_to(inp["ln2_g"][layer][None, :], (128, D_MODEL)))
    lnb = np.ascontiguousarray(np.broadcast_to(inp["ln2_b"][layer][None, :], (128, D_MODEL)))
    maps = []
    for c in range(ntc):
        yin = np.concatenate([yo[e // EPC][e % EPC, c * CAP:(c + 1) * CAP, :] for e in range(N_EXPERTS)], axis=0)
        maps.append({"yin": np.ascontiguousarray(yin), "selw": selw[c], "x1": x1[c], "lng": lng, "lnb": lnb, "cst": b_consts()})
    res = _run(_get_nc("b3", build_b3), maps)
    x2 = [np.asarray(res[c]["x2"]) for c in range(ntc)]
    xTn = [np.asarray(res[c]["xTo"]) for c in range(ntc)]
    return x2, xTn


def kernel(**inputs):
    inp = {k: np.asarray(v) for k, v in inputs.items()}
    x = inp["x"][0]
    xres = [np.ascontiguousarray(x[c * TPC:(c + 1) * TPC]) for c in range(NCORES)]
    res = _run(_get_nc("t0", build_t0), [{"x": xres[c], "cst": b_consts()} for c in range(NCORES)])
    xT_all = np.stack([np.asarray(res[c]["xTo"]) for c in range(NCORES)], axis=0)
    for layer in range(DEPTH):
        xres, xTn = run_layer(layer, inp, xT_all, xres)
        xT_all = np.stack(xTn, axis=0)
    return np.concatenate(xres, axis=0)[None].astype(np.float32)
```
